# Optimizing a Trainium2 kernel written in Bass

```python
import math
import jax
import jax.numpy as jnp
from jax import lax
import numpy as np


D_MODEL = 2048
BATCH = 4
SEQ = 2048
DEPTH = 4

GRID_W = 64
CTX_LEN = 256
RET_HEADS = 8
RET_QK_DIM = 64
RET_V_DIM = 128
DIFF_HEADS = 8
DIFF_QK_DIM = 64
DIFF_V_DIM = 128
RET_WIDTH = RET_HEADS * RET_V_DIM
DIFF_WIDTH = DIFF_HEADS * DIFF_V_DIM
MIX_WIDTH = RET_WIDTH + DIFF_WIDTH
PROJ_SPLITS = (RET_HEADS * RET_QK_DIM, RET_HEADS * RET_QK_DIM, RET_WIDTH, RET_WIDTH,
               DIFF_HEADS * 2 * DIFF_QK_DIM, DIFF_HEADS * 2 * DIFF_QK_DIM, DIFF_WIDTH)
PROJ_WIDTH = 512 + 512 + 1024 + 1024 + 1024 + 1024 + 1024
CHUNK = 128
QBLOCK = 128
N_EXPERTS = 16
EXPERT_FF = 1024
CAPACITY_FACTOR = 2
ROPE_BASE = 10000.0
EPS = 1e-6
N_MOD = 6

kernel_name = 'hybrid_retention_diffattn_ec_moe_dit'


def rmsnorm(x, gain):
    x32 = x.astype(jnp.float32)
    y = x32 * lax.rsqrt(jnp.mean(x32 * x32, axis=-1, keepdims=True) + EPS)
    return (y * gain.astype(jnp.float32)).astype(x.dtype)


def modulate(h, shift, scale):
    return h * (1.0 + scale) + shift


def head_groupnorm(y, gain):
    y32 = y.astype(jnp.float32)
    yc = y32 - jnp.mean(y32, axis=-1, keepdims=True)
    out = yc * lax.rsqrt(jnp.mean(yc * yc, axis=-1, keepdims=True) + EPS)
    b, l = y.shape[:2]
    return (out.reshape(b, l, -1) * gain.astype(jnp.float32)).astype(y.dtype)


def head_rmsnorm(y, gain):
    y32 = y.astype(jnp.float32)
    out = y32 * lax.rsqrt(jnp.mean(y32 * y32, axis=-1, keepdims=True) + EPS)
    b, l = y.shape[:2]
    return (out.reshape(b, l, -1) * gain.astype(jnp.float32)).astype(y.dtype)


def axial_rope_tables(rows, head_dim):
    n_freq = head_dim // 4
    freqs = ROPE_BASE ** (-jnp.arange(n_freq, dtype=jnp.float32) / n_freq)
    row = jnp.repeat(jnp.arange(rows, dtype=jnp.float32), GRID_W)
    col = jnp.tile(jnp.arange(GRID_W, dtype=jnp.float32), rows)
    ang = jnp.concatenate([row[:, None] * freqs, col[:, None] * freqs], axis=-1)
    return jnp.cos(ang), jnp.sin(ang)


def apply_rope(x, cos, sin):
    half = x.shape[-1] // 2
    shape = (1, cos.shape[0]) + (1,) * (x.ndim - 3) + (half,)
    cs = cos.reshape(shape).astype(x.dtype)
    sn = sin.reshape(shape).astype(x.dtype)
    x1, x2 = x[..., :half], x[..., half:]
    return jnp.concatenate([x1 * cs - x2 * sn, x1 * sn + x2 * cs], axis=-1)


def split_projection(p):
    b, l, _ = p.shape
    pieces = []
    start = 0
    for w in PROJ_SPLITS:
        pieces.append(p[..., start:start + w])
        start += w
    rq, rk, rv, rg, dq, dk, dv = pieces
    return (rq.reshape(b, l, RET_HEADS, RET_QK_DIM), rk.reshape(b, l, RET_HEADS, RET_QK_DIM),
            rv.reshape(b, l, RET_HEADS, RET_V_DIM), rg,
            dq.reshape(b, l, DIFF_HEADS, 2, DIFF_QK_DIM), dk.reshape(b, l, DIFF_HEADS, 2, DIFF_QK_DIM),
            dv.reshape(b, l, DIFF_HEADS, DIFF_V_DIM))


def retention_chunked(q, k, v, log_gamma, init_state, strict):
    b, h, l, dk = q.shape
    dv = v.shape[-1]
    n = l // CHUNK
    idx = jnp.arange(CHUNK, dtype=jnp.float32)
    diff = idx[:, None] - idx[None, :]
    mask = (diff > 0) if strict else (diff >= 0)
    lg = log_gamma.astype(jnp.float32)
    decay_in = jnp.where(mask[None], jnp.exp(lg[:, None, None] * jnp.where(mask, diff, 0.0)[None]), 0.0).astype(q.dtype)
    q_decay = jnp.exp(lg[:, None] * (idx + 1.0))[None, :, :, None].astype(q.dtype)
    k_decay = jnp.exp(lg[:, None] * (CHUNK - 1.0 - idx))[None, :, :, None].astype(q.dtype)
    chunk_decay = jnp.exp(lg * CHUNK)[None, :, None, None].astype(q.dtype)
    to_chunks = lambda t: t.reshape(b, h, n, CHUNK, t.shape[-1]).transpose(2, 0, 1, 3, 4)

    def step(state, inp):
        qi, ki, vi = inp
        scores = jnp.einsum('bhid,bhjd->bhij', qi, ki) * decay_in
        o = jnp.einsum('bhij,bhjv->bhiv', scores, vi) + jnp.einsum('bhid,bhdv->bhiv', qi * q_decay, state)
        state = state * chunk_decay + jnp.einsum('bhjd,bhjv->bhdv', ki * k_decay, vi)
        return state, o

    _, out = lax.scan(step, init_state, (to_chunks(q), to_chunks(k), to_chunks(v)))
    return out.transpose(1, 2, 0, 3, 4).reshape(b, h, l, dv)


def bidirectional_retention(q, k, v, log_g, init_f, init_b):
    fwd = retention_chunked(q, k, v, log_g[0], init_f, False)
    flip = lambda t: jnp.flip(t, axis=2)
    bwd = flip(retention_chunked(flip(q), flip(k), flip(v), log_g[1], init_b, True))
    return fwd + bwd


def retention_context_states(k, v, log_g):
    lc = k.shape[2]
    pos = jnp.arange(lc, dtype=jnp.float32)
    w_f = jnp.exp(log_g[0][:, None] * (lc - 1.0 - pos)).astype(k.dtype)
    w_b = jnp.exp(log_g[1][:, None] * pos).astype(k.dtype)
    s_f = jnp.einsum('bhld,bhlv,hl->bhdv', k, v, w_f)
    s_b = jnp.einsum('bhld,bhlv,hl->bhdv', k, v, w_b)
    return s_f, s_b


def diff_attention(q, keys, vals, lam):
    b, h, _, l, dk = q.shape
    nb = l // QBLOCK
    qb = q.reshape(b, h, 2, nb, QBLOCK, dk).transpose(3, 0, 1, 2, 4, 5)

    def block(qi):
        s = jnp.einsum('bhtqd,bhtkd->bhtqk', qi, keys).astype(jnp.float32)
        p = jax.nn.softmax(s, axis=-1)
        a = p[:, :, 0] - lam * p[:, :, 1]
        return jnp.einsum('bhqk,bhkv->bhqv', a.astype(vals.dtype), vals)

    out = lax.map(block, qb)
    return out.transpose(1, 0, 3, 2, 4).reshape(b, l, h, vals.shape[-1])


def merge_heads(ret, gate, diff, ret_gain, diff_gain, lam_init, w_out_l):
    ret_out = jax.nn.silu(gate) * head_groupnorm(ret.transpose(0, 2, 1, 3), ret_gain)
    diff_out = head_rmsnorm(diff, diff_gain) * (1.0 - lam_init)
    return jnp.concatenate([ret_out, diff_out], axis=-1) @ w_out_l


def expert_choice_ffn(h, w_router_l, w_gate_l, w_up_l, w_down_l):
    b, l, _ = h.shape
    cap = CAPACITY_FACTOR * l // N_EXPERTS
    aff = jax.nn.softmax((h @ w_router_l).astype(jnp.float32), axis=-1)
    gates, idx = lax.top_k(jnp.swapaxes(aff, 1, 2), cap)
    bidx = jnp.arange(b)[:, None, None]
    xs = h[bidx, idx]
    hid = jax.nn.silu(jnp.einsum('becd,edf->becf', xs, w_gate_l)) * jnp.einsum('becd,edf->becf', xs, w_up_l)
    ys = jnp.einsum('becf,efd->becd', hid, w_down_l) * gates[..., None].astype(h.dtype)
    return jnp.zeros_like(h).at[bidx, idx].add(ys)


def setup_inputs(seed: int = 0) -> dict:
    key = jax.random.key(seed)
    ks = jax.random.split(key, 20)
    f32 = jnp.float32
    nrm = lambda k, s: jax.random.normal(k, s, dtype=f32)
    base_decay = -jnp.log(2.0) * (5.0 + jnp.arange(RET_HEADS, dtype=f32))
    return {
        'x': nrm(ks[0], (BATCH, SEQ, D_MODEL)),
        'c': nrm(ks[1], (BATCH, D_MODEL)),
        'ctx': nrm(ks[2], (BATCH, CTX_LEN, D_MODEL)),
        'c_ctx': nrm(ks[3], (D_MODEL,)),
        'w_ada': nrm(ks[4], (DEPTH, D_MODEL, N_MOD * D_MODEL)) * (0.5 * D_MODEL ** -0.5),
        'b_ada': nrm(ks[5], (DEPTH, N_MOD * D_MODEL)) * 0.02,
        'norm_mix': 1.0 + 0.02 * nrm(ks[6], (DEPTH, D_MODEL)),
        'norm_ffn': 1.0 + 0.02 * nrm(ks[7], (DEPTH, D_MODEL)),
        'w_in': nrm(ks[8], (DEPTH, D_MODEL, PROJ_WIDTH)) * D_MODEL ** -0.5,
        'w_out': nrm(ks[9], (DEPTH, MIX_WIDTH, D_MODEL)) * MIX_WIDTH ** -0.5,
        'ret_log_decay': base_decay[None, None, :] + 0.1 * nrm(ks[10], (DEPTH, 2, RET_HEADS)),
        'ret_norm': 1.0 + 0.02 * nrm(ks[11], (DEPTH, RET_WIDTH)),
        'diff_lambda': 0.1 * nrm(ks[12], (DEPTH, 4, DIFF_QK_DIM)),
        'diff_norm': 1.0 + 0.02 * nrm(ks[13], (DEPTH, DIFF_WIDTH)),
        'w_router': nrm(ks[14], (DEPTH, D_MODEL, N_EXPERTS)) * D_MODEL ** -0.5,
        'w_gate': nrm(ks[15], (DEPTH, N_EXPERTS, D_MODEL, EXPERT_FF)) * D_MODEL ** -0.5,
        'w_up': nrm(ks[16], (DEPTH, N_EXPERTS, D_MODEL, EXPERT_FF)) * D_MODEL ** -0.5,
        'w_down': nrm(ks[17], (DEPTH, N_EXPERTS, EXPERT_FF, D_MODEL)) * EXPERT_FF ** -0.5,
        'norm_final': 1.0 + 0.02 * nrm(ks[18], (D_MODEL,)),
    }


def reference(x, c, ctx, c_ctx, w_ada, b_ada, norm_mix, norm_ffn, w_in, w_out, ret_log_decay,
              ret_norm, diff_lambda, diff_norm, w_router, w_gate, w_up, w_down, norm_final):
    rows = x.shape[1] // GRID_W
    cos, sin = axial_rope_tables(rows, RET_QK_DIM)
    silu_c = jax.nn.silu(c)
    silu_cc = jax.nn.silu(c_ctx)
    r_scale = RET_QK_DIM ** -0.5
    d_scale = DIFF_QK_DIM ** -0.5
    to_bhld = lambda t: t.transpose(0, 2, 1, 3)
    to_bhtld = lambda t: t.transpose(0, 2, 3, 1, 4)
    for layer in range(DEPTH):
        last = layer == DEPTH - 1
        mod_x = jnp.split((silu_c @ w_ada[layer] + b_ada[layer])[:, None, :], N_MOD, axis=-1)
        mod_c = jnp.split(silu_cc @ w_ada[layer] + b_ada[layer], N_MOD, axis=-1)
        hx = modulate(rmsnorm(x, norm_mix[layer]), mod_x[0], mod_x[1])
        hc = modulate(rmsnorm(ctx, norm_mix[layer]), mod_c[0], mod_c[1])
        rq_x, rk_x, rv_x, rg_x, dq_x, dk_x, dv_x = split_projection(hx @ w_in[layer])
        rq_c, rk_c, rv_c, rg_c, dq_c, dk_c, dv_c = split_projection(hc @ w_in[layer])

        log_g = jnp.log1p(-jnp.exp(ret_log_decay[layer].astype(jnp.float32)))
        q_c, k_c, v_c = to_bhld(rq_c * r_scale), to_bhld(rk_c), to_bhld(rv_c)
        state_f, state_b = retention_context_states(k_c, v_c, log_g)
        ret_x = bidirectional_retention(to_bhld(apply_rope(rq_x, cos, sin) * r_scale),
                                        to_bhld(apply_rope(rk_x, cos, sin)), to_bhld(rv_x),
                                        log_g, state_f, state_b)

        lam_init = 0.8 - 0.6 * math.exp(-0.3 * layer)
        lv = diff_lambda[layer].astype(jnp.float32)
        lam = jnp.exp(jnp.sum(lv[0] * lv[1])) - jnp.exp(jnp.sum(lv[2] * lv[3])) + lam_init
        dk_ct = to_bhtld(dk_c)
        dv_ct = to_bhld(dv_c)
        keys_x = jnp.concatenate([dk_ct, to_bhtld(apply_rope(dk_x, cos, sin))], axis=3)
        vals_x = jnp.concatenate([dv_ct, to_bhld(dv_x)], axis=2)
        diff_x = diff_attention(to_bhtld(apply_rope(dq_x, cos, sin) * d_scale), keys_x, vals_x, lam)

        x_mid = x + mod_x[2] * merge_heads(ret_x, rg_x, diff_x, ret_norm[layer], diff_norm[layer], lam_init, w_out[layer])

        if not last:
            zeros = jnp.zeros(state_f.shape, dtype=state_f.dtype)
            ret_c = bidirectional_retention(q_c, k_c, v_c, log_g, zeros, zeros)
            diff_c = diff_attention(to_bhtld(dq_c * d_scale), dk_ct, dv_ct, lam)
            ctx = ctx + mod_c[2] * merge_heads(ret_c, rg_c, diff_c, ret_norm[layer], diff_norm[layer], lam_init, w_out[layer])
            hc2 = modulate(rmsnorm(ctx, norm_ffn[layer]), mod_c[3], mod_c[4])
            ctx = ctx + mod_c[5] * expert_choice_ffn(hc2, w_router[layer], w_gate[layer], w_up[layer], w_down[layer])

        hx2 = modulate(rmsnorm(x_mid, norm_ffn[layer]), mod_x[3], mod_x[4])
        x = x_mid + mod_x[5] * expert_choice_ffn(hx2, w_router[layer], w_gate[layer], w_up[layer], w_down[layer])
    return rmsnorm(x, norm_final)
```

```python
import numpy as np
import ml_dtypes
import concourse.bass as bass
import concourse.mybir as mybir
from concourse.bass_utils import run_bass_kernel_spmd

F32 = mybir.dt.float32
BF16 = mybir.dt.bfloat16
U32 = mybir.dt.uint32
AF = mybir.ActivationFunctionType
ALU = mybir.AluOpType
AX = mybir.AxisListType

D = 2048
SEQ = 2048
CTX = 256
T = SEQ + CTX
NCH = T // 128
DEPTH = 4
NE = 16
FF = 1024
PROJ = 6144
EPS = 1e-6
CAPX = 256
CAPC = 32
NSLOT = CAPX + CAPC
DMIN = -(SEQ + CTX - 128) - 127
FLEN = 4608
SW = 4480


_uniq = [0]


def SBT(nc, name, shape, dt):
    _uniq[0] += 1
    return nc.sbuf_tensor(f"{name}_{_uniq[0]}", shape, dt)


class Res:
    __slots__ = ("name", "w", "r", "pw")

    def __init__(self, name):
        self.name = name
        self.w = None
        self.r = {}
        self.pw = []


class Prog:
    ENG = ("pe", "act", "dve", "pool", "sp")

    def __init__(self, nc):
        self.nc = nc
        self.e = dict(pe=nc.tensor, act=nc.scalar, dve=nc.vector, pool=nc.gpsimd, sp=nc.sync)
        self.psem = {k: nc.alloc_semaphore("p_" + k) for k in self.ENG}
        self.pcnt = {k: 0 for k in self.ENG}
        self.pend = {k: False for k in self.ENG}
        self.seen = {k: {} for k in self.ENG}
        self.dsem = {}
        self.dval = {}
        self.dnext = {}
        for q, n in (("sp", 20), ("pool", 20), ("act", 6)):
            self.dsem[q] = [nc.alloc_semaphore(f"d_{q}{i}") for i in range(n)]
            self.dval[q] = [0] * n
            self.dnext[q] = 0
        self.bsem = nc.alloc_semaphore("barrier")
        self.bcnt = 0
        self.ninst = 0

    def _semof(self, key):
        if key[0] == "e":
            return self.psem[key[1]]
        return self.dsem[key[1]][key[2]]

    def _need(self, eng, events):
        need = {}
        for ev in events:
            if ev is None:
                continue
            key, val = ev
            if key == ("e", "pe") and eng == "pe":
                continue
            if self.seen[eng].get(key, 0) >= val:
                continue
            if need.get(key, 0) < val:
                need[key] = val
        return need

    def _emit(self, eng, fn, need):
        items = list(need.items())
        for key, val in items[:-1]:
            self.e[eng].wait_ge(self._semof(key), val)
            self.ninst += 1
        ins = fn()
        if items:
            key, val = items[-1]
            ins._wait_ge(self._semof(key), val)
        for key, val in items:
            self.seen[eng][key] = val
        self.ninst += 1
        return ins

    def op(self, eng, fn, reads=(), writes=(), inc=True):
        evs = []
        for r in reads:
            evs.append(r.w)
            evs.extend(r.pw)
        for w in writes:
            if w.w is not None and not (w.w[0] == ("e", eng)):
                evs.append(w.w)
            evs.extend(w.pw)
            for k, v in w.r.items():
                if k != ("e", eng):
                    evs.append((k, v))
        need = self._need(eng, evs)
        ins = self._emit(eng, fn, need)
        for w in writes:
            w.pw = []
        if inc:
            self.pcnt[eng] += 1
            ins.then_inc(self.psem[eng], 1)
            self.pend[eng] = False
            val = self.pcnt[eng]
        else:
            self.pend[eng] = True
            val = self.pcnt[eng] + 1
        ev = (("e", eng), val)
        for w in writes:
            w.w = ev
            w.r = {}
        for r in reads:
            if r.r.get(ev[0], 0) < val:
                r.r[ev[0]] = val
        return ins

    def dma(self, q, fn, reads=(), writes=(), serial=False):
        eng = q
        i = self.dnext[q]
        self.dnext[q] = (i + 1) % len(self.dsem[q])
        key = ("d", q, i)
        evs = [(key, self.dval[q][i])] if self.dval[q][i] else []
        for r in reads:
            evs.append(r.w)
            evs.extend(r.pw)
        keep = []
        for w in writes:
            partial = (not serial) and (not w.r) and w.w is not None and w.w[0][0] == "d"
            if partial:
                keep.append((w, w.pw + [w.w]))
            else:
                keep.append((w, []))
                evs.append(w.w)
                evs.extend(w.pw)
                for k, v in w.r.items():
                    evs.append((k, v))
        need = self._need(eng, evs)
        ins = self._emit(eng, fn, need)
        self.dval[q][i] += 16
        ins.then_inc(self.dsem[q][i], 16)
        ev = (key, self.dval[q][i])
        for w, pw in keep:
            w.pw = pw
            w.w = ev
            w.r = {}
        for r in reads:
            if r.r.get(key, 0) < ev[1]:
                r.r[key] = ev[1]
        return ins

    def flush(self, eng):
        if self.pend[eng]:
            self.pcnt[eng] += 1
            self.e[eng].nop().then_inc(self.psem[eng], 1)
            self.pend[eng] = False
            self.ninst += 1

    def barrier(self):
        for k in self.ENG:
            self.flush(k)
        sp = self.e["sp"]
        for k in self.ENG:
            if k != "sp" and self.seen["sp"].get(("e", k), 0) < self.pcnt[k]:
                sp.wait_ge(self.psem[k], self.pcnt[k])
                self.ninst += 1
        for q in self.dsem:
            for i, s in enumerate(self.dsem[q]):
                v = self.dval[q][i]
                if v and self.seen["sp"].get(("d", q, i), 0) < v:
                    sp.wait_ge(s, v)
                    self.ninst += 1
        self.bcnt += 1
        sp.nop().then_inc(self.bsem, 1)
        self.ninst += 1
        for k in self.ENG:
            if k != "sp":
                self.e[k].wait_ge(self.bsem, self.bcnt)
                self.ninst += 1
        for k in self.ENG:
            for k2 in self.ENG:
                self.seen[k][("e", k2)] = self.pcnt[k2]
            for q in self.dsem:
                for i in range(len(self.dsem[q])):
                    self.seen[k][("d", q, i)] = self.dval[q][i]


class Tile:
    def __init__(self, t, name):
        self.t = t
        self.res = Res(name)

    def __getitem__(self, k):
        return self.t[k]


class Ctx:
    pass


class _Inputs:
    def __init__(self, nc, depth):
        self._nc = nc
        self._spec = {
            "x": ([SEQ, D], F32), "ctx": ([CTX, D], F32), "cvec": ([2, D], F32),
            "w_ada": ([depth, D, 6 * D], F32), "b_ada": ([depth, 6 * D], F32),
            "norm_mix": ([depth, D], F32), "norm_ffn": ([depth, D], F32),
            "w_in": ([depth, D, PROJ], F32), "w_out": ([depth, D, D], F32),
            "ret_log_decay": ([depth, 16], F32), "ret_norm": ([depth, 1024], F32),
            "diff_lambda": ([depth, 256], F32), "diff_norm": ([depth, 1024], F32),
            "w_router": ([depth, D, NE], F32), "w_gate": ([depth, NE, D, FF], F32),
            "w_up": ([depth, NE, D, FF], F32), "w_down": ([depth, NE, FF, D], F32),
            "norm_final": ([1, D], F32),
            "k_ident": ([128, 128], BF16), "k_identf": ([128, 128], F32),
            "k_rope": ([SEQ, 128], F32), "k_iota": ([128, NSLOT], F32),
            "k_tokid": ([128, NCH], F32), "k_strip": ([128, 2, SW], F32),
        }
        self.used = {}

    def __getattr__(self, name):
        if name.startswith("_") or name == "used":
            raise AttributeError(name)
        if name not in self.used:
            shape, dt = self._spec[name]
            self.used[name] = self._nc.dram_tensor(name, list(shape), dt, kind="ExternalInput").ap()
        return self.used[name]


def _mk_inputs_decl(nc, depth):
    return _Inputs(nc, depth)


def host_consts():
    k = {}
    k["k_ident"] = np.eye(128, dtype=np.float32).astype(ml_dtypes.bfloat16)
    k["k_identf"] = np.eye(128, dtype=np.float32)
    n_freq = 16
    freqs = (10000.0 ** (-np.arange(n_freq, dtype=np.float32) / n_freq)).astype(np.float32)
    row = np.repeat(np.arange(SEQ // 64, dtype=np.float32), 64)
    col = np.tile(np.arange(64, dtype=np.float32), SEQ // 64)
    ang = np.concatenate([row[:, None] * freqs, col[:, None] * freqs], axis=-1).astype(np.float32)
    cs, sn = np.cos(ang).astype(np.float32), np.sin(ang).astype(np.float32)
    k["k_rope"] = np.concatenate([cs * 0.125, sn * 0.125, cs, sn], axis=1).astype(np.float32)
    k["k_iota"] = np.tile(np.arange(NSLOT, dtype=np.float32)[None, :], (128, 1))
    k["k_tokid"] = (np.arange(NCH, dtype=np.float32)[None, :] * 128 + np.arange(128, dtype=np.float32)[:, None])
    s = np.arange(SW, dtype=np.float32)[None, :] - np.arange(128, dtype=np.float32)[:, None] - 2176.0
    k["k_strip"] = np.stack([np.maximum(s, 0), np.maximum(-s, 0)], axis=1).astype(np.float32)
    return k


def build(depth=DEPTH, phases=None, debug=False, final=True, l0=0):
    nc = bass.Bass("TRN2", target_bir_lowering=False)
    P = Prog(nc)
    g = _mk_inputs_decl(nc, depth)
    okind = "ExternalOutput"
    out = nc.dram_tensor("out", [SEQ, D], F32, kind=okind).ap()
    skind = "ExternalOutput" if debug else "Internal"
    dram = lambda name, shape, dt: nc.dram_tensor(name, list(shape), dt, kind=skind).ap()
    XS = dram("XS", [T, D], F32)
    MODB = [dram("MOD0", [2, 6 * D], F32), dram("MOD1", [2, 6 * D], F32)]
    QT = dram("QT", [24, 128, T], BF16)
    VD = dram("VD", [T, 16, 130], BF16)
    GD = dram("GD", [T, 1024], F32)
    MG = dram("MG", [T, D], BF16)
    HT2 = dram("HT2", [T, D], BF16)
    rXS, rQT, rVD, rGD, rMG, rHT2 = (Res(n) for n in ("XS", "QT", "VD", "GD", "MG", "HT2"))
    rMODB = [Res("MOD0"), Res("MOD1")]
    rIN = Res("inputs")

    def want(ph):
        return phases is None or ph in phases

    def sb(name, shape, dt):
        return Tile(nc.alloc_sbuf_tensor(name, list(shape), dt), name)

    ident = sb("ident", [128, 128], BF16)
    identf = sb("identf", [128, 128], F32)
    SC = sb("silu_c", [128, 16, 2], BF16)
    AFF = sb("AFF", [128, NCH, NE], F32)
    IDXU = sb("IDXU", [128, NE, 3], U32)
    GATE = sb("GATE", [128, NE, 3], F32)
    banks = [Tile(nc.alloc_psum_tensor(f"bank{i}", [128, 512], F32), f"bank{i}") for i in range(8)]

    def bank_bf(i):
        return banks[i].t[:].bitcast(BF16)

    P.dma("sp", lambda: nc.sync.dma_start(out=ident[:], in_=g.k_ident[:, :]), reads=[rIN], writes=[ident.res])
    P.dma("sp", lambda: nc.sync.dma_start(out=identf[:], in_=g.k_identf[:, :]), reads=[rIN], writes=[identf.res])
    P.dma("sp", lambda: nc.sync.dma_start(out=XS[0:CTX, :], in_=g.ctx[:, :]), reads=[rIN], writes=[rXS])
    P.dma("sp", lambda: nc.sync.dma_start(out=XS[CTX:T, :], in_=g.x[:, :]), reads=[rIN], writes=[rXS])
    with SBT(nc, "c_raw", [128, 16, 2], F32) as craw_t:
        craw = Tile(craw_t, "craw")
        for r in range(2):
            P.dma("sp", lambda r=r: nc.sync.dma_start(
                out=craw[:, :, r], in_=g.cvec[r, :].rearrange("(c p) -> p c", p=128),
                allow_slow_non_contiguous=True), reads=[rIN], writes=[craw.res])
        P.op("act", lambda: nc.scalar.activation(out=SC[:], in_=craw[:], func=AF.Silu),
             reads=[craw.res], writes=[SC.res])
        P.barrier()

    for L in range(depth):
        Lg = l0 + L
        last = (Lg == DEPTH - 1) and final
        tc0 = 2 if last else 0
        lam_init = 0.8 - 0.6 * float(np.exp(-0.3 * Lg))

        MOD, rMOD = MODB[L % 2], rMODB[L % 2]

        def mod_gen(Lw, ws, mb, mo, pbs):
            dstM, rdst = MODB[Lw % 2], rMODB[Lw % 2]
            for nb in range(24):
                w = ws[nb % 2]
                b_, o_ = mb[nb % 2], mo[nb % 2]
                srcw = g.w_ada[Lw][:, nb * 512:(nb + 1) * 512].rearrange("(c p) n -> p c n", p=128)
                for hh in range(2):
                    P.dma("pool", lambda w=w, srcw=srcw, hh=hh: nc.gpsimd.dma_start(
                        out=w.t[:, hh * 8:(hh + 1) * 8, :], in_=srcw[:, hh * 8:(hh + 1) * 8, :]),
                        reads=[rIN], writes=[w.res])
                P.dma("sp", lambda b_=b_, nb=nb: nc.sync.dma_start(
                    out=b_.t, in_=g.b_ada[Lw:Lw + 1, nb * 512:(nb + 1) * 512].to_broadcast([2, 512])),
                    reads=[rIN], writes=[b_.res])
                pb = pbs[nb % len(pbs)]
                for dc in range(16):
                    P.op("pe", lambda w=w, dc=dc, pb=pb: nc.tensor.matmul(
                        pb.t[0:2, :], lhsT=SC[:, dc, :], rhs=w.t[:, dc, :], start=(dc == 0), stop=(dc == 15)),
                        reads=[SC.res, w.res], writes=[pb.res], inc=(dc == 15))
                P.op("dve", lambda o_=o_, pb=pb, b_=b_: nc.vector.tensor_tensor(
                    out=o_.t, in0=pb.t[0:2, :], in1=b_.t, op=ALU.add),
                    reads=[pb.res, b_.res], writes=[o_.res])
                P.dma("sp", lambda o_=o_, nb=nb: nc.sync.dma_start(
                    out=dstM[:, nb * 512:(nb + 1) * 512], in_=o_.t), reads=[o_.res], writes=[rdst])
                yield nb

        def mod_tiles(w0, w1, mb_t, mo_t):
            ws = [Tile(w0, "mw0"), Tile(w1, "mw1")]
            mb = [Tile(mb_t[:, i, :], f"mb{i}") for i in range(2)]
            mo = [Tile(mo_t[:, i, :], f"mo{i}") for i in range(2)]
            return ws, mb, mo

        if want("mod") and (L == 0 or not want("topk")):
            with (SBT(nc, "mw0", [128, 16, 512], BF16) as w0, SBT(nc, "mw1", [128, 16, 512], BF16) as w1,
                  SBT(nc, "mb", [2, 2, 512], F32) as mb_t, SBT(nc, "mo", [2, 2, 512], F32) as mo_t):
                ws, mb, mo = mod_tiles(w0, w1, mb_t, mo_t)
                for _ in mod_gen(L, ws, mb, mo, [banks[0], banks[1]]):
                    pass
                P.barrier()

        def load_bc(tile, src_row):
            P.dma("sp", lambda: nc.sync.dma_start(out=tile[:], in_=src_row.to_broadcast([128, D])),
                  reads=[rMOD, rIN], writes=[tile.res])

        def norm_phase(which, hT, store_ht2, router):
            gain = (g.norm_mix if which == 0 else g.norm_ffn)[L:L + 1, :]
            s_shift, s_scale = (0, 1) if which == 0 else (3, 4)
            with (SBT(nc, "nA", [128, 2, D], F32) as nA_t, SBT(nc, "nB", [128, 2, D], F32) as nB_t,
                  SBT(nc, "nG", [128, D], F32) as nG_t, SBT(nc, "nX", [128, 2, D], F32) as nX_t,
                  SBT(nc, "nT", [128, 2, D], F32) as nT_t, SBT(nc, "nH", [128, 2, D], BF16) as nH_t,
                  SBT(nc, "nJ", [128, D], BF16) as nJ_t, SBT(nc, "nS", [128, 2, 4], F32) as nS_t,
                  SBT(nc, "nHT", [128, 2, 16, 128], BF16) as nHT_t, SBT(nc, "nWr", [128, 16, NE], BF16) as nWr_t,
                  SBT(nc, "nLg", [128, 2, 40], F32) as nLg_t):
                A = [Tile(nA_t[:, i, :], f"nA{i}") for i in range(2)]
                B = [Tile(nB_t[:, i, :], f"nB{i}") for i in range(2)]
                G = Tile(nG_t, "nG")
                X = [Tile(nX_t[:, i, :], f"nX{i}") for i in range(2)]
                TT = [Tile(nT_t[:, i, :], f"nT{i}") for i in range(2)]
                H = [Tile(nH_t[:, i, :], f"nH{i}") for i in range(2)]
                J = Tile(nJ_t, "nJ")
                S = [Tile(nS_t[:, i, :], f"nS{i}") for i in range(2)]
                HTc = [Tile(nHT_t[:, i], f"nHT{i}") for i in range(2)]
                Wr = Tile(nWr_t, "nWr")
                LG = [Tile(nLg_t[:, i, :], f"nLg{i}") for i in range(2)]
                load_bc(G, gain)
                for i, row in ((1, 0), (0, 1)):
                    load_bc(A[i], MOD[row:row + 1, s_scale * D:(s_scale + 1) * D])
                    load_bc(B[i], MOD[row:row + 1, s_shift * D:(s_shift + 1) * D])
                    P.op("dve", lambda i=i: nc.vector.scalar_tensor_tensor(
                        out=A[i].t, in0=A[i].t, scalar=1.0, in1=G[:], op0=ALU.add, op1=ALU.mult),
                        reads=[A[i].res, G.res], writes=[A[i].res])
                if router:
                    P.dma("pool", lambda: nc.gpsimd.dma_start(
                        out=Wr[:], in_=g.w_router[L].rearrange("(c p) e -> p c e", p=128)),
                        reads=[rIN], writes=[Wr.res])
                for tc in range(NCH):
                    if router and last and tc < 2:
                        continue
                    k = tc % 2
                    isx = 1 if tc >= 2 else 0
                    x_, t_, h_, s_ = X[k], TT[k], H[k], S[k]
                    P.dma("sp", lambda x_=x_, tc=tc: nc.sync.dma_start(out=x_.t, in_=XS[tc * 128:(tc + 1) * 128, :]),
                          reads=[rXS], writes=[x_.res])
                    P.op("act", lambda x_=x_, s_=s_: nc.scalar.activation(
                        out=J[:], in_=x_.t, func=AF.Square, accum_out=s_.t[:, 0:1]),
                        reads=[x_.res], writes=[J.res, s_.res])
                    P.op("dve", lambda s_=s_: nc.vector.tensor_scalar(
                        out=s_.t[:, 1:2], in0=s_.t[:, 0:1], scalar1=1.0 / D, scalar2=EPS, op0=ALU.mult, op1=ALU.add),
                        reads=[s_.res], writes=[s_.res])
                    P.op("act", lambda s_=s_: nc.scalar.activation(out=s_.t[:, 2:3], in_=s_.t[:, 1:2], func=AF.Sqrt),
                         reads=[s_.res], writes=[s_.res])
                    P.op("dve", lambda s_=s_: nc.vector.reciprocal(out=s_.t[:, 3:4], in_=s_.t[:, 2:3]),
                         reads=[s_.res], writes=[s_.res])
                    P.op("dve", lambda x_=x_, t_=t_, s_=s_, isx=isx: nc.vector.scalar_tensor_tensor(
                        out=t_.t, in0=x_.t, scalar=s_.t[:, 3:4], in1=A[isx].t, op0=ALU.mult, op1=ALU.mult),
                        reads=[x_.res, s_.res, A[isx].res], writes=[t_.res])
                    P.op("pool", lambda t_=t_, h_=h_, isx=isx: nc.gpsimd.tensor_tensor(
                        out=h_.t, in0=t_.t, in1=B[isx].t, op=ALU.add),
                        reads=[t_.res, B[isx].res], writes=[h_.res])
                    if store_ht2:
                        P.dma("sp", lambda h_=h_, tc=tc: nc.sync.dma_start(out=HT2[tc * 128:(tc + 1) * 128, :], in_=h_.t),
                              reads=[h_.res], writes=[rHT2])
                    for half in range(2):
                        pb = banks[2 + (tc % 2) * 2 + half]
                        pbv = bank_bf(2 + (tc % 2) * 2 + half)
                        for j in range(8):
                            dc = half * 8 + j
                            P.op("pe", lambda h_=h_, dc=dc, j=j, pbv=pbv: nc.tensor.transpose(
                                out=pbv[:, j * 128:(j + 1) * 128], in_=h_.t[:, dc * 128:(dc + 1) * 128], identity=ident[:]),
                                reads=[h_.res, ident.res], writes=[pb.res], inc=(j == 7))
                        if hT is not None:
                            dst = hT.t[:, half * 8:(half + 1) * 8, tc * 128:(tc + 1) * 128]
                            dres = hT.res
                        else:
                            dst = HTc[k].t[:, half * 8:(half + 1) * 8, :]
                            dres = HTc[k].res
                        ev_eng = "act" if half == 0 else "dve"
                        if ev_eng == "act":
                            P.op("act", lambda dst=dst, pbv=pbv: nc.scalar.copy(
                                out=dst, in_=pbv.rearrange("p (j t) -> p j t", j=8)),
                                reads=[pb.res], writes=[dres])
                        else:
                            P.op("dve", lambda dst=dst, pbv=pbv: nc.vector.tensor_copy(
                                out=dst, in_=pbv.rearrange("p (j t) -> p j t", j=8)),
                                reads=[pb.res], writes=[dres])
                    if router:
                        pl = banks[6 + tc % 2]
                        lg = LG[k]
                        for dc in range(16):
                            P.op("pe", lambda dc=dc, pl=pl, k=k: nc.tensor.matmul(
                                pl.t[:, 0:NE], lhsT=HTc[k].t[:, dc, :], rhs=Wr[:, dc, :], start=(dc == 0), stop=(dc == 15)),
                                reads=[HTc[k].res, Wr.res], writes=[pl.res], inc=(dc == 15))
                        P.op("dve", lambda pl=pl, lg=lg: nc.vector.tensor_reduce(
                            out=lg.t[:, 32:33], in_=pl.t[:, 0:NE], axis=AX.X, op=ALU.max, negate=True),
                            reads=[pl.res], writes=[lg.res])
                        P.op("act", lambda pl=pl, lg=lg: nc.scalar.activation(
                            out=lg.t[:, 0:NE], in_=pl.t[:, 0:NE], func=AF.Exp, bias=lg.t[:, 32:33], scale=1.0,
                            accum_out=lg.t[:, 33:34]), reads=[pl.res, lg.res], writes=[lg.res])
                        P.op("dve", lambda lg=lg: nc.vector.reciprocal(out=lg.t[:, 34:35], in_=lg.t[:, 33:34]),
                             reads=[lg.res], writes=[lg.res])
                        P.op("dve", lambda lg=lg, tc=tc: nc.vector.tensor_scalar(
                            out=AFF[:, tc, :], in0=lg.t[:, 0:NE], scalar1=lg.t[:, 34:35], scalar2=None, op0=ALU.mult),
                            reads=[lg.res], writes=[AFF.res])
                P.barrier()

        if want("inproj"):
            with SBT(nc, "hT", [128, 16, T], BF16) as hT_t:
                hT = Tile(hT_t, "hT")
                norm_phase(0, hT, False, False)
                with (SBT(nc, "iw", [128, 2, 16, 512], BF16) as iw_t, SBT(nc, "rope", [128, 16, 128], F32) as rope_t,
                      SBT(nc, "qk", [128, 2, 512], BF16) as qk_t, SBT(nc, "rt", [128, 2, 4, 256], F32) as rt_t,
                      SBT(nc, "qts", [128, 4, T], BF16) as qts_t, SBT(nc, "va", [128, 2, 4, 130], BF16) as va_t,
                      SBT(nc, "gs", [128, 2, 512], F32) as gs_t):
                    IW = [Tile(iw_t[:, i], f"iw{i}") for i in range(2)]
                    ROPE = Tile(rope_t, "rope")
                    QK = [Tile(qk_t[:, i, :], f"qk{i}") for i in range(2)]
                    RT = [Tile(rt_t[:, i], f"rt{i}") for i in range(2)]
                    QTS = Tile(qts_t, "qts")
                    VA = [Tile(va_t[:, i], f"va{i}") for i in range(2)]
                    GS = [Tile(gs_t[:, i, :], f"gs{i}") for i in range(2)]
                    P.dma("sp", lambda: nc.sync.dma_start(out=ROPE[:], in_=g.k_rope.rearrange("(c p) f -> p c f", p=128)),
                          reads=[rIN], writes=[ROPE.res])
                    for i in range(2):
                        P.op("pool", lambda i=i: nc.gpsimd.memset(VA[i].t[:, :, 128:130], 1.0), writes=[VA[i].res])
                    kinds = ["q", "k", "v", "v", "g", "g", "q", "q", "k", "k", "v", "v"]
                    qt_base = {0: 0, 1: 4, 6: 8, 7: 12, 8: 16, 9: 20}
                    v_base = {2: 0, 3: 4, 10: 8, 11: 12}
                    cnt = 0
                    pending = []

                    def flush_pending():
                        while pending:
                            pending.pop(0)()

                    for cb in range(12):
                        W = IW[cb % 2]
                        src = g.w_in[L][:, cb * 512:(cb + 1) * 512].rearrange("(c p) n -> p c n", p=128)
                        for hh in range(2):
                            P.dma("pool", lambda W=W, src=src, hh=hh: nc.gpsimd.dma_start(
                                out=W.t[:, hh * 8:(hh + 1) * 8, :], in_=src[:, hh * 8:(hh + 1) * 8, :]),
                                reads=[rIN], writes=[W.res])
                        kind = kinds[cb]
                        for tc in range(NCH):
                            pb = banks[cnt % 4]
                            k2 = cnt % 2
                            cnt += 1
                            for dc in range(16):
                                P.op("pe", lambda W=W, dc=dc, tc=tc, pb=pb: nc.tensor.matmul(
                                    pb.t[:, :], lhsT=hT.t[:, dc, tc * 128:(tc + 1) * 128], rhs=W.t[:, dc, :],
                                    start=(dc == 0), stop=(dc == 15)),
                                    reads=[hT.res, W.res], writes=[pb.res], inc=(dc == 15))
                            flush_pending()
                            if kind in ("q", "k"):
                                qk = QK[k2]
                                if tc < 2:
                                    if kind == "q":
                                        P.op("act", lambda qk=qk, pb=pb: nc.scalar.mul(qk.t, pb.t[:, :], 0.125),
                                             reads=[pb.res], writes=[qk.res])
                                    else:
                                        P.op("act", lambda qk=qk, pb=pb: nc.scalar.copy(out=qk.t, in_=pb.t[:, :]),
                                             reads=[pb.res], writes=[qk.res])
                                else:
                                    rt = RT[k2]
                                    o = 0 if kind == "q" else 64
                                    xc = tc - 2
                                    cosb = ROPE[:, xc:xc + 1, o:o + 32].to_broadcast([128, 8, 32])
                                    sinb = ROPE[:, xc:xc + 1, o + 32:o + 64].to_broadcast([128, 8, 32])
                                    pv = pb.t[:, :].rearrange("p (h two f) -> p h two f", h=8, two=2)
                                    x1, x2 = pv[:, :, 0, :], pv[:, :, 1, :]
                                    rtv = rt.t.rearrange("p a (h f) -> p a h f", h=8)
                                    for a, (xx, cc) in enumerate(((x1, cosb), (x2, sinb), (x1, sinb), (x2, cosb))):
                                        P.op("dve", lambda a=a, xx=xx, cc=cc, rtv=rtv: nc.vector.tensor_tensor(
                                            out=rtv[:, a], in0=xx, in1=cc, op=ALU.mult),
                                            reads=[pb.res, ROPE.res], writes=[rt.res])
                                    qv = qk.t.rearrange("p (h two f) -> p h two f", h=8, two=2)
                                    P.op("pool", lambda qv=qv, rtv=rtv: nc.gpsimd.tensor_tensor(
                                        out=qv[:, :, 0, :], in0=rtv[:, 0], in1=rtv[:, 1], op=ALU.subtract),
                                        reads=[rt.res], writes=[qk.res])
                                    P.op("pool", lambda qv=qv, rtv=rtv: nc.gpsimd.tensor_tensor(
                                        out=qv[:, :, 1, :], in0=rtv[:, 2], in1=rtv[:, 3], op=ALU.add),
                                        reads=[rt.res], writes=[qk.res])

                                def do_tr(qk=qk, k2=k2, tc=tc):
                                    pt = banks[4 + k2]
                                    ptv = bank_bf(4 + k2)
                                    for j in range(4):
                                        P.op("pe", lambda qk=qk, j=j, ptv=ptv: nc.tensor.transpose(
                                            out=ptv[:, j * 128:(j + 1) * 128], in_=qk.t[:, j * 128:(j + 1) * 128], identity=ident[:]),
                                            reads=[qk.res, ident.res], writes=[pt.res], inc=(j == 3))
                                    P.op("act", lambda ptv=ptv, tc=tc: nc.scalar.copy(
                                        out=QTS[:, :, tc * 128:(tc + 1) * 128], in_=ptv[:, 0:512].rearrange("p (j t) -> p j t", j=4)),
                                        reads=[pt.res], writes=[QTS.res])
                                pending.append(do_tr)
                            elif kind == "v":
                                va = VA[k2]
                                P.op("act", lambda va=va, pb=pb: nc.scalar.copy(
                                    out=va.t[:, :, 0:128], in_=pb.t[:, :].rearrange("p (h f) -> p h f", h=4)),
                                    reads=[pb.res], writes=[va.res])
                                hb = v_base[cb]
                                P.dma("sp", lambda va=va, tc=tc, hb=hb: nc.sync.dma_start(
                                    out=VD[tc * 128:(tc + 1) * 128, hb:hb + 4, :], in_=va.t), reads=[va.res], writes=[rVD])
                            else:
                                gs = GS[k2]
                                P.op("act", lambda gs=gs, pb=pb: nc.scalar.activation(out=gs.t, in_=pb.t[:, :], func=AF.Silu),
                                     reads=[pb.res], writes=[gs.res])
                                P.dma("sp", lambda gs=gs, tc=tc, cb=cb: nc.sync.dma_start(
                                    out=GD[tc * 128:(tc + 1) * 128, (cb - 4) * 512:(cb - 3) * 512], in_=gs.t),
                                    reads=[gs.res], writes=[rGD])
                        if kind in ("q", "k"):
                            flush_pending()
                            qb_ = qt_base[cb]
                            P.dma("sp", lambda qb_=qb_: nc.sync.dma_start(
                                out=QT[qb_:qb_ + 4].rearrange("j p t -> p j t"), in_=QTS[:]), reads=[QTS.res], writes=[rQT])
                    P.barrier()

        env = Ctx()
        env.__dict__.update(dict(nc=nc, P=P, g=g, L=L, last=last, tc0=tc0, lam_init=lam_init, banks=banks, bank_bf=bank_bf,
                                 ident=ident, identf=identf, AFF=AFF, IDXU=IDXU, GATE=GATE, XS=XS, MOD=MOD, QT=QT, VD=VD,
                                 GD=GD, MG=MG, HT2=HT2, rXS=rXS, rMOD=rMOD, rQT=rQT, rVD=rVD, rGD=rGD, rMG=rMG,
                                 rHT2=rHT2, rIN=rIN, norm_phase=norm_phase, load_bc=load_bc,
                                 mod_gen=mod_gen, mod_tiles=mod_tiles,
                                 next_mod=(L + 1 if (L + 1 < depth and want('mod')) else None)))
        if want("attn"):
            phase_attn(env)
        if want("outproj"):
            phase_outproj(env)
        if want("norm2"):
            norm_phase(1, None, True, True)
        if want("topk"):
            phase_topk(env)
        if want("moe"):
            phase_moe(env)

    if final:
        with (SBT(nc, "fG", [128, D], F32) as fG_t, SBT(nc, "fX", [128, 2, D], F32) as fX_t,
              SBT(nc, "fJ", [128, D], BF16) as fJ_t, SBT(nc, "fS", [128, 2, 4], F32) as fS_t,
              SBT(nc, "fY", [128, 2, D], F32) as fY_t):
            G = Tile(fG_t, "fG")
            J = Tile(fJ_t, "fJ")
            X = [Tile(fX_t[:, i, :], f"fX{i}") for i in range(2)]
            Y = [Tile(fY_t[:, i, :], f"fY{i}") for i in range(2)]
            S = [Tile(fS_t[:, i, :], f"fS{i}") for i in range(2)]
            rOUT = Res("out")
            P.dma("sp", lambda: nc.sync.dma_start(out=G[:], in_=g.norm_final[0:1, :].to_broadcast([128, D])),
                  reads=[rIN], writes=[G.res])
            for tc in range(2, NCH):
                k = tc % 2
                x_, y_, s_ = X[k], Y[k], S[k]
                P.dma("sp", lambda x_=x_, tc=tc: nc.sync.dma_start(out=x_.t, in_=XS[tc * 128:(tc + 1) * 128, :]),
                      reads=[rXS], writes=[x_.res])
                P.op("act", lambda x_=x_, s_=s_: nc.scalar.activation(
                    out=J[:], in_=x_.t, func=AF.Square, accum_out=s_.t[:, 0:1]), reads=[x_.res], writes=[J.res, s_.res])
                P.op("dve", lambda s_=s_: nc.vector.tensor_scalar(
                    out=s_.t[:, 1:2], in0=s_.t[:, 0:1], scalar1=1.0 / D, scalar2=EPS, op0=ALU.mult, op1=ALU.add),
                    reads=[s_.res], writes=[s_.res])
                P.op("act", lambda s_=s_: nc.scalar.activation(out=s_.t[:, 2:3], in_=s_.t[:, 1:2], func=AF.Sqrt),
                     reads=[s_.res], writes=[s_.res])
                P.op("dve", lambda s_=s_: nc.vector.reciprocal(out=s_.t[:, 3:4], in_=s_.t[:, 2:3]),
                     reads=[s_.res], writes=[s_.res])
                P.op("dve", lambda x_=x_, y_=y_, s_=s_: nc.vector.scalar_tensor_tensor(
                    out=y_.t, in0=x_.t, scalar=s_.t[:, 3:4], in1=G[:], op0=ALU.mult, op1=ALU.mult),
                    reads=[x_.res, s_.res, G.res], writes=[y_.res])
                P.dma("sp", lambda y_=y_, tc=tc: nc.sync.dma_start(out=out[(tc - 2) * 128:(tc - 1) * 128, :], in_=y_.t),
                      reads=[y_.res], writes=[rOUT])
            P.barrier()
    else:
        rOUT = Res("out")
        P.dma("sp", lambda: nc.sync.dma_start(out=out[:, :], in_=XS[CTX:T, :]), reads=[rXS], writes=[rOUT])
        P.barrier()
    nc._prog_ninst = P.ninst
    nc._used_inputs = list(g.used.keys())
    return nc


def phase_attn(E):
    nc, P, g, L = E.nc, E.P, E.g, E.L
    banks = E.banks
    lam_init = E.lam_init
    with (SBT(nc, "aStrip", [128, 2, SW], F32) as strip_t, SBT(nc, "aTh", [128, 2, SW], F32) as th_t,
          SBT(nc, "aT1", [128, SW], F32) as t1_t,
          SBT(nc, "aLg", [128, 48], F32) as lg_t, SBT(nc, "aLv", [128, 256 + 128 + 8], F32) as lv_t,
          SBT(nc, "aRG", [128, 1024], F32) as rg_t, SBT(nc, "aDG", [128, 1024], F32) as dg_t,
          SBT(nc, "aQ", [128, 2, T], BF16) as q_t, SBT(nc, "aK", [128, 2, T], BF16) as k_t,
          SBT(nc, "aV", [128, 2, NCH, 130], BF16) as v_t, SBT(nc, "aE", [128, 6, 512], BF16) as e_t,
          SBT(nc, "aO", [128, 8, 129], F32) as o_t, SBT(nc, "aW", [128, 3, 4, 128], F32) as w_t,
          SBT(nc, "aSt", [128, 64], F32) as st_t, SBT(nc, "aG", [128, 2, 4, 128], F32) as gt_t,
          SBT(nc, "aM", [128, 2, 4, 128], BF16) as m_t):
        STRIP = Tile(strip_t, "strip")
        TH = [Tile(th_t[:, i, :], f"th{i}") for i in range(2)]
        T1 = Tile(t1_t, "t1")
        LG = Tile(lg_t, "lg")
        LV = Tile(lv_t, "lv")
        RG = Tile(rg_t, "rg")
        DG = Tile(dg_t, "dg")
        Q = [Tile(q_t[:, i, :], f"aq{i}") for i in range(2)]
        K = [Tile(k_t[:, i, :], f"ak{i}") for i in range(2)]
        V = [Tile(v_t[:, i], f"av{i}") for i in range(2)]
        EE = [Tile(e_t[:, i, :], f"ae{i}") for i in range(6)]
        O = Tile(o_t, "ao")
        W = [Tile(w_t[:, i], f"aw{i}") for i in range(3)]
        ST = Tile(st_t, "ast")
        GT = [Tile(gt_t[:, i], f"ag{i}") for i in range(2)]
        M = [Tile(m_t[:, i], f"am{i}") for i in range(2)]
        rIN = E.rIN
        P.dma("sp", lambda: nc.sync.dma_start(out=STRIP[:], in_=g.k_strip[:, :, :]), reads=[rIN], writes=[STRIP.res])
        P.dma("sp", lambda: nc.sync.dma_start(out=LG[:, 0:16], in_=g.ret_log_decay[L:L + 1, :].to_broadcast([128, 16])),
              reads=[rIN], writes=[LG.res])
        P.op("act", lambda: nc.scalar.activation(out=LG[:, 16:32], in_=LG[:, 0:16], func=AF.Exp), reads=[LG.res], writes=[LG.res])
        P.op("act", lambda: nc.scalar.activation(out=LG[:, 32:48], in_=LG[:, 16:32], func=AF.Ln, scale=-1.0, bias=1.0),
             reads=[LG.res], writes=[LG.res])
        P.dma("sp", lambda: nc.sync.dma_start(out=LV[:, 0:256], in_=g.diff_lambda[L:L + 1, :].to_broadcast([128, 256])),
              reads=[rIN], writes=[LV.res])
        lvv = LV[:, 0:256].rearrange("p (a f) -> p a f", a=4)
        prod = LV[:, 256:384].rearrange("p (a f) -> p a f", a=2)
        P.op("dve", lambda: nc.vector.tensor_tensor(out=prod[:, 0, :], in0=lvv[:, 0, :], in1=lvv[:, 1, :], op=ALU.mult),
             reads=[LV.res], writes=[LV.res])
        P.op("dve", lambda: nc.vector.tensor_tensor(out=prod[:, 1, :], in0=lvv[:, 2, :], in1=lvv[:, 3, :], op=ALU.mult),
             reads=[LV.res], writes=[LV.res])
        P.op("dve", lambda: nc.vector.tensor_reduce(out=LV[:, 384:386], in_=prod, axis=AX.X, op=ALU.add),
             reads=[LV.res], writes=[LV.res])
        P.op("act", lambda: nc.scalar.activation(out=LV[:, 386:388], in_=LV[:, 384:386], func=AF.Exp), reads=[LV.res], writes=[LV.res])
        P.op("dve", lambda: nc.vector.tensor_tensor(out=LV[:, 388:389], in0=LV[:, 387:388], in1=LV[:, 386:387], op=ALU.subtract),
             reads=[LV.res], writes=[LV.res])
        P.op("dve", lambda: nc.vector.tensor_scalar(out=LV[:, 389:390], in0=LV[:, 388:389], scalar1=-lam_init, scalar2=None, op0=ALU.add),
             reads=[LV.res], writes=[LV.res])
        NLAM = LV[:, 389:390]
        P.dma("sp", lambda: nc.sync.dma_start(out=RG[:], in_=g.ret_norm[L:L + 1, :].to_broadcast([128, 1024])), reads=[rIN], writes=[RG.res])
        P.dma("sp", lambda: nc.sync.dma_start(out=DG[:], in_=g.diff_norm[L:L + 1, :].to_broadcast([128, 1024])), reads=[rIN], writes=[DG.res])
        P.op("dve", lambda: nc.vector.tensor_scalar(out=DG[:], in0=DG[:], scalar1=1.0 - lam_init, scalar2=None, op0=ALU.mult),
             reads=[DG.res], writes=[DG.res])

        P.op("pool", lambda: nc.gpsimd.memset(ST[:, 56:64], -0.5), writes=[ST.res])
        ecnt = [0]
        scnt = [0]

        def small_rstd(nqc, var_lo, out_lo):
            P.op("pool", lambda: nc.gpsimd.tensor_tensor(out=ST[:, out_lo:out_lo + nqc], in0=ST[:, var_lo:var_lo + nqc],
                                                         in1=ST[:, 56:56 + nqc], op=ALU.pow), reads=[ST.res], writes=[ST.res])

        def store_mg(mt, nqc, col0, row0):
            P.dma("sp", lambda: nc.sync.dma_start(
                out=E.MG[row0:row0 + nqc * 128, col0:col0 + 128].rearrange("(c p) f -> p c f", p=128), in_=mt.t[:, 0:nqc, :]),
                reads=[mt.res], writes=[E.rMG])

        def evac_ret(ab, nqc, h, row0, gt, mt):
            W0, W1, W2 = W
            for qc in range(nqc):
                P.op("act", lambda qc=qc: nc.scalar.activation(out=W1.t[:, qc, :], in_=ab.t[:, qc * 128:(qc + 1) * 128], func=AF.Copy,
                                                               accum_out=ST[:, qc:qc + 1]), reads=[ab.res], writes=[W1.res, ST.res])
                P.op("act", lambda qc=qc: nc.scalar.activation(out=W2.t[:, qc, :], in_=ab.t[:, qc * 128:(qc + 1) * 128], func=AF.Square,
                                                               accum_out=ST[:, 8 + qc:9 + qc]), reads=[ab.res], writes=[W2.res, ST.res])
            P.op("pool", lambda: nc.gpsimd.tensor_scalar(out=ST[:, 16:16 + nqc], in0=ST[:, 0:nqc], scalar1=1.0 / 128, scalar2=0.0, op0=ALU.mult, op1=ALU.add),
                 reads=[ST.res], writes=[ST.res])
            P.op("pool", lambda: nc.gpsimd.tensor_tensor(out=ST[:, 24:24 + nqc], in0=ST[:, 16:16 + nqc], in1=ST[:, 16:16 + nqc], op=ALU.mult),
                 reads=[ST.res], writes=[ST.res])
            P.op("pool", lambda: nc.gpsimd.tensor_scalar(out=ST[:, 8:8 + nqc], in0=ST[:, 8:8 + nqc], scalar1=1.0 / 128, scalar2=EPS, op0=ALU.mult, op1=ALU.add),
                 reads=[ST.res], writes=[ST.res])
            P.op("pool", lambda: nc.gpsimd.tensor_tensor(out=ST[:, 24:24 + nqc], in0=ST[:, 8:8 + nqc], in1=ST[:, 24:24 + nqc], op=ALU.subtract),
                 reads=[ST.res], writes=[ST.res])
            small_rstd(nqc, 24, 40)
            P.op("pool", lambda: nc.gpsimd.tensor_tensor(out=ST[:, 48:48 + nqc], in0=ST[:, 16:16 + nqc], in1=ST[:, 40:40 + nqc], op=ALU.mult),
                 reads=[ST.res], writes=[ST.res])
            P.op("pool", lambda: nc.gpsimd.tensor_scalar(out=ST[:, 48:48 + nqc], in0=ST[:, 48:48 + nqc], scalar1=-1.0, scalar2=0.0, op0=ALU.mult, op1=ALU.add),
                 reads=[ST.res], writes=[ST.res])
            for qc in range(nqc):
                P.op("act", lambda qc=qc: nc.scalar.activation(out=W0.t[:, qc, :], in_=ab.t[:, qc * 128:(qc + 1) * 128], func=AF.Identity,
                                                               scale=ST[:, 40 + qc:41 + qc], bias=ST[:, 48 + qc:49 + qc]),
                     reads=[ab.res, ST.res], writes=[W0.res])
            gv = RG[:, h * 128:(h + 1) * 128].unsqueeze(1).to_broadcast([128, nqc, 128])
            P.op("pool", lambda: nc.gpsimd.tensor_tensor(out=W1.t[:, 0:nqc, :], in0=W0.t[:, 0:nqc, :], in1=gv, op=ALU.mult),
                 reads=[W0.res, RG.res], writes=[W1.res])
            P.op("pool", lambda: nc.gpsimd.tensor_tensor(out=mt.t[:, 0:nqc, :], in0=W1.t[:, 0:nqc, :], in1=gt.t[:, 0:nqc, :], op=ALU.mult),
                 reads=[W1.res, gt.res], writes=[mt.res])
            store_mg(mt, nqc, h * 128, row0)

        def evac_diff(nqc, h, row0, mt):
            W0, W1, W2 = W
            na = 2 * nqc
            for b3 in range((na + 2) // 3):
                n_in = min(3, na - b3 * 3)
                P.op("dve", lambda b3=b3, n_in=n_in: nc.vector.tensor_copy(
                    out=O[:, b3 * 3:b3 * 3 + n_in, :], in_=banks[4 + b3].t[:, 0:n_in * 129].rearrange("p (c f) -> p c f", c=n_in)),
                    reads=[banks[4 + b3].res], writes=[O.res])
            P.op("dve", lambda: nc.vector.reciprocal(out=ST[:, 48:48 + na], in_=O[:, 0:na, 128]), reads=[O.res], writes=[ST.res])
            P.op("dve", lambda: nc.vector.tensor_scalar(out=ST[:, 48 + nqc:48 + na], in0=ST[:, 48 + nqc:48 + na], scalar1=NLAM, scalar2=None,
                                                        op0=ALU.mult), reads=[ST.res, LV.res], writes=[ST.res])
            P.op("dve", lambda: nc.vector.tensor_tensor(
                out=W1.t[:, 0:nqc, :], in0=O[:, 0:nqc, 0:128], in1=ST[:, 48:48 + nqc].unsqueeze(2).to_broadcast([128, nqc, 128]), op=ALU.mult),
                reads=[O.res, ST.res], writes=[W1.res])
            P.op("dve", lambda: nc.vector.tensor_tensor(
                out=W2.t[:, 0:nqc, :], in0=O[:, nqc:na, 0:128], in1=ST[:, 48 + nqc:48 + na].unsqueeze(2).to_broadcast([128, nqc, 128]), op=ALU.mult),
                reads=[O.res, ST.res], writes=[W2.res])
            P.op("pool", lambda: nc.gpsimd.tensor_tensor(out=W0.t[:, 0:nqc, :], in0=W1.t[:, 0:nqc, :], in1=W2.t[:, 0:nqc, :], op=ALU.add),
                 reads=[W1.res, W2.res], writes=[W0.res])
            P.op("pool", lambda: nc.gpsimd.tensor_tensor(out=W2.t[:, 0:nqc, :], in0=W0.t[:, 0:nqc, :], in1=W0.t[:, 0:nqc, :], op=ALU.mult),
                 reads=[W0.res], writes=[W2.res])
            P.op("dve", lambda: nc.vector.tensor_reduce(out=ST[:, 16:16 + nqc], in_=W2.t[:, 0:nqc, :], axis=AX.X, op=ALU.add),
                 reads=[W2.res], writes=[ST.res])
            P.op("dve", lambda: nc.vector.tensor_scalar(out=ST[:, 24:24 + nqc], in0=ST[:, 16:16 + nqc], scalar1=1.0 / 128, scalar2=EPS,
                                                        op0=ALU.mult, op1=ALU.add), reads=[ST.res], writes=[ST.res])
            small_rstd(nqc, 24, 40)
            P.op("dve", lambda: nc.vector.tensor_tensor(
                out=W1.t[:, 0:nqc, :], in0=W0.t[:, 0:nqc, :], in1=ST[:, 40:40 + nqc].unsqueeze(2).to_broadcast([128, nqc, 128]), op=ALU.mult),
                reads=[W0.res, ST.res], writes=[W1.res])
            gv = DG[:, h * 128:(h + 1) * 128].unsqueeze(1).to_broadcast([128, nqc, 128])
            P.op("pool", lambda: nc.gpsimd.tensor_tensor(out=mt.t[:, 0:nqc, :], in0=W1.t[:, 0:nqc, :], in1=gv, op=ALU.mult),
                 reads=[W1.res, DG.res], writes=[mt.res])
            store_mg(mt, nqc, 1024 + h * 128, row0)

        rcnt = [0]
        for hh in range(16):
            is_ret = hh < 8
            h = hh % 8
            k2 = hh % 2
            q_, k_, v_ = Q[k2], K[k2], V[k2]
            if is_ret:
                po = (h % 2) * 64
                P.dma("sp", lambda: nc.sync.dma_start(out=q_.t[0:64, :], in_=E.QT[h // 2, po:po + 64, :]), reads=[E.rQT], writes=[q_.res])
                P.dma("sp", lambda: nc.sync.dma_start(out=k_.t[0:64, :], in_=E.QT[4 + h // 2, po:po + 64, :]), reads=[E.rQT], writes=[k_.res])
                th = TH[h % 2]
                P.op("dve", lambda: nc.vector.tensor_scalar(out=T1[:], in0=STRIP[:, 0, :], scalar1=LG[:, 32 + h:33 + h], scalar2=None, op0=ALU.mult),
                     reads=[STRIP.res, LG.res], writes=[T1.res])
                P.op("dve", lambda: nc.vector.scalar_tensor_tensor(out=T1[:], in0=STRIP[:, 1, :], scalar=LG[:, 40 + h:41 + h], in1=T1[:],
                                                                   op0=ALU.mult, op1=ALU.add), reads=[STRIP.res, LG.res, T1.res], writes=[T1.res])
                P.op("act", lambda: nc.scalar.activation(out=th.t, in_=T1[:], func=AF.Exp), reads=[T1.res], writes=[th.res])
            else:
                P.dma("sp", lambda: nc.sync.dma_start(out=q_.t[:, :], in_=E.QT[8 + h, :, :]), reads=[E.rQT], writes=[q_.res])
                P.dma("sp", lambda: nc.sync.dma_start(out=k_.t[:, :], in_=E.QT[16 + h, :, :]), reads=[E.rQT], writes=[k_.res])
            P.dma("sp", lambda: nc.sync.dma_start(out=v_.t, in_=E.VD[:, hh, :].rearrange("(c p) e -> p c e", p=128)),
                  reads=[E.rVD], writes=[v_.res])
            groups = []
            if not E.last:
                ent = [(0, 0), (1, 128)]
                groups.append((0, 0, 256, ent, 0))
            for qb in range(4):
                if is_ret:
                    ent = [(0, -256), (1, -128)] + [(2 + j, j * 128) for j in range(16)] + [(0, 2048), (1, 2176)]
                else:
                    ent = [(j, 0) for j in range(NCH)]
                groups.append((CTX + qb * 512, CTX + qb * 512, 512, ent, qb * 512))
            for gi, (row0, qcol0, nq, ent, qpos0) in enumerate(groups):
                nqc = nq // 128
                gt, mt = GT[gi % 2], M[gi % 2]
                if is_ret:
                    P.dma("sp", lambda: nc.sync.dma_start(
                        out=gt.t[:, 0:nqc, :], in_=E.GD[row0:row0 + nq, h * 128:(h + 1) * 128].rearrange("(c p) f -> p c f", p=128)),
                        reads=[E.rGD], writes=[gt.res])
                npair = 1 if is_ret else 2

                def emit_S(ei):
                    kc, pos = ent[ei]
                    outs = []
                    for pr in range(npair):
                        bi = scnt[0] % 4
                        scnt[0] += 1
                        sb_ = banks[bi]
                        if is_ret:
                            lhs = k_.t[0:64, kc * 128:(kc + 1) * 128]
                            rhs = q_.t[0:64, qcol0:qcol0 + nq]
                        else:
                            lhs = k_.t[pr * 64:(pr + 1) * 64, kc * 128:(kc + 1) * 128]
                            rhs = q_.t[pr * 64:(pr + 1) * 64, qcol0:qcol0 + nq]
                        P.op("pe", lambda sb_=sb_, lhs=lhs, rhs=rhs: nc.tensor.matmul(sb_.t[:, 0:nq], lhsT=lhs, rhs=rhs, start=True, stop=True),
                             reads=[k_.res, q_.res], writes=[sb_.res])
                        outs.append(sb_)
                    return outs

                pend_S = emit_S(0)
                for ei in range(len(ent)):
                    kc, pos = ent[ei]
                    cur_S = pend_S
                    if ei + 1 < len(ent):
                        pend_S = emit_S(ei + 1)
                    es = []
                    for pr in range(npair):
                        et = EE[ecnt[0] % 6]
                        ecnt[0] += 1
                        sb_ = cur_S[pr]
                        if is_ret:
                            off = qpos0 - pos + 2176
                            P.op("dve", lambda et=et, sb_=sb_, off=off: nc.vector.tensor_tensor(
                                out=et.t[:, 0:nq], in0=sb_.t[:, 0:nq], in1=th.t[:, off:off + nq], op=ALU.mult),
                                reads=[sb_.res, th.res], writes=[et.res])
                        else:
                            P.op("act", lambda et=et, sb_=sb_: nc.scalar.activation(out=et.t[:, 0:nq], in_=sb_.t[:, 0:nq], func=AF.Exp),
                                 reads=[sb_.res], writes=[et.res])
                        es.append(et)
                    for pr in range(npair):
                        for qc in range(nqc):
                            if is_ret:
                                ab = banks[4 + rcnt[0] % 2]
                                dst = ab.t[:, qc * 128:(qc + 1) * 128]
                                rhs = v_.t[:, kc, 0:128]
                                st_flag = (ei == 0 and qc == 0)
                            else:
                                a = pr * nqc + qc
                                ab = banks[4 + a // 3]
                                sl = a % 3
                                dst = ab.t[:, sl * 129:(sl + 1) * 129]
                                rhs = v_.t[:, kc, 0:129]
                                st_flag = (ei == 0 and sl == 0)
                            lastmm = (ei == len(ent) - 1)
                            P.op("pe", lambda dst=dst, rhs=rhs, et=es[pr], qc=qc, st_flag=st_flag, lastmm=lastmm: nc.tensor.matmul(
                                dst, lhsT=et.t[:, qc * 128:(qc + 1) * 128], rhs=rhs, start=st_flag, stop=lastmm, skip_group_check=True),
                                reads=[es[pr].res, v_.res], writes=[ab.res], inc=(lastmm or qc == nqc - 1))
                if is_ret:
                    evac_ret(banks[4 + rcnt[0] % 2], nqc, h, row0, gt, mt)
                    rcnt[0] += 1
                else:
                    evac_diff(nqc, h, row0, mt)
        P.barrier()


def phase_outproj(E):
    nc, P, g, L = E.nc, E.P, E.g, E.L
    banks, bank_bf = E.banks, E.bank_bf
    with (SBT(nc, "oW", [128, 16, D], BF16) as ow_t, SBT(nc, "oG", [128, 2, D], F32) as og_t,
          SBT(nc, "oM", [128, 2, D], BF16) as om_t, SBT(nc, "oMT", [128, 2, 16, 128], BF16) as omt_t,
          SBT(nc, "oX", [128, 2, D], F32) as ox_t, SBT(nc, "oT", [128, 2, 512], F32) as ot_t):
        Wo = Tile(ow_t, "oW")
        G2 = [Tile(og_t[:, i, :], f"oG{i}") for i in range(2)]
        Mc = [Tile(om_t[:, i, :], f"oM{i}") for i in range(2)]
        MT = [Tile(omt_t[:, i], f"oMT{i}") for i in range(2)]
        X = [Tile(ox_t[:, i, :], f"oX{i}") for i in range(2)]
        Tm = [Tile(ot_t[:, i, :], f"oT{i}") for i in range(2)]
        src = g.w_out[L].rearrange("(c p) n -> p c n", p=128)
        for q4 in range(4):
            P.dma("pool", lambda q4=q4: nc.gpsimd.dma_start(out=Wo[:, q4 * 4:(q4 + 1) * 4, :], in_=src[:, q4 * 4:(q4 + 1) * 4, :]),
                  reads=[E.rIN], writes=[Wo.res])
        E.load_bc(G2[1], E.MOD[0:1, 2 * D:3 * D])
        E.load_bc(G2[0], E.MOD[1:2, 2 * D:3 * D])
        cnt = 0

        def stage_a(tc):
            k = tc % 2
            m_, mt_, x_ = Mc[k], MT[k], X[k]
            P.dma("sp", lambda: nc.sync.dma_start(out=m_.t, in_=E.MG[tc * 128:(tc + 1) * 128, :]), reads=[E.rMG], writes=[m_.res])
            P.dma("sp", lambda: nc.sync.dma_start(out=x_.t, in_=E.XS[tc * 128:(tc + 1) * 128, :]), reads=[E.rXS], writes=[x_.res])
            for half in range(2):
                bi = (tc % 2) * 2 + half
                pb, pbv = banks[bi], bank_bf(bi)
                for j in range(8):
                    dc = half * 8 + j
                    P.op("pe", lambda dc=dc, j=j, pbv=pbv: nc.tensor.transpose(
                        out=pbv[:, j * 128:(j + 1) * 128], in_=m_.t[:, dc * 128:(dc + 1) * 128], identity=E.ident[:]),
                        reads=[m_.res, E.ident.res], writes=[pb.res], inc=(j == 7))
                dst = mt_.t[:, half * 8:(half + 1) * 8, :]
                if half == 0:
                    P.op("act", lambda dst=dst, pbv=pbv: nc.scalar.copy(out=dst, in_=pbv.rearrange("p (j t) -> p j t", j=8)),
                         reads=[pb.res], writes=[mt_.res])
                else:
                    P.op("dve", lambda dst=dst, pbv=pbv: nc.vector.tensor_copy(out=dst, in_=pbv.rearrange("p (j t) -> p j t", j=8)),
                         reads=[pb.res], writes=[mt_.res])

        tcs = list(range(E.tc0, NCH))
        stage_a(tcs[0])
        for ti, tc in enumerate(tcs):
            k = tc % 2
            isx = 1 if tc >= 2 else 0
            m_, mt_, x_ = Mc[k], MT[k], X[k]
            for db in range(4):
                pb = banks[4 + cnt % 4]
                tm = Tm[cnt % 2]
                cnt += 1
                for mc in range(16):
                    P.op("pe", lambda mc=mc, pb=pb, db=db: nc.tensor.matmul(
                        pb.t[:, :], lhsT=mt_.t[:, mc, :], rhs=Wo[:, mc, db * 512:(db + 1) * 512], start=(mc == 0), stop=(mc == 15)),
                        reads=[mt_.res, Wo.res], writes=[pb.res], inc=(mc == 15))
                if db == 0 and ti + 1 < len(tcs):
                    stage_a(tcs[ti + 1])
                P.op("dve", lambda pb=pb, tm=tm, db=db: nc.vector.tensor_tensor(
                    out=tm.t, in0=pb.t[:, :], in1=G2[isx].t[:, db * 512:(db + 1) * 512], op=ALU.mult),
                    reads=[pb.res, G2[isx].res], writes=[tm.res])
                P.op("pool", lambda tm=tm, db=db: nc.gpsimd.tensor_tensor(
                    out=x_.t[:, db * 512:(db + 1) * 512], in0=x_.t[:, db * 512:(db + 1) * 512], in1=tm.t, op=ALU.add),
                    reads=[tm.res, x_.res], writes=[x_.res])
            P.dma("sp", lambda: nc.sync.dma_start(out=E.XS[tc * 128:(tc + 1) * 128, :], in_=x_.t), reads=[x_.res], writes=[E.rXS])
        P.barrier()


def phase_topk(E):
    import contextlib
    with contextlib.ExitStack() as stack:
        E.mod_alloc = None
        if E.next_mod is not None:
            nc = E.nc
            E.mod_alloc = (stack.enter_context(SBT(nc, "mw0", [128, 16, 512], BF16)),
                           stack.enter_context(SBT(nc, "mw1", [128, 16, 512], BF16)),
                           stack.enter_context(SBT(nc, "mb", [2, 2, 512], F32)),
                           stack.enter_context(SBT(nc, "mo", [2, 2, 512], F32)))
        _phase_topk(E)


def _phase_topk(E):
    nc, P, g, L = E.nc, E.P, E.g, E.L
    banks = E.banks
    AFF, IDXU, GATE, identf = E.AFF, E.IDXU, E.GATE, E.identf
    with (SBT(nc, "kAT", [16, T], F32) as at_t, SBT(nc, "kWK", [16, T], F32) as wk_t,
          SBT(nc, "kM8", [16, 16], F32) as m8_t, SBT(nc, "kMK", [16, T], F32) as mk_t,
          SBT(nc, "kPS", [16, T], F32) as ps_t, SBT(nc, "kON", [16, T], F32) as on_t,
          SBT(nc, "kSL", [128, NCH, NE], F32) as sl_t, SBT(nc, "kPm", [128, 3, 256], F32) as pm_t,
          SBT(nc, "kTG", [128, NCH, NE, 2], F32) as tg_t, SBT(nc, "kIO", [128, NSLOT], F32) as io_t,
          SBT(nc, "kTK", [128, NCH], F32) as tk_t, SBT(nc, "kIG", [128, NE, 3, 2], F32) as ig_t):
        AT, WK, M8, MK, PS, ON = (Tile(t, n) for t, n in ((at_t, "kAT"), (wk_t, "kWK"), (m8_t, "kM8"), (mk_t, "kMK"), (ps_t, "kPS"), (on_t, "kON")))
        SL, TG, IO, TK, IG = (Tile(t, n) for t, n in ((sl_t, "kSL"), (tg_t, "kTG"), (io_t, "kIO"), (tk_t, "kTK"), (ig_t, "kIG")))
        PM = [Tile(pm_t[:, i, :], f"kPm{i}") for i in range(3)]
        mgen = None
        if E.next_mod is not None:
            w0, w1, mb_t, mo_t = E.mod_alloc
            ws, mb, mo = E.mod_tiles(w0, w1, mb_t, mo_t)
            mgen = E.mod_gen(E.next_mod, ws, mb, mo, [banks[7]])

        def mod_step(n=1):
            if mgen is not None:
                for _ in range(n):
                    next(mgen, None)

        P.dma("sp", lambda: nc.sync.dma_start(out=IO[:], in_=g.k_iota[:, :]), reads=[E.rIN], writes=[IO.res])
        P.dma("sp", lambda: nc.sync.dma_start(out=TK[:], in_=g.k_tokid[:, :]), reads=[E.rIN], writes=[TK.res])
        P.op("pool", lambda: nc.gpsimd.memset(ON[:], 1.0), writes=[ON.res])
        P.op("pool", lambda: nc.gpsimd.memset(IG[:], 0.0), writes=[IG.res])
        tcs = list(range(E.tc0, NCH))
        for gi in range(0, len(tcs), 4):
            grp = tcs[gi:gi + 4]
            pb = banks[(gi // 4) % 2]
            for j, tc in enumerate(grp):
                P.op("pe", lambda j=j, tc=tc, pb=pb: nc.tensor.transpose(
                    out=pb.t[0:16, j * 128:(j + 1) * 128], in_=AFF[:, tc, :], identity=identf[:]),
                    reads=[AFF.res, identf.res], writes=[pb.res], inc=(j == len(grp) - 1))
            c0 = grp[0] * 128
            n = len(grp) * 128
            P.op("act", lambda pb=pb, c0=c0, n=n: nc.scalar.copy(out=AT[:, c0:c0 + n], in_=pb.t[0:16, 0:n]), reads=[pb.res], writes=[AT.res])
        ranges = [(CTX, T, CAPX, 0)] + ([] if E.last else [(0, CTX, CAPC, 1)])
        for (a, b, cap, ti) in ranges:
            P.op("dve", lambda a=a, b=b: nc.vector.tensor_copy(out=WK[:, a:b], in_=AT[:, a:b]), reads=[AT.res], writes=[WK.res])
            nr = cap // 8
            for r in range(nr):
                P.op("dve", lambda a=a, b=b, ti=ti: nc.vector.max(out=M8[:, ti * 8:ti * 8 + 8], in_=WK[:, a:b]), reads=[WK.res], writes=[M8.res])
                mod_step(1)
                if r < nr - 1:
                    P.op("dve", lambda a=a, b=b, ti=ti: nc.vector.match_replace(
                        out=WK[:, a:b], in_to_replace=M8[:, ti * 8:ti * 8 + 8], in_values=WK[:, a:b], imm_value=-1.0),
                        reads=[WK.res, M8.res], writes=[WK.res])
            thr = M8[:, ti * 8 + 7:ti * 8 + 8]
            P.op("dve", lambda a=a, b=b, thr=thr: nc.vector.tensor_scalar(out=MK[:, a:b], in0=AT[:, a:b], scalar1=thr, scalar2=None, op0=ALU.is_ge),
                 reads=[AT.res, M8.res], writes=[MK.res])
            P.op("dve", lambda a=a, b=b: nc.vector.tensor_tensor_scan(out=PS[:, a:b], data0=ON[:, a:b], data1=MK[:, a:b], initial=0.0,
                                                                       op0=ALU.mult, op1=ALU.add), reads=[ON.res, MK.res], writes=[PS.res])
            if ti == 1:
                P.op("dve", lambda a=a, b=b: nc.vector.tensor_scalar(out=PS[:, a:b], in0=PS[:, a:b], scalar1=float(CAPX), scalar2=None, op0=ALU.add),
                     reads=[PS.res], writes=[PS.res])
            P.op("dve", lambda a=a, b=b: nc.vector.tensor_tensor(out=PS[:, a:b], in0=PS[:, a:b], in1=MK[:, a:b], op=ALU.mult),
                 reads=[PS.res, MK.res], writes=[PS.res])
            P.op("dve", lambda a=a, b=b: nc.vector.tensor_scalar(out=PS[:, a:b], in0=PS[:, a:b], scalar1=-1.0, scalar2=None, op0=ALU.add),
                 reads=[PS.res], writes=[PS.res])
        for gi in range(0, len(tcs), 8):
            grp = tcs[gi:gi + 8]
            pb = banks[2 + (gi // 8) % 2]
            for j, tc in enumerate(grp):
                P.op("pe", lambda j=j, tc=tc, pb=pb: nc.tensor.transpose(
                    out=pb.t[:, j * 16:(j + 1) * 16], in_=PS[:, tc * 128:(tc + 1) * 128], identity=identf[0:16, 0:16]),
                    reads=[PS.res, identf.res], writes=[pb.res], inc=(j == len(grp) - 1))
            P.op("act", lambda pb=pb, grp=grp: nc.scalar.copy(
                out=SL[:, grp[0]:grp[0] + len(grp), :], in_=pb.t[:, 0:len(grp) * 16].rearrange("p (c e) -> p c e", e=16)),
                reads=[pb.res], writes=[SL.res])
        P.op("dve", lambda: nc.vector.tensor_copy(out=TG[:, :, :, 0], in_=TK[:, :].unsqueeze(2).to_broadcast([128, NCH, NE])),
             reads=[TK.res], writes=[TG.res])
        P.op("dve", lambda: nc.vector.tensor_copy(out=TG[:, :, :, 1], in_=AFF[:]), reads=[AFF.res, TG.res], writes=[TG.res])
        pc = 0
        bA, bB, bC = banks[4], banks[5], banks[6]
        for e in range(NE):
            for tc in tcs:
                pm = PM[pc % 3]
                pc += 1
                if tc >= 2:
                    P.op("dve", lambda pm=pm, tc=tc: nc.vector.tensor_scalar(
                        out=pm.t[:, 0:256], in0=IO[:, 0:256], scalar1=SL[:, tc, e:e + 1], scalar2=None, op0=ALU.is_equal),
                        reads=[IO.res, SL.res], writes=[pm.res])
                    for cc, bb in ((0, bA), (1, bB)):
                        P.op("pe", lambda pm=pm, tc=tc, cc=cc, bb=bb: nc.tensor.matmul(
                            bb.t[:, e * 2:(e + 1) * 2], lhsT=pm.t[:, cc * 128:(cc + 1) * 128], rhs=TG[:, tc, e, :],
                            start=(tc == 2), stop=(tc == NCH - 1), skip_group_check=True),
                            reads=[pm.res, TG.res], writes=[bb.res], inc=(cc == 1))
                else:
                    P.op("dve", lambda pm=pm, tc=tc: nc.vector.tensor_scalar(
                        out=pm.t[:, 0:32], in0=IO[:, 256:288], scalar1=SL[:, tc, e:e + 1], scalar2=None, op0=ALU.is_equal),
                        reads=[IO.res, SL.res], writes=[pm.res])
                    P.op("pe", lambda pm=pm, tc=tc: nc.tensor.matmul(
                        bC.t[0:32, e * 2:(e + 1) * 2], lhsT=pm.t[:, 0:32], rhs=TG[:, tc, e, :],
                        start=(tc == 0), stop=(tc == 1), skip_group_check=True),
                        reads=[pm.res, TG.res], writes=[bC.res])
        mod_step(24)
        P.op("act", lambda: nc.scalar.copy(out=IG[:, :, 0, :], in_=bA.t[:, 0:32].rearrange("p (e two) -> p e two", two=2)),
             reads=[bA.res, IG.res], writes=[IG.res])
        P.op("act", lambda: nc.scalar.copy(out=IG[:, :, 1, :], in_=bB.t[:, 0:32].rearrange("p (e two) -> p e two", two=2)),
             reads=[bB.res, IG.res], writes=[IG.res])
        if not E.last:
            P.op("act", lambda: nc.scalar.copy(out=IG[0:32, :, 2, :], in_=bC.t[0:32, 0:32].rearrange("p (e two) -> p e two", two=2)),
                 reads=[bC.res, IG.res], writes=[IG.res])
        P.op("dve", lambda: nc.vector.tensor_copy(out=IDXU[:], in_=IG[:, :, :, 0]), reads=[IG.res], writes=[IDXU.res])
        P.op("dve", lambda: nc.vector.tensor_copy(out=GATE[:], in_=IG[:, :, :, 1]), reads=[IG.res], writes=[GATE.res])
        P.barrier()


def phase_moe(E):
    nc, P, g, L = E.nc, E.P, E.g, E.L
    banks, bank_bf = E.banks, E.bank_bf
    IDXU, GATE = E.IDXU, E.GATE
    ncc = 2 if E.last else 3
    nsl = CAPX if E.last else NSLOT
    with (SBT(nc, "eG5", [128, 2, D], F32) as g5_t, SBT(nc, "eXG", [128, 2, 3, D], BF16) as xg_t,
          SBT(nc, "eXT", [128, 2, 16, NSLOT], BF16) as xt_t, SBT(nc, "eGU", [128, 2, 2, 16, 512], BF16) as gu_t,
          SBT(nc, "eWD", [128, 8, D], BF16) as wd_t, SBT(nc, "eHT", [128, 8, NSLOT], BF16) as ht_t,
          SBT(nc, "eSG", [128, 2, 512], F32) as sg_t, SBT(nc, "eHS", [128, 3, FF], BF16) as hs_t, SBT(nc, "eY", [128, 2, D], F32) as y_t):
        G5 = [Tile(g5_t[:, i, :], f"eG5{i}") for i in range(2)]
        XG = [Tile(xg_t[:, i], f"eXG{i}") for i in range(2)]
        XT = [Tile(xt_t[:, i], f"eXT{i}") for i in range(2)]
        GU = [Tile(gu_t[:, i], f"eGU{i}") for i in range(2)]
        WD = Tile(wd_t, "eWD")
        HT = Tile(ht_t, "eHT")
        HS = Tile(hs_t, "eHS")
        SG = [Tile(sg_t[:, i, :], f"eSG{i}") for i in range(2)]
        Y = [Tile(y_t[:, i, :], f"eY{i}") for i in range(2)]
        E.load_bc(G5[1], E.MOD[0:1, 5 * D:6 * D])
        E.load_bc(G5[0], E.MOD[1:2, 5 * D:6 * D])
        ycnt = 0
        bcnt = 0

        def issue_gather(e):
            xg = XG[e % 2]
            for cc in range(ncc):
                M_ = 128 if cc < 2 else CAPC
                P.dma("pool", lambda cc=cc, M_=M_: nc.gpsimd.indirect_dma_start(
                    out=xg.t[0:M_, cc, :], out_offset=None, in_=E.HT2[:, :],
                    in_offset=bass.IndirectOffsetOnAxis(ap=IDXU[0:M_, e, cc:cc + 1], axis=0)),
                    reads=[IDXU.res, E.rHT2], writes=[xg.res])

        def issue_gu(e, fh):
            gu = GU[fh]
            for wi, wsrc in enumerate((g.w_gate, g.w_up)):
                src_ = wsrc[L, e][:, fh * 512:(fh + 1) * 512].rearrange("(c p) n -> p c n", p=128)
                for hh in range(2):
                    P.dma("pool", lambda wi=wi, src_=src_, hh=hh: nc.gpsimd.dma_start(
                        out=gu.t[:, wi, hh * 8:(hh + 1) * 8, :], in_=src_[:, hh * 8:(hh + 1) * 8, :]), reads=[E.rIN], writes=[gu.res])

        def issue_wd(e):
            srcd = g.w_down[L, e].rearrange("(c p) n -> p c n", p=128)
            for hh in range(2):
                P.dma("pool", lambda hh=hh: nc.gpsimd.dma_start(out=WD[:, hh * 4:(hh + 1) * 4, :], in_=srcd[:, hh * 4:(hh + 1) * 4, :]),
                      reads=[E.rIN], writes=[WD.res])

        def do_transposes(e):
            xg, xt = XG[e % 2], XT[e % 2]
            for dc in range(16):
                bi = dc % 2
                pb, pbv = banks[bi], bank_bf(bi)
                for cc in range(ncc):
                    M_ = 128 if cc < 2 else CAPC
                    P.op("pe", lambda cc=cc, M_=M_, dc=dc, pbv=pbv: nc.tensor.transpose(
                        out=pbv[:, cc * 128:cc * 128 + M_], in_=xg.t[0:M_, cc, dc * 128:(dc + 1) * 128], identity=E.ident[0:M_, 0:M_]),
                        reads=[xg.res, E.ident.res], writes=[pb.res], inc=(cc == ncc - 1))
                if dc % 2 == 0:
                    P.op("act", lambda dc=dc, pbv=pbv: nc.scalar.copy(out=xt.t[:, dc, 0:nsl], in_=pbv[:, 0:nsl]), reads=[pb.res], writes=[xt.res])
                else:
                    P.op("dve", lambda dc=dc, pbv=pbv: nc.vector.tensor_copy(out=xt.t[:, dc, 0:nsl], in_=pbv[:, 0:nsl]), reads=[pb.res], writes=[xt.res])

        issue_gather(0)
        issue_gu(0, 0)
        issue_gu(0, 1)
        issue_wd(0)
        do_transposes(0)
        for e in range(NE):
            xt = XT[e % 2]
            if e + 1 < NE:
                issue_gather(e + 1)
            for fh in range(2):
                gu = GU[fh]
                for sc in range(ncc):
                    M_ = 128 if sc < 2 else CAPC
                    pg, pu = banks[2 + (bcnt % 2) * 2], banks[3 + (bcnt % 2) * 2]
                    sg = SG[bcnt % 2]
                    bcnt += 1
                    for wi, pb in ((0, pg), (1, pu)):
                        for dc in range(16):
                            P.op("pe", lambda wi=wi, pb=pb, dc=dc, gu=gu, sc=sc, M_=M_: nc.tensor.matmul(
                                pb.t[0:M_, :], lhsT=xt.t[:, dc, sc * 128:sc * 128 + M_], rhs=gu.t[:, wi, dc, :],
                                start=(dc == 0), stop=(dc == 15)), reads=[gu.res, xt.res], writes=[pb.res], inc=(dc == 15))
                    P.op("act", lambda pg=pg, sg=sg, M_=M_: nc.scalar.activation(out=sg.t[0:M_, :], in_=pg.t[0:M_, :], func=AF.Silu),
                         reads=[pg.res], writes=[sg.res])
                    P.op("dve", lambda pu=pu, sg=sg, fh=fh, sc=sc, M_=M_: nc.vector.tensor_tensor(
                        out=HS[0:M_, sc, fh * 512:(fh + 1) * 512], in0=pu.t[0:M_, :], in1=sg.t[0:M_, :], op=ALU.mult),
                        reads=[pu.res, sg.res], writes=[HS.res])
                if e + 1 < NE:
                    issue_gu(e + 1, fh)
                    if fh == 1:
                        do_transposes(e + 1)
            for fc in range(8):
                bi = fc % 2
                pb, pbv = banks[bi], bank_bf(bi)
                for sc in range(ncc):
                    M_ = 128 if sc < 2 else CAPC
                    P.op("pe", lambda sc=sc, M_=M_, fc=fc, pbv=pbv: nc.tensor.transpose(
                        out=pbv[:, sc * 128:sc * 128 + M_], in_=HS[0:M_, sc, fc * 128:(fc + 1) * 128], identity=E.ident[0:M_, 0:M_]),
                        reads=[HS.res, E.ident.res], writes=[pb.res], inc=(sc == ncc - 1))
                if fc % 2 == 0:
                    P.op("act", lambda fc=fc, pbv=pbv: nc.scalar.copy(out=HT[:, fc, 0:nsl], in_=pbv[:, 0:nsl]), reads=[pb.res], writes=[HT.res])
                else:
                    P.op("dve", lambda fc=fc, pbv=pbv: nc.vector.tensor_copy(out=HT[:, fc, 0:nsl], in_=pbv[:, 0:nsl]), reads=[pb.res], writes=[HT.res])
            for cc in range(ncc):
                M_ = 128 if cc < 2 else CAPC
                isx = 1 if cc < 2 else 0
                y = Y[ycnt % 2]
                ycnt += 1
                for db in range(4):
                    pb = banks[6 + db % 2]
                    for fc in range(8):
                        P.op("pe", lambda fc=fc, pb=pb, db=db, cc=cc, M_=M_: nc.tensor.matmul(
                            pb.t[0:M_, :], lhsT=HT[:, fc, cc * 128:cc * 128 + M_], rhs=WD[:, fc, db * 512:(db + 1) * 512],
                            start=(fc == 0), stop=(fc == 7)), reads=[HT.res, WD.res], writes=[pb.res], inc=(fc == 7))
                    P.op("dve", lambda pb=pb, db=db, cc=cc, M_=M_, y=y, isx=isx: nc.vector.scalar_tensor_tensor(
                        out=y.t[0:M_, db * 512:(db + 1) * 512], in0=pb.t[0:M_, :], scalar=GATE[0:M_, e, cc:cc + 1],
                        in1=G5[isx].t[0:M_, db * 512:(db + 1) * 512], op0=ALU.mult, op1=ALU.mult),
                        reads=[pb.res, GATE.res, G5[isx].res], writes=[y.res])
                P.dma("pool", lambda cc=cc, M_=M_, y=y: nc.gpsimd.indirect_dma_start(
                    out=E.XS[:, :], out_offset=bass.IndirectOffsetOnAxis(ap=IDXU[0:M_, e, cc:cc + 1], axis=0),
                    in_=y.t[0:M_, :], in_offset=None, compute_op=ALU.add),
                    reads=[y.res, IDXU.res, E.rXS], writes=[E.rXS], serial=True)
            if e + 1 < NE:
                issue_wd(e + 1)
        P.barrier()


NCORES = 4


def kernel(x, c, ctx, c_ctx, w_ada, b_ada, norm_mix, norm_ffn, w_in, w_out, ret_log_decay,
           ret_norm, diff_lambda, diff_norm, w_router, w_gate, w_up, w_down, norm_final):
    f = lambda a: np.ascontiguousarray(np.asarray(a, dtype=np.float32))
    nc = build(depth=DEPTH)
    shared = dict(
        w_ada=f(w_ada), b_ada=f(b_ada), norm_mix=f(norm_mix), norm_ffn=f(norm_ffn), w_in=f(w_in), w_out=f(w_out),
        ret_log_decay=f(ret_log_decay).reshape(DEPTH, 16), ret_norm=f(ret_norm),
        diff_lambda=f(diff_lambda).reshape(DEPTH, 256), diff_norm=f(diff_norm), w_router=f(w_router),
        w_gate=f(w_gate), w_up=f(w_up), w_down=f(w_down), norm_final=f(norm_final).reshape(1, D))
    shared.update(host_consts())
    x, c, ctx, c_ctx = f(x), f(c), f(ctx), f(c_ctx)
    in_maps = []
    for b in range(NCORES):
        m = dict(shared)
        m.update(x=x[b], ctx=ctx[b], cvec=np.ascontiguousarray(np.stack([c[b], c_ctx])))
        in_maps.append({k: m[k] for k in nc._used_inputs})
    res = run_bass_kernel_spmd(nc, in_maps, core_ids=list(range(NCORES)))
    return np.stack([np.asarray(res.results[b]["out"], dtype=np.float32) for b in range(NCORES)])
```

```python
import numpy as np
import ml_dtypes
import concourse.bass as bass
import concourse.mybir as mybir
from concourse.bass_utils import run_bass_kernel_spmd

F32 = mybir.dt.float32
BF16 = mybir.dt.bfloat16
U32 = mybir.dt.uint32
AF = mybir.ActivationFunctionType
ALU = mybir.AluOpType
AX = mybir.AxisListType

D = 2048
SEQ = 2048
CTX = 256
T = SEQ + CTX
NCH = T // 128
DEPTH = 4
NE = 16
FF = 1024
PROJ = 6144
EPS = 1e-6
CAPX = 256
CAPC = 32
NSLOT = CAPX + CAPC
DMIN = -(SEQ + CTX - 128) - 127
FLEN = 4608
SW = 4480


_uniq = [0]


def SBT(nc, name, shape, dt):
    _uniq[0] += 1
    return nc.sbuf_tensor(f"{name}_{_uniq[0]}", shape, dt)


class Res:
    __slots__ = ("name", "w", "r", "pw")

    def __init__(self, name):
        self.name = name
        self.w = None
        self.r = {}
        self.pw = []


class Prog:
    ENG = ("pe", "act", "dve", "pool", "sp")

    def __init__(self, nc):
        self.nc = nc
        self.e = dict(pe=nc.tensor, act=nc.scalar, dve=nc.vector, pool=nc.gpsimd, sp=nc.sync)
        self.psem = {k: nc.alloc_semaphore("p_" + k) for k in self.ENG}
        self.pcnt = {k: 0 for k in self.ENG}
        self.pend = {k: False for k in self.ENG}
        self.seen = {k: {} for k in self.ENG}
        self.dsem = {}
        self.dval = {}
        self.dnext = {}
        for q, n in (("sp", 20), ("pool", 20), ("act", 6)):
            self.dsem[q] = [nc.alloc_semaphore(f"d_{q}{i}") for i in range(n)]
            self.dval[q] = [0] * n
            self.dnext[q] = 0
        self.bsem = nc.alloc_semaphore("barrier")
        self.bcnt = 0
        self.ninst = 0

    def _semof(self, key):
        if key[0] == "e":
            return self.psem[key[1]]
        return self.dsem[key[1]][key[2]]

    def _need(self, eng, events):
        need = {}
        for ev in events:
            if ev is None:
                continue
            key, val = ev
            if key == ("e", "pe") and eng == "pe":
                continue
            if self.seen[eng].get(key, 0) >= val:
                continue
            if need.get(key, 0) < val:
                need[key] = val
        return need

    def _emit(self, eng, fn, need):
        items = list(need.items())
        for key, val in items[:-1]:
            self.e[eng].wait_ge(self._semof(key), val)
            self.ninst += 1
        ins = fn()
        if items:
            key, val = items[-1]
            ins._wait_ge(self._semof(key), val)
        for key, val in items:
            self.seen[eng][key] = val
        self.ninst += 1
        return ins

    def op(self, eng, fn, reads=(), writes=(), inc=True):
        evs = []
        for r in reads:
            evs.append(r.w)
            evs.extend(r.pw)
        for w in writes:
            if w.w is not None and not (w.w[0] == ("e", eng)):
                evs.append(w.w)
            evs.extend(w.pw)
            for k, v in w.r.items():
                if k != ("e", eng):
                    evs.append((k, v))
        need = self._need(eng, evs)
        ins = self._emit(eng, fn, need)
        for w in writes:
            w.pw = []
        if inc:
            self.pcnt[eng] += 1
            ins.then_inc(self.psem[eng], 1)
            self.pend[eng] = False
            val = self.pcnt[eng]
        else:
            self.pend[eng] = True
            val = self.pcnt[eng] + 1
        ev = (("e", eng), val)
        for w in writes:
            w.w = ev
            w.r = {}
        for r in reads:
            if r.r.get(ev[0], 0) < val:
                r.r[ev[0]] = val
        return ins

    def dma(self, q, fn, reads=(), writes=(), serial=False):
        eng = q
        i = self.dnext[q]
        self.dnext[q] = (i + 1) % len(self.dsem[q])
        key = ("d", q, i)
        evs = [(key, self.dval[q][i])] if self.dval[q][i] else []
        for r in reads:
            evs.append(r.w)
            evs.extend(r.pw)
        keep = []
        for w in writes:
            partial = (not serial) and (not w.r) and w.w is not None and w.w[0][0] == "d"
            if partial:
                keep.append((w, w.pw + [w.w]))
            else:
                keep.append((w, []))
                evs.append(w.w)
                evs.extend(w.pw)
                for k, v in w.r.items():
                    evs.append((k, v))
        need = self._need(eng, evs)
        ins = self._emit(eng, fn, need)
        self.dval[q][i] += 16
        ins.then_inc(self.dsem[q][i], 16)
        ev = (key, self.dval[q][i])
        for w, pw in keep:
            w.pw = pw
            w.w = ev
            w.r = {}
        for r in reads:
            if r.r.get(key, 0) < ev[1]:
                r.r[key] = ev[1]
        return ins

    def flush(self, eng):
        if self.pend[eng]:
            self.pcnt[eng] += 1
            self.e[eng].nop().then_inc(self.psem[eng], 1)
            self.pend[eng] = False
            self.ninst += 1

    def barrier(self):
        for k in self.ENG:
            self.flush(k)
        sp = self.e["sp"]
        for k in self.ENG:
            if k != "sp" and self.seen["sp"].get(("e", k), 0) < self.pcnt[k]:
                sp.wait_ge(self.psem[k], self.pcnt[k])
                self.ninst += 1
        for q in self.dsem:
            for i, s in enumerate(self.dsem[q]):
                v = self.dval[q][i]
                if v and self.seen["sp"].get(("d", q, i), 0) < v:
                    sp.wait_ge(s, v)
                    self.ninst += 1
        self.bcnt += 1
        sp.nop().then_inc(self.bsem, 1)
        self.ninst += 1
        for k in self.ENG:
            if k != "sp":
                self.e[k].wait_ge(self.bsem, self.bcnt)
                self.ninst += 1
        for k in self.ENG:
            for k2 in self.ENG:
                self.seen[k][("e", k2)] = self.pcnt[k2]
            for q in self.dsem:
                for i in range(len(self.dsem[q])):
                    self.seen[k][("d", q, i)] = self.dval[q][i]


class Tile:
    def __init__(self, t, name):
        self.t = t
        self.res = Res(name)

    def __getitem__(self, k):
        return self.t[k]


class Ctx:
    pass


class _Inputs:
    def __init__(self, nc, depth):
        self._nc = nc
        self._spec = {
            "x": ([SEQ, D], F32), "ctx": ([CTX, D], F32), "cvec": ([2, D], F32),
            "w_ada": ([depth, D, 6 * D], F32), "b_ada": ([depth, 6 * D], F32),
            "norm_mix": ([depth, D], F32), "norm_ffn": ([depth, D], F32),
            "w_in": ([depth, D, PROJ], F32), "w_out": ([depth, D, D], F32),
            "ret_log_decay": ([depth, 16], F32), "ret_norm": ([depth, 1024], F32),
            "diff_lambda": ([depth, 256], F32), "diff_norm": ([depth, 1024], F32),
            "w_router": ([depth, D, NE], F32), "w_gate": ([depth, NE, D, FF], F32),
            "w_up": ([depth, NE, D, FF], F32), "w_down": ([depth, NE, FF, D], F32),
            "norm_final": ([1, D], F32),
            "k_ident": ([128, 128], BF16), "k_identf": ([128, 128], F32),
            "k_rope": ([SEQ, 128], F32), "k_iota": ([128, NSLOT], F32),
            "k_tokid": ([128, NCH], F32), "k_strip": ([128, 2, SW], F32),
        }
        self.used = {}

    def __getattr__(self, name):
        if name.startswith("_") or name == "used":
            raise AttributeError(name)
        if name not in self.used:
            shape, dt = self._spec[name]
            self.used[name] = self._nc.dram_tensor(name, list(shape), dt, kind="ExternalInput").ap()
        return self.used[name]


def _mk_inputs_decl(nc, depth):
    return _Inputs(nc, depth)


def host_consts():
    k = {}
    k["k_ident"] = np.eye(128, dtype=np.float32).astype(ml_dtypes.bfloat16)
    k["k_identf"] = np.eye(128, dtype=np.float32)
    n_freq = 16
    freqs = (10000.0 ** (-np.arange(n_freq, dtype=np.float32) / n_freq)).astype(np.float32)
    row = np.repeat(np.arange(SEQ // 64, dtype=np.float32), 64)
    col = np.tile(np.arange(64, dtype=np.float32), SEQ // 64)
    ang = np.concatenate([row[:, None] * freqs, col[:, None] * freqs], axis=-1).astype(np.float32)
    cs, sn = np.cos(ang).astype(np.float32), np.sin(ang).astype(np.float32)
    k["k_rope"] = np.concatenate([cs * 0.125, sn * 0.125, cs, sn], axis=1).astype(np.float32)
    k["k_iota"] = np.tile(np.arange(NSLOT, dtype=np.float32)[None, :], (128, 1))
    k["k_tokid"] = (np.arange(NCH, dtype=np.float32)[None, :] * 128 + np.arange(128, dtype=np.float32)[:, None])
    s = np.arange(SW, dtype=np.float32)[None, :] - np.arange(128, dtype=np.float32)[:, None] - 2176.0
    k["k_strip"] = np.stack([np.maximum(s, 0), np.maximum(-s, 0)], axis=1).astype(np.float32)
    return k


def build(depth=DEPTH, phases=None, debug=False, final=True, l0=0):
    nc = bass.Bass("TRN2", target_bir_lowering=False)
    P = Prog(nc)
    g = _mk_inputs_decl(nc, depth)
    okind = "ExternalOutput"
    out = nc.dram_tensor("out", [SEQ, D], F32, kind=okind).ap()
    skind = "ExternalOutput" if debug else "Internal"
    dram = lambda name, shape, dt: nc.dram_tensor(name, list(shape), dt, kind=skind).ap()
    XS = dram("XS", [T, D], F32)
    MODB = [dram("MOD0", [2, 6 * D], F32), dram("MOD1", [2, 6 * D], F32)]
    QT = dram("QT", [24, 128, T], BF16)
    VD = dram("VD", [T, 16, 130], BF16)
    GD = dram("GD", [T, 1024], F32)
    MG = dram("MG", [T, D], BF16)
    HT2 = dram("HT2", [T, D], BF16)
    rXS, rQT, rVD, rGD, rMG, rHT2 = (Res(n) for n in ("XS", "QT", "VD", "GD", "MG", "HT2"))
    rMODB = [Res("MOD0"), Res("MOD1")]
    rIN = Res("inputs")

    def want(ph):
        return phases is None or ph in phases

    def sb(name, shape, dt):
        return Tile(nc.alloc_sbuf_tensor(name, list(shape), dt), name)

    ident = sb("ident", [128, 128], BF16)
    identf = sb("identf", [128, 128], F32)
    SC = sb("silu_c", [128, 16, 2], BF16)
    AFF = sb("AFF", [128, NCH, NE], F32)
    IDXU = sb("IDXU", [128, NE, 3], U32)
    GATE = sb("GATE", [128, NE, 3], F32)
    banks = [Tile(nc.alloc_psum_tensor(f"bank{i}", [128, 512], F32), f"bank{i}") for i in range(8)]

    def bank_bf(i):
        return banks[i].t[:].bitcast(BF16)

    P.dma("sp", lambda: nc.sync.dma_start(out=ident[:], in_=g.k_ident[:, :]), reads=[rIN], writes=[ident.res])
    P.dma("sp", lambda: nc.sync.dma_start(out=identf[:], in_=g.k_identf[:, :]), reads=[rIN], writes=[identf.res])
    P.dma("sp", lambda: nc.sync.dma_start(out=XS[0:CTX, :], in_=g.ctx[:, :]), reads=[rIN], writes=[rXS])
    P.dma("sp", lambda: nc.sync.dma_start(out=XS[CTX:T, :], in_=g.x[:, :]), reads=[rIN], writes=[rXS])
    with SBT(nc, "c_raw", [128, 16, 2], F32) as craw_t:
        craw = Tile(craw_t, "craw")
        for r in range(2):
            P.dma("sp", lambda r=r: nc.sync.dma_start(
                out=craw[:, :, r], in_=g.cvec[r, :].rearrange("(c p) -> p c", p=128),
                allow_slow_non_contiguous=True), reads=[rIN], writes=[craw.res])
        P.op("act", lambda: nc.scalar.activation(out=SC[:], in_=craw[:], func=AF.Silu),
             reads=[craw.res], writes=[SC.res])
        P.barrier()

    for L in range(depth):
        Lg = l0 + L
        last = (Lg == DEPTH - 1) and final
        tc0 = 2 if last else 0
        lam_init = 0.8 - 0.6 * float(np.exp(-0.3 * Lg))

        MOD, rMOD = MODB[L % 2], rMODB[L % 2]

        def mod_gen(Lw, ws, mb, mo, pbs):
            dstM, rdst = MODB[Lw % 2], rMODB[Lw % 2]
            for nb in range(24):
                w = ws[nb % 2]
                b_, o_ = mb[nb % 2], mo[nb % 2]
                srcw = g.w_ada[Lw][:, nb * 512:(nb + 1) * 512].rearrange("(c p) n -> p c n", p=128)
                for hh in range(2):
                    P.dma("pool", lambda w=w, srcw=srcw, hh=hh: nc.gpsimd.dma_start(
                        out=w.t[:, hh * 8:(hh + 1) * 8, :], in_=srcw[:, hh * 8:(hh + 1) * 8, :]),
                        reads=[rIN], writes=[w.res])
                P.dma("sp", lambda b_=b_, nb=nb: nc.sync.dma_start(
                    out=b_.t, in_=g.b_ada[Lw:Lw + 1, nb * 512:(nb + 1) * 512].to_broadcast([2, 512])),
                    reads=[rIN], writes=[b_.res])
                pb = pbs[nb % len(pbs)]
                for dc in range(16):
                    P.op("pe", lambda w=w, dc=dc, pb=pb: nc.tensor.matmul(
                        pb.t[0:2, :], lhsT=SC[:, dc, :], rhs=w.t[:, dc, :], start=(dc == 0), stop=(dc == 15)),
                        reads=[SC.res, w.res], writes=[pb.res], inc=(dc == 15))
                P.op("act", lambda o_=o_, pb=pb: nc.scalar.copy(out=o_.t, in_=pb.t[0:2, :]), reads=[pb.res], writes=[o_.res])
                P.op("pool", lambda o_=o_, b_=b_: nc.gpsimd.tensor_tensor(out=o_.t, in0=o_.t, in1=b_.t, op=ALU.add),
                     reads=[o_.res, b_.res], writes=[o_.res])
                P.dma("sp", lambda o_=o_, nb=nb: nc.sync.dma_start(
                    out=dstM[:, nb * 512:(nb + 1) * 512], in_=o_.t), reads=[o_.res], writes=[rdst])
                yield nb

        def mod_tiles(w0, w1, mb_t, mo_t):
            ws = [Tile(w0, "mw0"), Tile(w1, "mw1")]
            mb = [Tile(mb_t[:, i, :], f"mb{i}") for i in range(2)]
            mo = [Tile(mo_t[:, i, :], f"mo{i}") for i in range(2)]
            return ws, mb, mo

        if want("mod") and (L == 0 or not want("topk")):
            with (SBT(nc, "mw0", [128, 16, 512], BF16) as w0, SBT(nc, "mw1", [128, 16, 512], BF16) as w1,
                  SBT(nc, "mb", [2, 2, 512], F32) as mb_t, SBT(nc, "mo", [2, 2, 512], F32) as mo_t):
                ws, mb, mo = mod_tiles(w0, w1, mb_t, mo_t)
                for _ in mod_gen(L, ws, mb, mo, [banks[0], banks[1]]):
                    pass
                P.barrier()

        def load_bc(tile, src_row):
            P.dma("sp", lambda: nc.sync.dma_start(out=tile[:], in_=src_row.to_broadcast([128, D])),
                  reads=[rMOD, rIN], writes=[tile.res])

        def norm_phase(which, hT, store_ht2, router):
            gain = (g.norm_mix if which == 0 else g.norm_ffn)[L:L + 1, :]
            s_shift, s_scale = (0, 1) if which == 0 else (3, 4)
            with (SBT(nc, "nA", [128, 2, D], F32) as nA_t, SBT(nc, "nB", [128, 2, D], F32) as nB_t,
                  SBT(nc, "nG", [128, D], F32) as nG_t, SBT(nc, "nX", [128, 2, D], F32) as nX_t,
                  SBT(nc, "nT", [128, 2, D], F32) as nT_t, SBT(nc, "nH", [128, 2, D], BF16) as nH_t,
                  SBT(nc, "nJ", [128, D], BF16) as nJ_t, SBT(nc, "nS", [128, 2, 4], F32) as nS_t,
                  SBT(nc, "nHT", [128, 2, 16, 128], BF16) as nHT_t, SBT(nc, "nWr", [128, 16, NE], BF16) as nWr_t,
                  SBT(nc, "nLg", [128, 2, 40], F32) as nLg_t):
                A = [Tile(nA_t[:, i, :], f"nA{i}") for i in range(2)]
                B = [Tile(nB_t[:, i, :], f"nB{i}") for i in range(2)]
                G = Tile(nG_t, "nG")
                X = [Tile(nX_t[:, i, :], f"nX{i}") for i in range(2)]
                TT = [Tile(nT_t[:, i, :], f"nT{i}") for i in range(2)]
                H = [Tile(nH_t[:, i, :], f"nH{i}") for i in range(2)]
                J = Tile(nJ_t, "nJ")
                S = [Tile(nS_t[:, i, :], f"nS{i}") for i in range(2)]
                HTc = [Tile(nHT_t[:, i], f"nHT{i}") for i in range(2)]
                Wr = Tile(nWr_t, "nWr")
                LG = [Tile(nLg_t[:, i, :], f"nLg{i}") for i in range(2)]
                load_bc(G, gain)
                for i, row in ((1, 0), (0, 1)):
                    load_bc(A[i], MOD[row:row + 1, s_scale * D:(s_scale + 1) * D])
                    load_bc(B[i], MOD[row:row + 1, s_shift * D:(s_shift + 1) * D])
                    P.op("dve", lambda i=i: nc.vector.scalar_tensor_tensor(
                        out=A[i].t, in0=A[i].t, scalar=1.0, in1=G[:], op0=ALU.add, op1=ALU.mult),
                        reads=[A[i].res, G.res], writes=[A[i].res])
                if router:
                    P.dma("pool", lambda: nc.gpsimd.dma_start(
                        out=Wr[:], in_=g.w_router[L].rearrange("(c p) e -> p c e", p=128)),
                        reads=[rIN], writes=[Wr.res])
                for tc in range(NCH):
                    if router and last and tc < 2:
                        continue
                    k = tc % 2
                    isx = 1 if tc >= 2 else 0
                    x_, t_, h_, s_ = X[k], TT[k], H[k], S[k]
                    P.dma("sp", lambda x_=x_, tc=tc: nc.sync.dma_start(out=x_.t, in_=XS[tc * 128:(tc + 1) * 128, :]),
                          reads=[rXS], writes=[x_.res])
                    P.op("act", lambda x_=x_, s_=s_: nc.scalar.activation(
                        out=J[:], in_=x_.t, func=AF.Square, accum_out=s_.t[:, 0:1]),
                        reads=[x_.res], writes=[J.res, s_.res])
                    P.op("dve", lambda s_=s_: nc.vector.tensor_scalar(
                        out=s_.t[:, 1:2], in0=s_.t[:, 0:1], scalar1=1.0 / D, scalar2=EPS, op0=ALU.mult, op1=ALU.add),
                        reads=[s_.res], writes=[s_.res])
                    P.op("act", lambda s_=s_: nc.scalar.activation(out=s_.t[:, 2:3], in_=s_.t[:, 1:2], func=AF.Sqrt),
                         reads=[s_.res], writes=[s_.res])
                    P.op("dve", lambda s_=s_: nc.vector.reciprocal(out=s_.t[:, 3:4], in_=s_.t[:, 2:3]),
                         reads=[s_.res], writes=[s_.res])
                    P.op("dve", lambda x_=x_, t_=t_, s_=s_, isx=isx: nc.vector.scalar_tensor_tensor(
                        out=t_.t, in0=x_.t, scalar=s_.t[:, 3:4], in1=A[isx].t, op0=ALU.mult, op1=ALU.mult),
                        reads=[x_.res, s_.res, A[isx].res], writes=[t_.res])
                    P.op("pool", lambda t_=t_, h_=h_, isx=isx: nc.gpsimd.tensor_tensor(
                        out=h_.t, in0=t_.t, in1=B[isx].t, op=ALU.add),
                        reads=[t_.res, B[isx].res], writes=[h_.res])
                    if store_ht2:
                        P.dma("sp", lambda h_=h_, tc=tc: nc.sync.dma_start(out=HT2[tc * 128:(tc + 1) * 128, :], in_=h_.t),
                              reads=[h_.res], writes=[rHT2])
                    for half in range(2):
                        pb = banks[2 + (tc % 2) * 2 + half]
                        pbv = bank_bf(2 + (tc % 2) * 2 + half)
                        for j in range(8):
                            dc = half * 8 + j
                            P.op("pe", lambda h_=h_, dc=dc, j=j, pbv=pbv: nc.tensor.transpose(
                                out=pbv[:, j * 128:(j + 1) * 128], in_=h_.t[:, dc * 128:(dc + 1) * 128], identity=ident[:]),
                                reads=[h_.res, ident.res], writes=[pb.res], inc=(j == 7))
                        if hT is not None:
                            dst = hT.t[:, half * 8:(half + 1) * 8, tc * 128:(tc + 1) * 128]
                            dres = hT.res
                        else:
                            dst = HTc[k].t[:, half * 8:(half + 1) * 8, :]
                            dres = HTc[k].res
                        ev_eng = "act" if half == 0 else "dve"
                        if ev_eng == "act":
                            P.op("act", lambda dst=dst, pbv=pbv: nc.scalar.copy(
                                out=dst, in_=pbv.rearrange("p (j t) -> p j t", j=8)),
                                reads=[pb.res], writes=[dres])
                        else:
                            P.op("dve", lambda dst=dst, pbv=pbv: nc.vector.tensor_copy(
                                out=dst, in_=pbv.rearrange("p (j t) -> p j t", j=8)),
                                reads=[pb.res], writes=[dres])
                    if router:
                        pl = banks[6 + tc % 2]
                        lg = LG[k]
                        for dc in range(16):
                            P.op("pe", lambda dc=dc, pl=pl, k=k: nc.tensor.matmul(
                                pl.t[:, 0:NE], lhsT=HTc[k].t[:, dc, :], rhs=Wr[:, dc, :], start=(dc == 0), stop=(dc == 15)),
                                reads=[HTc[k].res, Wr.res], writes=[pl.res], inc=(dc == 15))
                        P.op("dve", lambda pl=pl, lg=lg: nc.vector.tensor_reduce(
                            out=lg.t[:, 32:33], in_=pl.t[:, 0:NE], axis=AX.X, op=ALU.max, negate=True),
                            reads=[pl.res], writes=[lg.res])
                        P.op("act", lambda pl=pl, lg=lg: nc.scalar.activation(
                            out=lg.t[:, 0:NE], in_=pl.t[:, 0:NE], func=AF.Exp, bias=lg.t[:, 32:33], scale=1.0,
                            accum_out=lg.t[:, 33:34]), reads=[pl.res, lg.res], writes=[lg.res])
                        P.op("dve", lambda lg=lg: nc.vector.reciprocal(out=lg.t[:, 34:35], in_=lg.t[:, 33:34]),
                             reads=[lg.res], writes=[lg.res])
                        P.op("dve", lambda lg=lg, tc=tc: nc.vector.tensor_scalar(
                            out=AFF[:, tc, :], in0=lg.t[:, 0:NE], scalar1=lg.t[:, 34:35], scalar2=None, op0=ALU.mult),
                            reads=[lg.res], writes=[AFF.res])
                P.barrier()

        if want("inproj"):
            with SBT(nc, "hT", [128, 16, T], BF16) as hT_t:
                hT = Tile(hT_t, "hT")
                norm_phase(0, hT, False, False)
                with (SBT(nc, "iw", [128, 2, 16, 512], BF16) as iw_t, SBT(nc, "rope", [128, 16, 128], F32) as rope_t,
                      SBT(nc, "qk", [128, 2, 512], BF16) as qk_t, SBT(nc, "rt", [128, 2, 4, 256], F32) as rt_t,
                      SBT(nc, "qts", [128, 4, T], BF16) as qts_t, SBT(nc, "va", [128, 2, 4, 130], BF16) as va_t,
                      SBT(nc, "gs", [128, 2, 512], F32) as gs_t):
                    IW = [Tile(iw_t[:, i], f"iw{i}") for i in range(2)]
                    ROPE = Tile(rope_t, "rope")
                    QK = [Tile(qk_t[:, i, :], f"qk{i}") for i in range(2)]
                    RT = [Tile(rt_t[:, i], f"rt{i}") for i in range(2)]
                    QTS = Tile(qts_t, "qts")
                    VA = [Tile(va_t[:, i], f"va{i}") for i in range(2)]
                    GS = [Tile(gs_t[:, i, :], f"gs{i}") for i in range(2)]
                    P.dma("sp", lambda: nc.sync.dma_start(out=ROPE[:], in_=g.k_rope.rearrange("(c p) f -> p c f", p=128)),
                          reads=[rIN], writes=[ROPE.res])
                    for i in range(2):
                        P.op("pool", lambda i=i: nc.gpsimd.memset(VA[i].t[:, :, 128:130], 1.0), writes=[VA[i].res])
                    kinds = ["q", "k", "v", "v", "g", "g", "q", "q", "k", "k", "v", "v"]
                    qt_base = {0: 0, 1: 4, 6: 8, 7: 12, 8: 16, 9: 20}
                    v_base = {2: 0, 3: 4, 10: 8, 11: 12}
                    cnt = 0
                    pending = []

                    def flush_pending():
                        while pending:
                            pending.pop(0)()

                    for cb in range(12):
                        W = IW[cb % 2]
                        src = g.w_in[L][:, cb * 512:(cb + 1) * 512].rearrange("(c p) n -> p c n", p=128)
                        for hh in range(2):
                            P.dma("pool", lambda W=W, src=src, hh=hh: nc.gpsimd.dma_start(
                                out=W.t[:, hh * 8:(hh + 1) * 8, :], in_=src[:, hh * 8:(hh + 1) * 8, :]),
                                reads=[rIN], writes=[W.res])
                        kind = kinds[cb]
                        for tc in range(NCH):
                            pb = banks[cnt % 4]
                            k2 = cnt % 2
                            cnt += 1
                            for dc in range(16):
                                P.op("pe", lambda W=W, dc=dc, tc=tc, pb=pb: nc.tensor.matmul(
                                    pb.t[:, :], lhsT=hT.t[:, dc, tc * 128:(tc + 1) * 128], rhs=W.t[:, dc, :],
                                    start=(dc == 0), stop=(dc == 15)),
                                    reads=[hT.res, W.res], writes=[pb.res], inc=(dc == 15))
                            flush_pending()
                            if kind in ("q", "k"):
                                qk = QK[k2]
                                if tc < 2:
                                    if kind == "q":
                                        P.op("act", lambda qk=qk, pb=pb: nc.scalar.mul(qk.t, pb.t[:, :], 0.125),
                                             reads=[pb.res], writes=[qk.res])
                                    else:
                                        P.op("act", lambda qk=qk, pb=pb: nc.scalar.copy(out=qk.t, in_=pb.t[:, :]),
                                             reads=[pb.res], writes=[qk.res])
                                else:
                                    rt = RT[k2]
                                    o = 0 if kind == "q" else 64
                                    xc = tc - 2
                                    cosb = ROPE[:, xc:xc + 1, o:o + 32].to_broadcast([128, 8, 32])
                                    sinb = ROPE[:, xc:xc + 1, o + 32:o + 64].to_broadcast([128, 8, 32])
                                    pv = pb.t[:, :].rearrange("p (h two f) -> p h two f", h=8, two=2)
                                    x1, x2 = pv[:, :, 0, :], pv[:, :, 1, :]
                                    rtv = rt.t.rearrange("p a (h f) -> p a h f", h=8)
                                    for a, (xx, cc) in enumerate(((x1, cosb), (x2, sinb), (x1, sinb), (x2, cosb))):
                                        P.op("dve", lambda a=a, xx=xx, cc=cc, rtv=rtv: nc.vector.tensor_tensor(
                                            out=rtv[:, a], in0=xx, in1=cc, op=ALU.mult),
                                            reads=[pb.res, ROPE.res], writes=[rt.res])
                                    qv = qk.t.rearrange("p (h two f) -> p h two f", h=8, two=2)
                                    P.op("pool", lambda qv=qv, rtv=rtv: nc.gpsimd.tensor_tensor(
                                        out=qv[:, :, 0, :], in0=rtv[:, 0], in1=rtv[:, 1], op=ALU.subtract),
                                        reads=[rt.res], writes=[qk.res])
                                    P.op("pool", lambda qv=qv, rtv=rtv: nc.gpsimd.tensor_tensor(
                                        out=qv[:, :, 1, :], in0=rtv[:, 2], in1=rtv[:, 3], op=ALU.add),
                                        reads=[rt.res], writes=[qk.res])

                                def do_tr(qk=qk, k2=k2, tc=tc):
                                    pt = banks[4 + k2]
                                    ptv = bank_bf(4 + k2)
                                    for j in range(4):
                                        P.op("pe", lambda qk=qk, j=j, ptv=ptv: nc.tensor.transpose(
                                            out=ptv[:, j * 128:(j + 1) * 128], in_=qk.t[:, j * 128:(j + 1) * 128], identity=ident[:]),
                                            reads=[qk.res, ident.res], writes=[pt.res], inc=(j == 3))
                                    P.op("act", lambda ptv=ptv, tc=tc: nc.scalar.copy(
                                        out=QTS[:, :, tc * 128:(tc + 1) * 128], in_=ptv[:, 0:512].rearrange("p (j t) -> p j t", j=4)),
                                        reads=[pt.res], writes=[QTS.res])
                                pending.append(do_tr)
                            elif kind == "v":
                                va = VA[k2]
                                P.op("act", lambda va=va, pb=pb: nc.scalar.copy(
                                    out=va.t[:, :, 0:128], in_=pb.t[:, :].rearrange("p (h f) -> p h f", h=4)),
                                    reads=[pb.res], writes=[va.res])
                                hb = v_base[cb]
                                P.dma("sp", lambda va=va, tc=tc, hb=hb: nc.sync.dma_start(
                                    out=VD[tc * 128:(tc + 1) * 128, hb:hb + 4, :], in_=va.t), reads=[va.res], writes=[rVD])
                            else:
                                gs = GS[k2]
                                P.op("act", lambda gs=gs, pb=pb: nc.scalar.activation(out=gs.t, in_=pb.t[:, :], func=AF.Silu),
                                     reads=[pb.res], writes=[gs.res])
                                P.dma("sp", lambda gs=gs, tc=tc, cb=cb: nc.sync.dma_start(
                                    out=GD[tc * 128:(tc + 1) * 128, (cb - 4) * 512:(cb - 3) * 512], in_=gs.t),
                                    reads=[gs.res], writes=[rGD])
                        if kind in ("q", "k"):
                            flush_pending()
                            qb_ = qt_base[cb]
                            P.dma("sp", lambda qb_=qb_: nc.sync.dma_start(
                                out=QT[qb_:qb_ + 4].rearrange("j p t -> p j t"), in_=QTS[:]), reads=[QTS.res], writes=[rQT])
                    P.barrier()

        env = Ctx()
        env.__dict__.update(dict(nc=nc, P=P, g=g, L=L, last=last, tc0=tc0, lam_init=lam_init, banks=banks, bank_bf=bank_bf,
                                 ident=ident, identf=identf, AFF=AFF, IDXU=IDXU, GATE=GATE, XS=XS, MOD=MOD, QT=QT, VD=VD,
                                 GD=GD, MG=MG, HT2=HT2, rXS=rXS, rMOD=rMOD, rQT=rQT, rVD=rVD, rGD=rGD, rMG=rMG,
                                 rHT2=rHT2, rIN=rIN, norm_phase=norm_phase, load_bc=load_bc,
                                 mod_gen=mod_gen, mod_tiles=mod_tiles,
                                 next_mod=(L + 1 if (L + 1 < depth and want('mod')) else None)))
        if want("attn"):
            phase_attn(env)
        if want("outproj"):
            phase_outproj(env)
        if want("norm2"):
            norm_phase(1, None, True, True)
        if want("topk"):
            phase_topk(env)
        if want("moe"):
            phase_moe(env)

    if final:
        with (SBT(nc, "fG", [128, D], F32) as fG_t, SBT(nc, "fX", [128, 2, D], F32) as fX_t,
              SBT(nc, "fJ", [128, D], BF16) as fJ_t, SBT(nc, "fS", [128, 2, 4], F32) as fS_t,
              SBT(nc, "fY", [128, 2, D], F32) as fY_t):
            G = Tile(fG_t, "fG")
            J = Tile(fJ_t, "fJ")
            X = [Tile(fX_t[:, i, :], f"fX{i}") for i in range(2)]
            Y = [Tile(fY_t[:, i, :], f"fY{i}") for i in range(2)]
            S = [Tile(fS_t[:, i, :], f"fS{i}") for i in range(2)]
            rOUT = Res("out")
            P.dma("sp", lambda: nc.sync.dma_start(out=G[:], in_=g.norm_final[0:1, :].to_broadcast([128, D])),
                  reads=[rIN], writes=[G.res])
            for tc in range(2, NCH):
                k = tc % 2
                x_, y_, s_ = X[k], Y[k], S[k]
                P.dma("sp", lambda x_=x_, tc=tc: nc.sync.dma_start(out=x_.t, in_=XS[tc * 128:(tc + 1) * 128, :]),
                      reads=[rXS], writes=[x_.res])
                P.op("act", lambda x_=x_, s_=s_: nc.scalar.activation(
                    out=J[:], in_=x_.t, func=AF.Square, accum_out=s_.t[:, 0:1]), reads=[x_.res], writes=[J.res, s_.res])
                P.op("dve", lambda s_=s_: nc.vector.tensor_scalar(
                    out=s_.t[:, 1:2], in0=s_.t[:, 0:1], scalar1=1.0 / D, scalar2=EPS, op0=ALU.mult, op1=ALU.add),
                    reads=[s_.res], writes=[s_.res])
                P.op("act", lambda s_=s_: nc.scalar.activation(out=s_.t[:, 2:3], in_=s_.t[:, 1:2], func=AF.Sqrt),
                     reads=[s_.res], writes=[s_.res])
                P.op("dve", lambda s_=s_: nc.vector.reciprocal(out=s_.t[:, 3:4], in_=s_.t[:, 2:3]),
                     reads=[s_.res], writes=[s_.res])
                P.op("dve", lambda x_=x_, y_=y_, s_=s_: nc.vector.scalar_tensor_tensor(
                    out=y_.t, in0=x_.t, scalar=s_.t[:, 3:4], in1=G[:], op0=ALU.mult, op1=ALU.mult),
                    reads=[x_.res, s_.res, G.res], writes=[y_.res])
                P.dma("sp", lambda y_=y_, tc=tc: nc.sync.dma_start(out=out[(tc - 2) * 128:(tc - 1) * 128, :], in_=y_.t),
                      reads=[y_.res], writes=[rOUT])
            P.barrier()
    else:
        rOUT = Res("out")
        P.dma("sp", lambda: nc.sync.dma_start(out=out[:, :], in_=XS[CTX:T, :]), reads=[rXS], writes=[rOUT])
        P.barrier()
    nc._prog_ninst = P.ninst
    nc._used_inputs = list(g.used.keys())
    return nc


def phase_attn(E):
    nc, P, g, L = E.nc, E.P, E.g, E.L
    banks = E.banks
    lam_init = E.lam_init
    with (SBT(nc, "aStrip", [128, 2, SW], F32) as strip_t, SBT(nc, "aTh", [128, 2, SW], F32) as th_t,
          SBT(nc, "aT1", [128, SW], F32) as t1_t,
          SBT(nc, "aLg", [128, 48], F32) as lg_t, SBT(nc, "aLv", [128, 256 + 128 + 8], F32) as lv_t,
          SBT(nc, "aRG", [128, 1024], F32) as rg_t, SBT(nc, "aDG", [128, 1024], F32) as dg_t,
          SBT(nc, "aQ", [128, 2, T], BF16) as q_t, SBT(nc, "aK", [128, 2, T], BF16) as k_t,
          SBT(nc, "aV", [128, 4, NCH, 130], BF16) as v_t, SBT(nc, "aE", [128, 6, 512], BF16) as e_t,
          SBT(nc, "aO", [128, 8, 129], F32) as o_t, SBT(nc, "aW", [128, 3, 4, 128], F32) as w_t,
          SBT(nc, "aSt", [128, 64], F32) as st_t, SBT(nc, "aG", [128, 4, 4, 128], F32) as gt_t,
          SBT(nc, "aM", [128, 4, 4, 128], BF16) as m_t):
        STRIP = Tile(strip_t, "strip")
        TH = [Tile(th_t[:, i, :], f"th{i}") for i in range(2)]
        T1 = Tile(t1_t, "t1")
        LG = Tile(lg_t, "lg")
        LV = Tile(lv_t, "lv")
        RG = Tile(rg_t, "rg")
        DG = Tile(dg_t, "dg")
        Q = [Tile(q_t[:, i, :], f"aq{i}") for i in range(2)]
        K = [Tile(k_t[:, i, :], f"ak{i}") for i in range(2)]
        V = [Tile(v_t[:, i], f"av{i}") for i in range(4)]
        EE = [Tile(e_t[:, i, :], f"ae{i}") for i in range(6)]
        O = Tile(o_t, "ao")
        W = [Tile(w_t[:, i], f"aw{i}") for i in range(3)]
        ST = Tile(st_t, "ast")
        GT = [Tile(gt_t[:, i], f"ag{i}") for i in range(4)]
        M = [Tile(m_t[:, i], f"am{i}") for i in range(4)]
        rIN = E.rIN
        P.dma("sp", lambda: nc.sync.dma_start(out=STRIP[:], in_=g.k_strip[:, :, :]), reads=[rIN], writes=[STRIP.res])
        P.dma("sp", lambda: nc.sync.dma_start(out=LG[:, 0:16], in_=g.ret_log_decay[L:L + 1, :].to_broadcast([128, 16])),
              reads=[rIN], writes=[LG.res])
        P.op("act", lambda: nc.scalar.activation(out=LG[:, 16:32], in_=LG[:, 0:16], func=AF.Exp), reads=[LG.res], writes=[LG.res])
        P.op("act", lambda: nc.scalar.activation(out=LG[:, 32:48], in_=LG[:, 16:32], func=AF.Ln, scale=-1.0, bias=1.0),
             reads=[LG.res], writes=[LG.res])
        P.dma("sp", lambda: nc.sync.dma_start(out=LV[:, 0:256], in_=g.diff_lambda[L:L + 1, :].to_broadcast([128, 256])),
              reads=[rIN], writes=[LV.res])
        lvv = LV[:, 0:256].rearrange("p (a f) -> p a f", a=4)
        prod = LV[:, 256:384].rearrange("p (a f) -> p a f", a=2)
        P.op("dve", lambda: nc.vector.tensor_tensor(out=prod[:, 0, :], in0=lvv[:, 0, :], in1=lvv[:, 1, :], op=ALU.mult),
             reads=[LV.res], writes=[LV.res])
        P.op("dve", lambda: nc.vector.tensor_tensor(out=prod[:, 1, :], in0=lvv[:, 2, :], in1=lvv[:, 3, :], op=ALU.mult),
             reads=[LV.res], writes=[LV.res])
        P.op("dve", lambda: nc.vector.tensor_reduce(out=LV[:, 384:386], in_=prod, axis=AX.X, op=ALU.add),
             reads=[LV.res], writes=[LV.res])
        P.op("act", lambda: nc.scalar.activation(out=LV[:, 386:388], in_=LV[:, 384:386], func=AF.Exp), reads=[LV.res], writes=[LV.res])
        P.op("dve", lambda: nc.vector.tensor_tensor(out=LV[:, 388:389], in0=LV[:, 387:388], in1=LV[:, 386:387], op=ALU.subtract),
             reads=[LV.res], writes=[LV.res])
        P.op("dve", lambda: nc.vector.tensor_scalar(out=LV[:, 389:390], in0=LV[:, 388:389], scalar1=-lam_init, scalar2=None, op0=ALU.add),
             reads=[LV.res], writes=[LV.res])
        NLAM = LV[:, 389:390]
        P.dma("sp", lambda: nc.sync.dma_start(out=RG[:], in_=g.ret_norm[L:L + 1, :].to_broadcast([128, 1024])), reads=[rIN], writes=[RG.res])
        P.dma("sp", lambda: nc.sync.dma_start(out=DG[:], in_=g.diff_norm[L:L + 1, :].to_broadcast([128, 1024])), reads=[rIN], writes=[DG.res])
        P.op("dve", lambda: nc.vector.tensor_scalar(out=DG[:], in0=DG[:], scalar1=1.0 - lam_init, scalar2=None, op0=ALU.mult),
             reads=[DG.res], writes=[DG.res])

        P.op("pool", lambda: nc.gpsimd.memset(ST[:, 56:64], -0.5), writes=[ST.res])
        ecnt = [0]
        scnt = [0]

        def small_rstd(nqc, var_lo, out_lo):
            P.op("pool", lambda: nc.gpsimd.tensor_tensor(out=ST[:, out_lo:out_lo + nqc], in0=ST[:, var_lo:var_lo + nqc],
                                                         in1=ST[:, 56:56 + nqc], op=ALU.pow), reads=[ST.res], writes=[ST.res])

        def store_mg(mt, nqc, col0, row0):
            P.dma("sp", lambda: nc.sync.dma_start(
                out=E.MG[row0:row0 + nqc * 128, col0:col0 + 128].rearrange("(c p) f -> p c f", p=128), in_=mt.t[:, 0:nqc, :]),
                reads=[mt.res], writes=[E.rMG])

        def evac_ret(ab, nqc, h, row0, gt, mt):
            W0, W1, W2 = W
            for qc in range(nqc):
                P.op("act", lambda qc=qc: nc.scalar.activation(out=W1.t[:, qc, :], in_=ab.t[:, qc * 128:(qc + 1) * 128], func=AF.Copy,
                                                               accum_out=ST[:, qc:qc + 1]), reads=[ab.res], writes=[W1.res, ST.res])
                P.op("act", lambda qc=qc: nc.scalar.activation(out=W2.t[:, qc, :], in_=ab.t[:, qc * 128:(qc + 1) * 128], func=AF.Square,
                                                               accum_out=ST[:, 8 + qc:9 + qc]), reads=[ab.res], writes=[W2.res, ST.res])
            P.op("pool", lambda: nc.gpsimd.tensor_scalar(out=ST[:, 16:16 + nqc], in0=ST[:, 0:nqc], scalar1=1.0 / 128, scalar2=0.0, op0=ALU.mult, op1=ALU.add),
                 reads=[ST.res], writes=[ST.res])
            P.op("pool", lambda: nc.gpsimd.tensor_tensor(out=ST[:, 24:24 + nqc], in0=ST[:, 16:16 + nqc], in1=ST[:, 16:16 + nqc], op=ALU.mult),
                 reads=[ST.res], writes=[ST.res])
            P.op("pool", lambda: nc.gpsimd.tensor_scalar(out=ST[:, 8:8 + nqc], in0=ST[:, 8:8 + nqc], scalar1=1.0 / 128, scalar2=EPS, op0=ALU.mult, op1=ALU.add),
                 reads=[ST.res], writes=[ST.res])
            P.op("pool", lambda: nc.gpsimd.tensor_tensor(out=ST[:, 24:24 + nqc], in0=ST[:, 8:8 + nqc], in1=ST[:, 24:24 + nqc], op=ALU.subtract),
                 reads=[ST.res], writes=[ST.res])
            small_rstd(nqc, 24, 40)
            P.op("pool", lambda: nc.gpsimd.tensor_tensor(out=ST[:, 48:48 + nqc], in0=ST[:, 16:16 + nqc], in1=ST[:, 40:40 + nqc], op=ALU.mult),
                 reads=[ST.res], writes=[ST.res])
            P.op("pool", lambda: nc.gpsimd.tensor_scalar(out=ST[:, 48:48 + nqc], in0=ST[:, 48:48 + nqc], scalar1=-1.0, scalar2=0.0, op0=ALU.mult, op1=ALU.add),
                 reads=[ST.res], writes=[ST.res])
            for qc in range(nqc):
                P.op("act", lambda qc=qc: nc.scalar.activation(out=W0.t[:, qc, :], in_=ab.t[:, qc * 128:(qc + 1) * 128], func=AF.Identity,
                                                               scale=ST[:, 40 + qc:41 + qc], bias=ST[:, 48 + qc:49 + qc]),
                     reads=[ab.res, ST.res], writes=[W0.res])
            gv = RG[:, h * 128:(h + 1) * 128].unsqueeze(1).to_broadcast([128, nqc, 128])
            P.op("pool", lambda: nc.gpsimd.tensor_tensor(out=W1.t[:, 0:nqc, :], in0=W0.t[:, 0:nqc, :], in1=gv, op=ALU.mult),
                 reads=[W0.res, RG.res], writes=[W1.res])
            P.op("pool", lambda: nc.gpsimd.tensor_tensor(out=mt.t[:, 0:nqc, :], in0=W1.t[:, 0:nqc, :], in1=gt.t[:, 0:nqc, :], op=ALU.mult),
                 reads=[W1.res, gt.res], writes=[mt.res])
            store_mg(mt, nqc, h * 128, row0)

        def evac_diff(nqc, h, row0, mt):
            W0, W1, W2 = W
            na = 2 * nqc
            for b3 in range((na + 2) // 3):
                n_in = min(3, na - b3 * 3)
                P.op("dve", lambda b3=b3, n_in=n_in: nc.vector.tensor_copy(
                    out=O[:, b3 * 3:b3 * 3 + n_in, :], in_=banks[4 + b3].t[:, 0:n_in * 129].rearrange("p (c f) -> p c f", c=n_in)),
                    reads=[banks[4 + b3].res], writes=[O.res])
            P.op("dve", lambda: nc.vector.reciprocal(out=ST[:, 48:48 + na], in_=O[:, 0:na, 128]), reads=[O.res], writes=[ST.res])
            P.op("dve", lambda: nc.vector.tensor_scalar(out=ST[:, 48 + nqc:48 + na], in0=ST[:, 48 + nqc:48 + na], scalar1=NLAM, scalar2=None,
                                                        op0=ALU.mult), reads=[ST.res, LV.res], writes=[ST.res])
            P.op("dve", lambda: nc.vector.tensor_tensor(
                out=W1.t[:, 0:nqc, :], in0=O[:, 0:nqc, 0:128], in1=ST[:, 48:48 + nqc].unsqueeze(2).to_broadcast([128, nqc, 128]), op=ALU.mult),
                reads=[O.res, ST.res], writes=[W1.res])
            P.op("dve", lambda: nc.vector.tensor_tensor(
                out=W2.t[:, 0:nqc, :], in0=O[:, nqc:na, 0:128], in1=ST[:, 48 + nqc:48 + na].unsqueeze(2).to_broadcast([128, nqc, 128]), op=ALU.mult),
                reads=[O.res, ST.res], writes=[W2.res])
            P.op("pool", lambda: nc.gpsimd.tensor_tensor(out=W0.t[:, 0:nqc, :], in0=W1.t[:, 0:nqc, :], in1=W2.t[:, 0:nqc, :], op=ALU.add),
                 reads=[W1.res, W2.res], writes=[W0.res])
            P.op("pool", lambda: nc.gpsimd.tensor_tensor(out=W2.t[:, 0:nqc, :], in0=W0.t[:, 0:nqc, :], in1=W0.t[:, 0:nqc, :], op=ALU.mult),
                 reads=[W0.res], writes=[W2.res])
            P.op("dve", lambda: nc.vector.tensor_reduce(out=ST[:, 16:16 + nqc], in_=W2.t[:, 0:nqc, :], axis=AX.X, op=ALU.add),
                 reads=[W2.res], writes=[ST.res])
            P.op("dve", lambda: nc.vector.tensor_scalar(out=ST[:, 24:24 + nqc], in0=ST[:, 16:16 + nqc], scalar1=1.0 / 128, scalar2=EPS,
                                                        op0=ALU.mult, op1=ALU.add), reads=[ST.res], writes=[ST.res])
            small_rstd(nqc, 24, 40)
            P.op("dve", lambda: nc.vector.tensor_tensor(
                out=W1.t[:, 0:nqc, :], in0=W0.t[:, 0:nqc, :], in1=ST[:, 40:40 + nqc].unsqueeze(2).to_broadcast([128, nqc, 128]), op=ALU.mult),
                reads=[W0.res, ST.res], writes=[W1.res])
            gv = DG[:, h * 128:(h + 1) * 128].unsqueeze(1).to_broadcast([128, nqc, 128])
            P.op("pool", lambda: nc.gpsimd.tensor_tensor(out=mt.t[:, 0:nqc, :], in0=W1.t[:, 0:nqc, :], in1=gv, op=ALU.mult),
                 reads=[W1.res, DG.res], writes=[mt.res])
            store_mg(mt, nqc, 1024 + h * 128, row0)

        rcnt = [0]
        units = [("ret", c) for c in range(4)] + [("diff", h) for h in range(8)]
        for ui, (ukind, uidx) in enumerate(units):
            is_ret = ukind == "ret"
            k2 = ui % 2
            q_, k_ = Q[k2], K[k2]
            if is_ret:
                heads = [2 * uidx, 2 * uidx + 1]
                qsrc, ksrc = E.QT[uidx, :, :], E.QT[4 + uidx, :, :]
                vhead = [heads[0], heads[1]]
            else:
                heads = [uidx, uidx]
                qsrc, ksrc = E.QT[8 + uidx, :, :], E.QT[16 + uidx, :, :]
                vhead = [8 + uidx]
            P.dma("sp", lambda: nc.sync.dma_start(out=q_.t[:, :], in_=qsrc), reads=[E.rQT], writes=[q_.res])
            P.dma("sp", lambda: nc.sync.dma_start(out=k_.t[:, :], in_=ksrc), reads=[E.rQT], writes=[k_.res])
            vs = []
            for vi, vh in enumerate(vhead):
                v_ = V[k2 * 2 + vi]
                P.dma("sp", lambda v_=v_, vh=vh: nc.sync.dma_start(out=v_.t, in_=E.VD[:, vh, :].rearrange("(c p) e -> p c e", p=128)),
                      reads=[E.rVD], writes=[v_.res])
                vs.append(v_)
            if not is_ret:
                vs.append(vs[0])
            if is_ret:
                for pr in range(2):
                    h = heads[pr]
                    th = TH[pr]
                    P.op("dve", lambda h=h: nc.vector.tensor_scalar(out=T1[:], in0=STRIP[:, 0, :], scalar1=LG[:, 32 + h:33 + h], scalar2=None, op0=ALU.mult),
                         reads=[STRIP.res, LG.res], writes=[T1.res])
                    P.op("dve", lambda h=h: nc.vector.scalar_tensor_tensor(out=T1[:], in0=STRIP[:, 1, :], scalar=LG[:, 40 + h:41 + h], in1=T1[:],
                                                                          op0=ALU.mult, op1=ALU.add), reads=[STRIP.res, LG.res, T1.res], writes=[T1.res])
                    P.op("act", lambda th=th: nc.scalar.activation(out=th.t, in_=T1[:], func=AF.Exp), reads=[T1.res], writes=[th.res])
            groups = []
            if not E.last:
                ent = [(0, 0), (1, 128)]
                groups.append((0, 0, 256, ent, 0))
            for qb in range(4):
                if is_ret:
                    ent = [(0, -256), (1, -128)] + [(2 + j, j * 128) for j in range(16)] + [(0, 2048), (1, 2176)]
                else:
                    ent = [(j, 0) for j in range(NCH)]
                groups.append((CTX + qb * 512, CTX + qb * 512, 512, ent, qb * 512))
            for gi, (row0, qcol0, nq, ent, qpos0) in enumerate(groups):
                nqc = nq // 128
                gts = [GT[(gi % 2) * 2 + pr] for pr in range(2)]
                mts = [M[(gi % 2) * 2 + pr] for pr in range(2)]
                if is_ret:
                    for pr in range(2):
                        h = heads[pr]
                        P.dma("sp", lambda pr=pr, h=h: nc.sync.dma_start(
                            out=gts[pr].t[:, 0:nqc, :], in_=E.GD[row0:row0 + nq, h * 128:(h + 1) * 128].rearrange("(c p) f -> p c f", p=128)),
                            reads=[E.rGD], writes=[gts[pr].res])
                    rbank = [banks[4 + (rcnt[0] % 2) * 2 + pr] for pr in range(2)]
                    rcnt[0] += 1

                def emit_S(ei):
                    kc, pos = ent[ei]
                    outs = []
                    for pr in range(2):
                        bi = scnt[0] % 4
                        scnt[0] += 1
                        sb_ = banks[bi]
                        lhs = k_.t[pr * 64:(pr + 1) * 64, kc * 128:(kc + 1) * 128]
                        rhs = q_.t[pr * 64:(pr + 1) * 64, qcol0:qcol0 + nq]
                        P.op("pe", lambda sb_=sb_, lhs=lhs, rhs=rhs: nc.tensor.matmul(sb_.t[:, 0:nq], lhsT=lhs, rhs=rhs, start=True, stop=True),
                             reads=[k_.res, q_.res], writes=[sb_.res])
                        outs.append(sb_)
                    return outs

                pend_S = emit_S(0)
                for ei in range(len(ent)):
                    kc, pos = ent[ei]
                    cur_S = pend_S
                    if ei + 1 < len(ent):
                        pend_S = emit_S(ei + 1)
                    es = []
                    for pr in range(2):
                        et = EE[ecnt[0] % 6]
                        ecnt[0] += 1
                        sb_ = cur_S[pr]
                        if is_ret:
                            off = qpos0 - pos + 2176
                            th = TH[pr]
                            P.op("dve", lambda et=et, sb_=sb_, off=off, th=th: nc.vector.tensor_tensor(
                                out=et.t[:, 0:nq], in0=sb_.t[:, 0:nq], in1=th.t[:, off:off + nq], op=ALU.mult),
                                reads=[sb_.res, th.res], writes=[et.res])
                        else:
                            P.op("act", lambda et=et, sb_=sb_: nc.scalar.activation(out=et.t[:, 0:nq], in_=sb_.t[:, 0:nq], func=AF.Exp),
                                 reads=[sb_.res], writes=[et.res])
                        es.append(et)
                    for pr in range(2):
                        v_ = vs[pr]
                        for qc in range(nqc):
                            if is_ret:
                                ab = rbank[pr]
                                dst = ab.t[:, qc * 128:(qc + 1) * 128]
                                rhs = v_.t[:, kc, 0:128]
                                st_flag = (ei == 0 and qc == 0)
                            else:
                                a = pr * nqc + qc
                                ab = banks[4 + a // 3]
                                sl = a % 3
                                dst = ab.t[:, sl * 129:(sl + 1) * 129]
                                rhs = v_.t[:, kc, 0:129]
                                st_flag = (ei == 0 and sl == 0)
                            lastmm = (ei == len(ent) - 1)
                            P.op("pe", lambda dst=dst, rhs=rhs, et=es[pr], qc=qc, st_flag=st_flag, lastmm=lastmm: nc.tensor.matmul(
                                dst, lhsT=et.t[:, qc * 128:(qc + 1) * 128], rhs=rhs, start=st_flag, stop=lastmm, skip_group_check=True),
                                reads=[es[pr].res, v_.res], writes=[ab.res], inc=(lastmm or qc == nqc - 1))
                if is_ret:
                    for pr in range(2):
                        evac_ret(rbank[pr], nqc, heads[pr], row0, gts[pr], mts[pr])
                else:
                    evac_diff(nqc, uidx, row0, mts[0])
        P.barrier()


def phase_outproj(E):
    nc, P, g, L = E.nc, E.P, E.g, E.L
    banks, bank_bf = E.banks, E.bank_bf
    with (SBT(nc, "oW", [128, 16, D], BF16) as ow_t, SBT(nc, "oG", [128, 2, D], F32) as og_t,
          SBT(nc, "oM", [128, 2, D], BF16) as om_t, SBT(nc, "oMT", [128, 2, 16, 128], BF16) as omt_t,
          SBT(nc, "oX", [128, 2, D], F32) as ox_t, SBT(nc, "oT", [128, 2, 512], F32) as ot_t):
        Wo = Tile(ow_t, "oW")
        G2 = [Tile(og_t[:, i, :], f"oG{i}") for i in range(2)]
        Mc = [Tile(om_t[:, i, :], f"oM{i}") for i in range(2)]
        MT = [Tile(omt_t[:, i], f"oMT{i}") for i in range(2)]
        X = [Tile(ox_t[:, i, :], f"oX{i}") for i in range(2)]
        Tm = [Tile(ot_t[:, i, :], f"oT{i}") for i in range(2)]
        src = g.w_out[L].rearrange("(c p) n -> p c n", p=128)
        for q4 in range(4):
            P.dma("pool", lambda q4=q4: nc.gpsimd.dma_start(out=Wo[:, q4 * 4:(q4 + 1) * 4, :], in_=src[:, q4 * 4:(q4 + 1) * 4, :]),
                  reads=[E.rIN], writes=[Wo.res])
        E.load_bc(G2[1], E.MOD[0:1, 2 * D:3 * D])
        E.load_bc(G2[0], E.MOD[1:2, 2 * D:3 * D])
        cnt = 0

        def stage_a(tc):
            k = tc % 2
            m_, mt_, x_ = Mc[k], MT[k], X[k]
            P.dma("sp", lambda: nc.sync.dma_start(out=m_.t, in_=E.MG[tc * 128:(tc + 1) * 128, :]), reads=[E.rMG], writes=[m_.res])
            P.dma("sp", lambda: nc.sync.dma_start(out=x_.t, in_=E.XS[tc * 128:(tc + 1) * 128, :]), reads=[E.rXS], writes=[x_.res])
            for half in range(2):
                bi = (tc % 2) * 2 + half
                pb, pbv = banks[bi], bank_bf(bi)
                for j in range(8):
                    dc = half * 8 + j
                    P.op("pe", lambda dc=dc, j=j, pbv=pbv: nc.tensor.transpose(
                        out=pbv[:, j * 128:(j + 1) * 128], in_=m_.t[:, dc * 128:(dc + 1) * 128], identity=E.ident[:]),
                        reads=[m_.res, E.ident.res], writes=[pb.res], inc=(j == 7))
                dst = mt_.t[:, half * 8:(half + 1) * 8, :]
                if half == 0:
                    P.op("act", lambda dst=dst, pbv=pbv: nc.scalar.copy(out=dst, in_=pbv.rearrange("p (j t) -> p j t", j=8)),
                         reads=[pb.res], writes=[mt_.res])
                else:
                    P.op("dve", lambda dst=dst, pbv=pbv: nc.vector.tensor_copy(out=dst, in_=pbv.rearrange("p (j t) -> p j t", j=8)),
                         reads=[pb.res], writes=[mt_.res])

        tcs = list(range(E.tc0, NCH))
        stage_a(tcs[0])
        for ti, tc in enumerate(tcs):
            k = tc % 2
            isx = 1 if tc >= 2 else 0
            m_, mt_, x_ = Mc[k], MT[k], X[k]
            for db in range(4):
                pb = banks[4 + cnt % 4]
                tm = Tm[cnt % 2]
                cnt += 1
                for mc in range(16):
                    P.op("pe", lambda mc=mc, pb=pb, db=db: nc.tensor.matmul(
                        pb.t[:, :], lhsT=mt_.t[:, mc, :], rhs=Wo[:, mc, db * 512:(db + 1) * 512], start=(mc == 0), stop=(mc == 15)),
                        reads=[mt_.res, Wo.res], writes=[pb.res], inc=(mc == 15))
                if db == 0 and ti + 1 < len(tcs):
                    stage_a(tcs[ti + 1])
                P.op("dve", lambda pb=pb, tm=tm, db=db: nc.vector.tensor_tensor(
                    out=tm.t, in0=pb.t[:, :], in1=G2[isx].t[:, db * 512:(db + 1) * 512], op=ALU.mult),
                    reads=[pb.res, G2[isx].res], writes=[tm.res])
                P.op("pool", lambda tm=tm, db=db: nc.gpsimd.tensor_tensor(
                    out=x_.t[:, db * 512:(db + 1) * 512], in0=x_.t[:, db * 512:(db + 1) * 512], in1=tm.t, op=ALU.add),
                    reads=[tm.res, x_.res], writes=[x_.res])
            P.dma("sp", lambda: nc.sync.dma_start(out=E.XS[tc * 128:(tc + 1) * 128, :], in_=x_.t), reads=[x_.res], writes=[E.rXS])
        P.barrier()


def phase_topk(E):
    import contextlib
    with contextlib.ExitStack() as stack:
        E.mod_alloc = None
        if E.next_mod is not None:
            nc = E.nc
            E.mod_alloc = (stack.enter_context(SBT(nc, "mw0", [128, 16, 512], BF16)),
                           stack.enter_context(SBT(nc, "mw1", [128, 16, 512], BF16)),
                           stack.enter_context(SBT(nc, "mb", [2, 2, 512], F32)),
                           stack.enter_context(SBT(nc, "mo", [2, 2, 512], F32)))
        _phase_topk(E)


def _phase_topk(E):
    nc, P, g, L = E.nc, E.P, E.g, E.L
    banks = E.banks
    AFF, IDXU, GATE, identf = E.AFF, E.IDXU, E.GATE, E.identf
    with (SBT(nc, "kAT", [16, T], F32) as at_t, SBT(nc, "kWK", [16, T], F32) as wk_t,
          SBT(nc, "kM8", [16, 16], F32) as m8_t, SBT(nc, "kMK", [16, T], F32) as mk_t,
          SBT(nc, "kPS", [16, T], F32) as ps_t, SBT(nc, "kON", [16, T], F32) as on_t,
          SBT(nc, "kSL", [128, NCH, NE], F32) as sl_t, SBT(nc, "kPm", [128, 3, 256], F32) as pm_t,
          SBT(nc, "kTG", [128, NCH, NE, 2], F32) as tg_t, SBT(nc, "kIO", [128, NSLOT], F32) as io_t,
          SBT(nc, "kTK", [128, NCH], F32) as tk_t, SBT(nc, "kIG", [128, NE, 3, 2], F32) as ig_t):
        AT, WK, M8, MK, PS, ON = (Tile(t, n) for t, n in ((at_t, "kAT"), (wk_t, "kWK"), (m8_t, "kM8"), (mk_t, "kMK"), (ps_t, "kPS"), (on_t, "kON")))
        SL, TG, IO, TK, IG = (Tile(t, n) for t, n in ((sl_t, "kSL"), (tg_t, "kTG"), (io_t, "kIO"), (tk_t, "kTK"), (ig_t, "kIG")))
        PM = [Tile(pm_t[:, i, :], f"kPm{i}") for i in range(3)]
        mgen = None
        if E.next_mod is not None:
            w0, w1, mb_t, mo_t = E.mod_alloc
            ws, mb, mo = E.mod_tiles(w0, w1, mb_t, mo_t)
            mgen = E.mod_gen(E.next_mod, ws, mb, mo, [banks[7]])

        def mod_step(n=1):
            if mgen is not None:
                for _ in range(n):
                    next(mgen, None)

        P.dma("sp", lambda: nc.sync.dma_start(out=IO[:], in_=g.k_iota[:, :]), reads=[E.rIN], writes=[IO.res])
        P.dma("sp", lambda: nc.sync.dma_start(out=TK[:], in_=g.k_tokid[:, :]), reads=[E.rIN], writes=[TK.res])
        P.op("pool", lambda: nc.gpsimd.memset(ON[:], 1.0), writes=[ON.res])
        P.op("pool", lambda: nc.gpsimd.memset(IG[:], 0.0), writes=[IG.res])
        tcs = list(range(E.tc0, NCH))
        for gi in range(0, len(tcs), 4):
            grp = tcs[gi:gi + 4]
            pb = banks[(gi // 4) % 2]
            for j, tc in enumerate(grp):
                P.op("pe", lambda j=j, tc=tc, pb=pb: nc.tensor.transpose(
                    out=pb.t[0:16, j * 128:(j + 1) * 128], in_=AFF[:, tc, :], identity=identf[:]),
                    reads=[AFF.res, identf.res], writes=[pb.res], inc=(j == len(grp) - 1))
            c0 = grp[0] * 128
            n = len(grp) * 128
            P.op("act", lambda pb=pb, c0=c0, n=n: nc.scalar.copy(out=AT[:, c0:c0 + n], in_=pb.t[0:16, 0:n]), reads=[pb.res], writes=[AT.res])
        ranges = [(CTX, T, CAPX, 0)] + ([] if E.last else [(0, CTX, CAPC, 1)])
        for (a, b, cap, ti) in ranges:
            P.op("dve", lambda a=a, b=b: nc.vector.tensor_copy(out=WK[:, a:b], in_=AT[:, a:b]), reads=[AT.res], writes=[WK.res])
            nr = cap // 8
            for r in range(nr):
                P.op("dve", lambda a=a, b=b, ti=ti: nc.vector.max(out=M8[:, ti * 8:ti * 8 + 8], in_=WK[:, a:b]), reads=[WK.res], writes=[M8.res])
                mod_step(1)
                if r < nr - 1:
                    P.op("dve", lambda a=a, b=b, ti=ti: nc.vector.match_replace(
                        out=WK[:, a:b], in_to_replace=M8[:, ti * 8:ti * 8 + 8], in_values=WK[:, a:b], imm_value=-1.0),
                        reads=[WK.res, M8.res], writes=[WK.res])
            thr = M8[:, ti * 8 + 7:ti * 8 + 8]
            P.op("dve", lambda a=a, b=b, thr=thr: nc.vector.tensor_scalar(out=MK[:, a:b], in0=AT[:, a:b], scalar1=thr, scalar2=None, op0=ALU.is_ge),
                 reads=[AT.res, M8.res], writes=[MK.res])
            P.op("dve", lambda a=a, b=b: nc.vector.tensor_tensor_scan(out=PS[:, a:b], data0=ON[:, a:b], data1=MK[:, a:b], initial=0.0,
                                                                       op0=ALU.mult, op1=ALU.add), reads=[ON.res, MK.res], writes=[PS.res])
            if ti == 1:
                P.op("dve", lambda a=a, b=b: nc.vector.tensor_scalar(out=PS[:, a:b], in0=PS[:, a:b], scalar1=float(CAPX), scalar2=None, op0=ALU.add),
                     reads=[PS.res], writes=[PS.res])
            P.op("dve", lambda a=a, b=b: nc.vector.tensor_tensor(out=PS[:, a:b], in0=PS[:, a:b], in1=MK[:, a:b], op=ALU.mult),
                 reads=[PS.res, MK.res], writes=[PS.res])
            P.op("dve", lambda a=a, b=b: nc.vector.tensor_scalar(out=PS[:, a:b], in0=PS[:, a:b], scalar1=-1.0, scalar2=None, op0=ALU.add),
                 reads=[PS.res], writes=[PS.res])
        for gi in range(0, len(tcs), 8):
            grp = tcs[gi:gi + 8]
            pb = banks[2 + (gi // 8) % 2]
            for j, tc in enumerate(grp):
                P.op("pe", lambda j=j, tc=tc, pb=pb: nc.tensor.transpose(
                    out=pb.t[:, j * 16:(j + 1) * 16], in_=PS[:, tc * 128:(tc + 1) * 128], identity=identf[0:16, 0:16]),
                    reads=[PS.res, identf.res], writes=[pb.res], inc=(j == len(grp) - 1))
            P.op("act", lambda pb=pb, grp=grp: nc.scalar.copy(
                out=SL[:, grp[0]:grp[0] + len(grp), :], in_=pb.t[:, 0:len(grp) * 16].rearrange("p (c e) -> p c e", e=16)),
                reads=[pb.res], writes=[SL.res])
        P.op("dve", lambda: nc.vector.tensor_copy(out=TG[:, :, :, 0], in_=TK[:, :].unsqueeze(2).to_broadcast([128, NCH, NE])),
             reads=[TK.res], writes=[TG.res])
        P.op("dve", lambda: nc.vector.tensor_copy(out=TG[:, :, :, 1], in_=AFF[:]), reads=[AFF.res, TG.res], writes=[TG.res])
        pc = 0
        bA, bB, bC = banks[4], banks[5], banks[6]
        for e in range(NE):
            for tc in tcs:
                pm = PM[pc % 3]
                pc += 1
                if tc >= 2:
                    P.op("dve", lambda pm=pm, tc=tc: nc.vector.tensor_scalar(
                        out=pm.t[:, 0:256], in0=IO[:, 0:256], scalar1=SL[:, tc, e:e + 1], scalar2=None, op0=ALU.is_equal),
                        reads=[IO.res, SL.res], writes=[pm.res])
                    for cc, bb in ((0, bA), (1, bB)):
                        P.op("pe", lambda pm=pm, tc=tc, cc=cc, bb=bb: nc.tensor.matmul(
                            bb.t[:, e * 2:(e + 1) * 2], lhsT=pm.t[:, cc * 128:(cc + 1) * 128], rhs=TG[:, tc, e, :],
                            start=(tc == 2), stop=(tc == NCH - 1), skip_group_check=True),
                            reads=[pm.res, TG.res], writes=[bb.res], inc=(cc == 1))
                else:
                    P.op("dve", lambda pm=pm, tc=tc: nc.vector.tensor_scalar(
                        out=pm.t[:, 0:32], in0=IO[:, 256:288], scalar1=SL[:, tc, e:e + 1], scalar2=None, op0=ALU.is_equal),
                        reads=[IO.res, SL.res], writes=[pm.res])
                    P.op("pe", lambda pm=pm, tc=tc: nc.tensor.matmul(
                        bC.t[0:32, e * 2:(e + 1) * 2], lhsT=pm.t[:, 0:32], rhs=TG[:, tc, e, :],
                        start=(tc == 0), stop=(tc == 1), skip_group_check=True),
                        reads=[pm.res, TG.res], writes=[bC.res])
        mod_step(24)
        P.op("act", lambda: nc.scalar.copy(out=IG[:, :, 0, :], in_=bA.t[:, 0:32].rearrange("p (e two) -> p e two", two=2)),
             reads=[bA.res, IG.res], writes=[IG.res])
        P.op("act", lambda: nc.scalar.copy(out=IG[:, :, 1, :], in_=bB.t[:, 0:32].rearrange("p (e two) -> p e two", two=2)),
             reads=[bB.res, IG.res], writes=[IG.res])
        if not E.last:
            P.op("act", lambda: nc.scalar.copy(out=IG[0:32, :, 2, :], in_=bC.t[0:32, 0:32].rearrange("p (e two) -> p e two", two=2)),
                 reads=[bC.res, IG.res], writes=[IG.res])
        P.op("dve", lambda: nc.vector.tensor_copy(out=IDXU[:], in_=IG[:, :, :, 0]), reads=[IG.res], writes=[IDXU.res])
        P.op("dve", lambda: nc.vector.tensor_copy(out=GATE[:], in_=IG[:, :, :, 1]), reads=[IG.res], writes=[GATE.res])
        P.barrier()


def phase_moe(E):
    nc, P, g, L = E.nc, E.P, E.g, E.L
    banks, bank_bf = E.banks, E.bank_bf
    IDXU, GATE = E.IDXU, E.GATE
    ncc = 2 if E.last else 3
    nsl = CAPX if E.last else NSLOT
    with (SBT(nc, "eG5", [128, 2, D], F32) as g5_t, SBT(nc, "eXG", [128, 2, 3, D], BF16) as xg_t,
          SBT(nc, "eXT", [128, 2, 16, NSLOT], BF16) as xt_t, SBT(nc, "eGU", [128, 2, 2, 16, 512], BF16) as gu_t,
          SBT(nc, "eWD", [128, 8, D], BF16) as wd_t, SBT(nc, "eHT", [128, 8, NSLOT], BF16) as ht_t,
          SBT(nc, "eSG", [128, 2, 512], F32) as sg_t, SBT(nc, "eHS", [128, 3, FF], BF16) as hs_t, SBT(nc, "eY", [128, 2, D], F32) as y_t):
        G5 = [Tile(g5_t[:, i, :], f"eG5{i}") for i in range(2)]
        XG = [Tile(xg_t[:, i], f"eXG{i}") for i in range(2)]
        XT = [Tile(xt_t[:, i], f"eXT{i}") for i in range(2)]
        GU = [Tile(gu_t[:, i], f"eGU{i}") for i in range(2)]
        WD = Tile(wd_t, "eWD")
        HT = Tile(ht_t, "eHT")
        HS = Tile(hs_t, "eHS")
        SG = [Tile(sg_t[:, i, :], f"eSG{i}") for i in range(2)]
        Y = [Tile(y_t[:, i, :], f"eY{i}") for i in range(2)]
        E.load_bc(G5[1], E.MOD[0:1, 5 * D:6 * D])
        E.load_bc(G5[0], E.MOD[1:2, 5 * D:6 * D])
        ycnt = 0
        bcnt = 0

        def issue_gather(e):
            xg = XG[e % 2]
            for cc in range(ncc):
                M_ = 128 if cc < 2 else CAPC
                P.dma("pool", lambda cc=cc, M_=M_: nc.gpsimd.indirect_dma_start(
                    out=xg.t[0:M_, cc, :], out_offset=None, in_=E.HT2[:, :],
                    in_offset=bass.IndirectOffsetOnAxis(ap=IDXU[0:M_, e, cc:cc + 1], axis=0)),
                    reads=[IDXU.res, E.rHT2], writes=[xg.res])

        def issue_gu(e, fh):
            gu = GU[fh]
            for wi, wsrc in enumerate((g.w_gate, g.w_up)):
                src_ = wsrc[L, e][:, fh * 512:(fh + 1) * 512].rearrange("(c p) n -> p c n", p=128)
                for hh in range(2):
                    P.dma("pool", lambda wi=wi, src_=src_, hh=hh: nc.gpsimd.dma_start(
                        out=gu.t[:, wi, hh * 8:(hh + 1) * 8, :], in_=src_[:, hh * 8:(hh + 1) * 8, :]), reads=[E.rIN], writes=[gu.res])

        def issue_wd(e):
            srcd = g.w_down[L, e].rearrange("(c p) n -> p c n", p=128)
            for hh in range(2):
                P.dma("pool", lambda hh=hh: nc.gpsimd.dma_start(out=WD[:, hh * 4:(hh + 1) * 4, :], in_=srcd[:, hh * 4:(hh + 1) * 4, :]),
                      reads=[E.rIN], writes=[WD.res])

        def do_transposes(e):
            xg, xt = XG[e % 2], XT[e % 2]
            for dc in range(16):
                bi = dc % 2
                pb, pbv = banks[bi], bank_bf(bi)
                for cc in range(ncc):
                    M_ = 128 if cc < 2 else CAPC
                    P.op("pe", lambda cc=cc, M_=M_, dc=dc, pbv=pbv: nc.tensor.transpose(
                        out=pbv[:, cc * 128:cc * 128 + M_], in_=xg.t[0:M_, cc, dc * 128:(dc + 1) * 128], identity=E.ident[0:M_, 0:M_]),
                        reads=[xg.res, E.ident.res], writes=[pb.res], inc=(cc == ncc - 1))
                if dc % 2 == 0:
                    P.op("act", lambda dc=dc, pbv=pbv: nc.scalar.copy(out=xt.t[:, dc, 0:nsl], in_=pbv[:, 0:nsl]), reads=[pb.res], writes=[xt.res])
                else:
                    P.op("dve", lambda dc=dc, pbv=pbv: nc.vector.tensor_copy(out=xt.t[:, dc, 0:nsl], in_=pbv[:, 0:nsl]), reads=[pb.res], writes=[xt.res])

        issue_gather(0)
        issue_gu(0, 0)
        issue_gu(0, 1)
        issue_wd(0)
        do_transposes(0)
        for e in range(NE):
            xt = XT[e % 2]
            if e + 1 < NE:
                issue_gather(e + 1)
            for fh in range(2):
                gu = GU[fh]
                for sc in range(ncc):
                    M_ = 128 if sc < 2 else CAPC
                    pg, pu = banks[2 + (bcnt % 2) * 2], banks[3 + (bcnt % 2) * 2]
                    sg = SG[bcnt % 2]
                    bcnt += 1
                    for wi, pb in ((0, pg), (1, pu)):
                        for dc in range(16):
                            P.op("pe", lambda wi=wi, pb=pb, dc=dc, gu=gu, sc=sc, M_=M_: nc.tensor.matmul(
                                pb.t[0:M_, :], lhsT=xt.t[:, dc, sc * 128:sc * 128 + M_], rhs=gu.t[:, wi, dc, :],
                                start=(dc == 0), stop=(dc == 15)), reads=[gu.res, xt.res], writes=[pb.res], inc=(dc == 15))
                    P.op("act", lambda pg=pg, sg=sg, M_=M_: nc.scalar.activation(out=sg.t[0:M_, :], in_=pg.t[0:M_, :], func=AF.Silu),
                         reads=[pg.res], writes=[sg.res])
                    P.op("dve", lambda pu=pu, sg=sg, fh=fh, sc=sc, M_=M_: nc.vector.tensor_tensor(
                        out=HS[0:M_, sc, fh * 512:(fh + 1) * 512], in0=pu.t[0:M_, :], in1=sg.t[0:M_, :], op=ALU.mult),
                        reads=[pu.res, sg.res], writes=[HS.res])
                if e + 1 < NE:
                    issue_gu(e + 1, fh)
                    if fh == 1:
                        do_transposes(e + 1)
            for fc in range(8):
                bi = fc % 2
                pb, pbv = banks[bi], bank_bf(bi)
                for sc in range(ncc):
                    M_ = 128 if sc < 2 else CAPC
                    P.op("pe", lambda sc=sc, M_=M_, fc=fc, pbv=pbv: nc.tensor.transpose(
                        out=pbv[:, sc * 128:sc * 128 + M_], in_=HS[0:M_, sc, fc * 128:(fc + 1) * 128], identity=E.ident[0:M_, 0:M_]),
                        reads=[HS.res, E.ident.res], writes=[pb.res], inc=(sc == ncc - 1))
                if fc % 2 == 0:
                    P.op("act", lambda fc=fc, pbv=pbv: nc.scalar.copy(out=HT[:, fc, 0:nsl], in_=pbv[:, 0:nsl]), reads=[pb.res], writes=[HT.res])
                else:
                    P.op("dve", lambda fc=fc, pbv=pbv: nc.vector.tensor_copy(out=HT[:, fc, 0:nsl], in_=pbv[:, 0:nsl]), reads=[pb.res], writes=[HT.res])
            for cc in range(ncc):
                M_ = 128 if cc < 2 else CAPC
                isx = 1 if cc < 2 else 0
                y = Y[ycnt % 2]
                ycnt += 1
                for db in range(4):
                    pb = banks[6 + db % 2]
                    for fc in range(8):
                        P.op("pe", lambda fc=fc, pb=pb, db=db, cc=cc, M_=M_: nc.tensor.matmul(
                            pb.t[0:M_, :], lhsT=HT[:, fc, cc * 128:cc * 128 + M_], rhs=WD[:, fc, db * 512:(db + 1) * 512],
                            start=(fc == 0), stop=(fc == 7)), reads=[HT.res, WD.res], writes=[pb.res], inc=(fc == 7))
                    P.op("dve", lambda pb=pb, db=db, cc=cc, M_=M_, y=y, isx=isx: nc.vector.scalar_tensor_tensor(
                        out=y.t[0:M_, db * 512:(db + 1) * 512], in0=pb.t[0:M_, :], scalar=GATE[0:M_, e, cc:cc + 1],
                        in1=G5[isx].t[0:M_, db * 512:(db + 1) * 512], op0=ALU.mult, op1=ALU.mult),
                        reads=[pb.res, GATE.res, G5[isx].res], writes=[y.res])
                P.dma("pool", lambda cc=cc, M_=M_, y=y: nc.gpsimd.indirect_dma_start(
                    out=E.XS[:, :], out_offset=bass.IndirectOffsetOnAxis(ap=IDXU[0:M_, e, cc:cc + 1], axis=0),
                    in_=y.t[0:M_, :], in_offset=None, compute_op=ALU.add),
                    reads=[y.res, IDXU.res, E.rXS], writes=[E.rXS], serial=True)
            if e + 1 < NE:
                issue_wd(e + 1)
        P.barrier()


NCORES = 4


def kernel(x, c, ctx, c_ctx, w_ada, b_ada, norm_mix, norm_ffn, w_in, w_out, ret_log_decay,
           ret_norm, diff_lambda, diff_norm, w_router, w_gate, w_up, w_down, norm_final):
    f = lambda a: np.ascontiguousarray(np.asarray(a, dtype=np.float32))
    nc = build(depth=DEPTH)
    shared = dict(
        w_ada=f(w_ada), b_ada=f(b_ada), norm_mix=f(norm_mix), norm_ffn=f(norm_ffn), w_in=f(w_in), w_out=f(w_out),
        ret_log_decay=f(ret_log_decay).reshape(DEPTH, 16), ret_norm=f(ret_norm),
        diff_lambda=f(diff_lambda).reshape(DEPTH, 256), diff_norm=f(diff_norm), w_router=f(w_router),
        w_gate=f(w_gate), w_up=f(w_up), w_down=f(w_down), norm_final=f(norm_final).reshape(1, D))
    shared.update(host_consts())
    x, c, ctx, c_ctx = f(x), f(c), f(ctx), f(c_ctx)
    in_maps = []
    for b in range(NCORES):
        m = dict(shared)
        m.update(x=x[b], ctx=ctx[b], cvec=np.ascontiguousarray(np.stack([c[b], c_ctx])))
        in_maps.append({k: m[k] for k in nc._used_inputs})
    res = run_bass_kernel_spmd(nc, in_maps, core_ids=list(range(NCORES)))
    return np.stack([np.asarray(res.results[b]["out"], dtype=np.float32) for b in range(NCORES)])
```

```python
import numpy as np
import ml_dtypes
import concourse.bass as bass
import concourse.mybir as mybir
from concourse.bass_utils import run_bass_kernel_spmd

F32 = mybir.dt.float32
BF16 = mybir.dt.bfloat16
U32 = mybir.dt.uint32
AF = mybir.ActivationFunctionType
ALU = mybir.AluOpType
AX = mybir.AxisListType

D = 2048
SEQ = 2048
CTX = 256
T = SEQ + CTX
NCH = T // 128
DEPTH = 4
NE = 16
FF = 1024
PROJ = 6144
EPS = 1e-6
CAPX = 256
CAPC = 32
NSLOT = CAPX + CAPC
DMIN = -(SEQ + CTX - 128) - 127
FLEN = 4608
SW = 4480


_uniq = [0]


def SBT(nc, name, shape, dt):
    _uniq[0] += 1
    return nc.sbuf_tensor(f"{name}_{_uniq[0]}", shape, dt)


class Res:
    __slots__ = ("name", "w", "r", "pw")

    def __init__(self, name):
        self.name = name
        self.w = None
        self.r = {}
        self.pw = []


class Prog:
    ENG = ("pe", "act", "dve", "pool", "sp")

    def __init__(self, nc):
        self.nc = nc
        self.e = dict(pe=nc.tensor, act=nc.scalar, dve=nc.vector, pool=nc.gpsimd, sp=nc.sync)
        self.psem = {k: nc.alloc_semaphore("p_" + k) for k in self.ENG}
        self.pcnt = {k: 0 for k in self.ENG}
        self.pend = {k: False for k in self.ENG}
        self.seen = {k: {} for k in self.ENG}
        self.dsem = {}
        self.dval = {}
        self.dnext = {}
        for q, n in (("sp", 20), ("pool", 20), ("act", 6)):
            self.dsem[q] = [nc.alloc_semaphore(f"d_{q}{i}") for i in range(n)]
            self.dval[q] = [0] * n
            self.dnext[q] = 0
        self.bsem = nc.alloc_semaphore("barrier")
        self.bcnt = 0
        self.ninst = 0

    def _semof(self, key):
        if key[0] == "e":
            return self.psem[key[1]]
        return self.dsem[key[1]][key[2]]

    def _need(self, eng, events):
        need = {}
        for ev in events:
            if ev is None:
                continue
            key, val = ev
            if key == ("e", "pe") and eng == "pe":
                continue
            if self.seen[eng].get(key, 0) >= val:
                continue
            if need.get(key, 0) < val:
                need[key] = val
        return need

    def _emit(self, eng, fn, need):
        items = list(need.items())
        for key, val in items[:-1]:
            self.e[eng].wait_ge(self._semof(key), val)
            self.ninst += 1
        ins = fn()
        if items:
            key, val = items[-1]
            ins._wait_ge(self._semof(key), val)
        for key, val in items:
            self.seen[eng][key] = val
        self.ninst += 1
        return ins

    def op(self, eng, fn, reads=(), writes=(), inc=True):
        evs = []
        for r in reads:
            evs.append(r.w)
            evs.extend(r.pw)
        for w in writes:
            if w.w is not None and not (w.w[0] == ("e", eng)):
                evs.append(w.w)
            evs.extend(w.pw)
            for k, v in w.r.items():
                if k != ("e", eng):
                    evs.append((k, v))
        need = self._need(eng, evs)
        ins = self._emit(eng, fn, need)
        for w in writes:
            w.pw = []
        if inc:
            self.pcnt[eng] += 1
            ins.then_inc(self.psem[eng], 1)
            self.pend[eng] = False
            val = self.pcnt[eng]
        else:
            self.pend[eng] = True
            val = self.pcnt[eng] + 1
        ev = (("e", eng), val)
        for w in writes:
            w.w = ev
            w.r = {}
        for r in reads:
            if r.r.get(ev[0], 0) < val:
                r.r[ev[0]] = val
        return ins

    def dma(self, q, fn, reads=(), writes=(), serial=False):
        eng = q
        i = self.dnext[q]
        self.dnext[q] = (i + 1) % len(self.dsem[q])
        key = ("d", q, i)
        evs = [(key, self.dval[q][i])] if self.dval[q][i] else []
        for r in reads:
            evs.append(r.w)
            evs.extend(r.pw)
        keep = []
        for w in writes:
            partial = (not serial) and (not w.r) and w.w is not None and w.w[0][0] == "d"
            if partial:
                keep.append((w, w.pw + [w.w]))
            else:
                keep.append((w, []))
                evs.append(w.w)
                evs.extend(w.pw)
                for k, v in w.r.items():
                    evs.append((k, v))
        need = self._need(eng, evs)
        ins = self._emit(eng, fn, need)
        self.dval[q][i] += 16
        ins.then_inc(self.dsem[q][i], 16)
        ev = (key, self.dval[q][i])
        for w, pw in keep:
            w.pw = pw
            w.w = ev
            w.r = {}
        for r in reads:
            if r.r.get(key, 0) < ev[1]:
                r.r[key] = ev[1]
        return ins

    def flush(self, eng):
        if self.pend[eng]:
            self.pcnt[eng] += 1
            self.e[eng].nop().then_inc(self.psem[eng], 1)
            self.pend[eng] = False
            self.ninst += 1

    def barrier(self):
        for k in self.ENG:
            self.flush(k)
        sp = self.e["sp"]
        for k in self.ENG:
            if k != "sp" and self.seen["sp"].get(("e", k), 0) < self.pcnt[k]:
                sp.wait_ge(self.psem[k], self.pcnt[k])
                self.ninst += 1
        for q in self.dsem:
            for i, s in enumerate(self.dsem[q]):
                v = self.dval[q][i]
                if v and self.seen["sp"].get(("d", q, i), 0) < v:
                    sp.wait_ge(s, v)
                    self.ninst += 1
        self.bcnt += 1
        sp.nop().then_inc(self.bsem, 1)
        self.ninst += 1
        for k in self.ENG:
            if k != "sp":
                self.e[k].wait_ge(self.bsem, self.bcnt)
                self.ninst += 1
        for k in self.ENG:
            for k2 in self.ENG:
                self.seen[k][("e", k2)] = self.pcnt[k2]
            for q in self.dsem:
                for i in range(len(self.dsem[q])):
                    self.seen[k][("d", q, i)] = self.dval[q][i]


class Tile:
    def __init__(self, t, name):
        self.t = t
        self.res = Res(name)

    def __getitem__(self, k):
        return self.t[k]


class Ctx:
    pass


class _Inputs:
    def __init__(self, nc, depth):
        self._nc = nc
        self._spec = {
            "x": ([SEQ, D], F32), "ctx": ([CTX, D], F32), "cvec": ([2, D], F32),
            "w_ada": ([depth, D, 6 * D], F32), "b_ada": ([depth, 6 * D], F32),
            "norm_mix": ([depth, D], F32), "norm_ffn": ([depth, D], F32),
            "w_in": ([depth, D, PROJ], F32), "w_out": ([depth, D, D], F32),
            "ret_log_decay": ([depth, 16], F32), "ret_norm": ([depth, 1024], F32),
            "diff_lambda": ([depth, 256], F32), "diff_norm": ([depth, 1024], F32),
            "w_router": ([depth, D, NE], F32), "w_gate": ([depth, NE, D, FF], F32),
            "w_up": ([depth, NE, D, FF], F32), "w_down": ([depth, NE, FF, D], F32),
            "norm_final": ([1, D], F32),
            "k_ident": ([128, 128], BF16), "k_identf": ([128, 128], F32),
            "k_rope": ([SEQ, 128], F32), "k_iota": ([128, NSLOT], F32),
            "k_tokid": ([128, NCH], F32), "k_strip": ([128, 2, SW], F32),
        }
        self.used = {}

    def __getattr__(self, name):
        if name.startswith("_") or name == "used":
            raise AttributeError(name)
        if name not in self.used:
            shape, dt = self._spec[name]
            self.used[name] = self._nc.dram_tensor(name, list(shape), dt, kind="ExternalInput").ap()
        return self.used[name]


def _mk_inputs_decl(nc, depth):
    return _Inputs(nc, depth)


def host_consts():
    k = {}
    k["k_ident"] = np.eye(128, dtype=np.float32).astype(ml_dtypes.bfloat16)
    k["k_identf"] = np.eye(128, dtype=np.float32)
    n_freq = 16
    freqs = (10000.0 ** (-np.arange(n_freq, dtype=np.float32) / n_freq)).astype(np.float32)
    row = np.repeat(np.arange(SEQ // 64, dtype=np.float32), 64)
    col = np.tile(np.arange(64, dtype=np.float32), SEQ // 64)
    ang = np.concatenate([row[:, None] * freqs, col[:, None] * freqs], axis=-1).astype(np.float32)
    cs, sn = np.cos(ang).astype(np.float32), np.sin(ang).astype(np.float32)
    k["k_rope"] = np.concatenate([cs * 0.125, sn * 0.125, cs, sn], axis=1).astype(np.float32)
    k["k_iota"] = np.tile(np.arange(NSLOT, dtype=np.float32)[None, :], (128, 1))
    k["k_tokid"] = (np.arange(NCH, dtype=np.float32)[None, :] * 128 + np.arange(128, dtype=np.float32)[:, None])
    s = np.arange(SW, dtype=np.float32)[None, :] - np.arange(128, dtype=np.float32)[:, None] - 2176.0
    k["k_strip"] = np.stack([np.maximum(s, 0), np.maximum(-s, 0)], axis=1).astype(np.float32)
    return k


def build(depth=DEPTH, phases=None, debug=False, final=True, l0=0):
    nc = bass.Bass("TRN2", target_bir_lowering=False)
    P = Prog(nc)
    g = _mk_inputs_decl(nc, depth)
    okind = "ExternalOutput"
    out = nc.dram_tensor("out", [SEQ, D], F32, kind=okind).ap()
    skind = "ExternalOutput" if debug else "Internal"
    dram = lambda name, shape, dt: nc.dram_tensor(name, list(shape), dt, kind=skind).ap()
    XS = dram("XS", [T, D], F32)
    MODB = [dram("MOD0", [2, 6 * D], F32), dram("MOD1", [2, 6 * D], F32)]
    QT = dram("QT", [24, 128, T], BF16)
    VD = dram("VD", [T, 16, 130], BF16)
    GD = dram("GD", [T, 1024], F32)
    MG = dram("MG", [T, D], BF16)
    HT2 = dram("HT2", [T, D], BF16)
    rXS, rQT, rVD, rGD, rMG, rHT2 = (Res(n) for n in ("XS", "QT", "VD", "GD", "MG", "HT2"))
    rMODB = [Res("MOD0"), Res("MOD1")]
    rIN = Res("inputs")

    def want(ph):
        return phases is None or ph in phases

    def sb(name, shape, dt):
        return Tile(nc.alloc_sbuf_tensor(name, list(shape), dt), name)

    ident = sb("ident", [128, 128], BF16)
    identf = sb("identf", [128, 128], F32)
    SC = sb("silu_c", [128, 16, 2], BF16)
    AFF = sb("AFF", [128, NCH, NE], F32)
    IDXU = sb("IDXU", [128, NE, 3], U32)
    GATE = sb("GATE", [128, NE, 3], F32)
    banks = [Tile(nc.alloc_psum_tensor(f"bank{i}", [128, 512], F32), f"bank{i}") for i in range(8)]

    def bank_bf(i):
        return banks[i].t[:].bitcast(BF16)

    P.dma("sp", lambda: nc.sync.dma_start(out=ident[:], in_=g.k_ident[:, :]), reads=[rIN], writes=[ident.res])
    P.dma("sp", lambda: nc.sync.dma_start(out=identf[:], in_=g.k_identf[:, :]), reads=[rIN], writes=[identf.res])
    P.dma("sp", lambda: nc.sync.dma_start(out=XS[0:CTX, :], in_=g.ctx[:, :]), reads=[rIN], writes=[rXS])
    P.dma("sp", lambda: nc.sync.dma_start(out=XS[CTX:T, :], in_=g.x[:, :]), reads=[rIN], writes=[rXS])
    with SBT(nc, "c_raw", [128, 16, 2], F32) as craw_t:
        craw = Tile(craw_t, "craw")
        for r in range(2):
            P.dma("sp", lambda r=r: nc.sync.dma_start(
                out=craw[:, :, r], in_=g.cvec[r, :].rearrange("(c p) -> p c", p=128),
                allow_slow_non_contiguous=True), reads=[rIN], writes=[craw.res])
        P.op("act", lambda: nc.scalar.activation(out=SC[:], in_=craw[:], func=AF.Silu),
             reads=[craw.res], writes=[SC.res])
        P.barrier()

    for L in range(depth):
        Lg = l0 + L
        last = (Lg == DEPTH - 1) and final
        tc0 = 2 if last else 0
        lam_init = 0.8 - 0.6 * float(np.exp(-0.3 * Lg))

        MOD, rMOD = MODB[L % 2], rMODB[L % 2]

        def mod_gen(Lw, ws, mb, mo, pbs):
            dstM, rdst = MODB[Lw % 2], rMODB[Lw % 2]
            for nb in range(24):
                w = ws[nb % 2]
                b_, o_ = mb[nb % 2], mo[nb % 2]
                srcw = g.w_ada[Lw][:, nb * 512:(nb + 1) * 512].rearrange("(c p) n -> p c n", p=128)
                for hh in range(2):
                    P.dma("pool", lambda w=w, srcw=srcw, hh=hh: nc.gpsimd.dma_start(
                        out=w.t[:, hh * 8:(hh + 1) * 8, :], in_=srcw[:, hh * 8:(hh + 1) * 8, :]),
                        reads=[rIN], writes=[w.res])
                P.dma("sp", lambda b_=b_, nb=nb: nc.sync.dma_start(
                    out=b_.t, in_=g.b_ada[Lw:Lw + 1, nb * 512:(nb + 1) * 512].to_broadcast([2, 512])),
                    reads=[rIN], writes=[b_.res])
                pb = pbs[nb % len(pbs)]
                for dc in range(16):
                    P.op("pe", lambda w=w, dc=dc, pb=pb: nc.tensor.matmul(
                        pb.t[0:2, :], lhsT=SC[:, dc, :], rhs=w.t[:, dc, :], start=(dc == 0), stop=(dc == 15)),
                        reads=[SC.res, w.res], writes=[pb.res], inc=(dc == 15))
                P.op("dve", lambda o_=o_, pb=pb, b_=b_: nc.vector.tensor_tensor(
                    out=o_.t, in0=pb.t[0:2, :], in1=b_.t, op=ALU.add),
                    reads=[pb.res, b_.res], writes=[o_.res])
                P.dma("sp", lambda o_=o_, nb=nb: nc.sync.dma_start(
                    out=dstM[:, nb * 512:(nb + 1) * 512], in_=o_.t), reads=[o_.res], writes=[rdst])
                yield nb

        def mod_tiles(w0, w1, mb_t, mo_t):
            ws = [Tile(w0, "mw0"), Tile(w1, "mw1")]
            mb = [Tile(mb_t[:, i, :], f"mb{i}") for i in range(2)]
            mo = [Tile(mo_t[:, i, :], f"mo{i}") for i in range(2)]
            return ws, mb, mo

        if want("mod") and (L == 0 or not want("topk")):
            with (SBT(nc, "mw0", [128, 16, 512], BF16) as w0, SBT(nc, "mw1", [128, 16, 512], BF16) as w1,
                  SBT(nc, "mb", [2, 2, 512], F32) as mb_t, SBT(nc, "mo", [2, 2, 512], F32) as mo_t):
                ws, mb, mo = mod_tiles(w0, w1, mb_t, mo_t)
                for _ in mod_gen(L, ws, mb, mo, [banks[0], banks[1]]):
                    pass
                P.barrier()

        def load_bc(tile, src_row):
            P.dma("sp", lambda: nc.sync.dma_start(out=tile[:], in_=src_row.to_broadcast([128, D])),
                  reads=[rMOD, rIN], writes=[tile.res])

        def norm_phase(which, hT, store_ht2, router):
            gain = (g.norm_mix if which == 0 else g.norm_ffn)[L:L + 1, :]
            s_shift, s_scale = (0, 1) if which == 0 else (3, 4)
            with (SBT(nc, "nA", [128, 2, D], F32) as nA_t, SBT(nc, "nB", [128, 2, D], F32) as nB_t,
                  SBT(nc, "nG", [128, D], F32) as nG_t, SBT(nc, "nX", [128, 2, D], F32) as nX_t,
                  SBT(nc, "nT", [128, 2, D], F32) as nT_t, SBT(nc, "nH", [128, 2, D], BF16) as nH_t,
                  SBT(nc, "nJ", [128, D], BF16) as nJ_t, SBT(nc, "nS", [128, 2, 4], F32) as nS_t,
                  SBT(nc, "nHT", [128, 2, 16, 128], BF16) as nHT_t, SBT(nc, "nWr", [128, 16, NE], BF16) as nWr_t,
                  SBT(nc, "nLg", [128, 2, 40], F32) as nLg_t):
                A = [Tile(nA_t[:, i, :], f"nA{i}") for i in range(2)]
                B = [Tile(nB_t[:, i, :], f"nB{i}") for i in range(2)]
                G = Tile(nG_t, "nG")
                X = [Tile(nX_t[:, i, :], f"nX{i}") for i in range(2)]
                TT = [Tile(nT_t[:, i, :], f"nT{i}") for i in range(2)]
                H = [Tile(nH_t[:, i, :], f"nH{i}") for i in range(2)]
                J = Tile(nJ_t, "nJ")
                S = [Tile(nS_t[:, i, :], f"nS{i}") for i in range(2)]
                HTc = [Tile(nHT_t[:, i], f"nHT{i}") for i in range(2)]
                Wr = Tile(nWr_t, "nWr")
                LG = [Tile(nLg_t[:, i, :], f"nLg{i}") for i in range(2)]
                load_bc(G, gain)
                for i, row in ((1, 0), (0, 1)):
                    load_bc(A[i], MOD[row:row + 1, s_scale * D:(s_scale + 1) * D])
                    load_bc(B[i], MOD[row:row + 1, s_shift * D:(s_shift + 1) * D])
                    P.op("dve", lambda i=i: nc.vector.scalar_tensor_tensor(
                        out=A[i].t, in0=A[i].t, scalar=1.0, in1=G[:], op0=ALU.add, op1=ALU.mult),
                        reads=[A[i].res, G.res], writes=[A[i].res])
                if router:
                    P.dma("pool", lambda: nc.gpsimd.dma_start(
                        out=Wr[:], in_=g.w_router[L].rearrange("(c p) e -> p c e", p=128)),
                        reads=[rIN], writes=[Wr.res])
                for tc in range(NCH):
                    if router and last and tc < 2:
                        continue
                    k = tc % 2
                    isx = 1 if tc >= 2 else 0
                    x_, t_, h_, s_ = X[k], TT[k], H[k], S[k]
                    P.dma("sp", lambda x_=x_, tc=tc: nc.sync.dma_start(out=x_.t, in_=XS[tc * 128:(tc + 1) * 128, :]),
                          reads=[rXS], writes=[x_.res])
                    P.op("act", lambda x_=x_, s_=s_: nc.scalar.activation(
                        out=J[:], in_=x_.t, func=AF.Square, accum_out=s_.t[:, 0:1]),
                        reads=[x_.res], writes=[J.res, s_.res])
                    P.op("dve", lambda s_=s_: nc.vector.tensor_scalar(
                        out=s_.t[:, 1:2], in0=s_.t[:, 0:1], scalar1=1.0 / D, scalar2=EPS, op0=ALU.mult, op1=ALU.add),
                        reads=[s_.res], writes=[s_.res])
                    P.op("act", lambda s_=s_: nc.scalar.activation(out=s_.t[:, 2:3], in_=s_.t[:, 1:2], func=AF.Sqrt),
                         reads=[s_.res], writes=[s_.res])
                    P.op("dve", lambda s_=s_: nc.vector.reciprocal(out=s_.t[:, 3:4], in_=s_.t[:, 2:3]),
                         reads=[s_.res], writes=[s_.res])
                    P.op("dve", lambda x_=x_, t_=t_, s_=s_, isx=isx: nc.vector.scalar_tensor_tensor(
                        out=t_.t, in0=x_.t, scalar=s_.t[:, 3:4], in1=A[isx].t, op0=ALU.mult, op1=ALU.mult),
                        reads=[x_.res, s_.res, A[isx].res], writes=[t_.res])
                    P.op("pool", lambda t_=t_, h_=h_, isx=isx: nc.gpsimd.tensor_tensor(
                        out=h_.t, in0=t_.t, in1=B[isx].t, op=ALU.add),
                        reads=[t_.res, B[isx].res], writes=[h_.res])
                    if store_ht2:
                        P.dma("sp", lambda h_=h_, tc=tc: nc.sync.dma_start(out=HT2[tc * 128:(tc + 1) * 128, :], in_=h_.t),
                              reads=[h_.res], writes=[rHT2])
                    for half in range(2):
                        pb = banks[2 + (tc % 2) * 2 + half]
                        pbv = bank_bf(2 + (tc % 2) * 2 + half)
                        for j in range(8):
                            dc = half * 8 + j
                            P.op("pe", lambda h_=h_, dc=dc, j=j, pbv=pbv: nc.tensor.transpose(
                                out=pbv[:, j * 128:(j + 1) * 128], in_=h_.t[:, dc * 128:(dc + 1) * 128], identity=ident[:]),
                                reads=[h_.res, ident.res], writes=[pb.res], inc=(j == 7))
                        if hT is not None:
                            dst = hT.t[:, half * 8:(half + 1) * 8, tc * 128:(tc + 1) * 128]
                            dres = hT.res
                        else:
                            dst = HTc[k].t[:, half * 8:(half + 1) * 8, :]
                            dres = HTc[k].res
                        ev_eng = "act" if half == 0 else "dve"
                        if ev_eng == "act":
                            P.op("act", lambda dst=dst, pbv=pbv: nc.scalar.copy(
                                out=dst, in_=pbv.rearrange("p (j t) -> p j t", j=8)),
                                reads=[pb.res], writes=[dres])
                        else:
                            P.op("dve", lambda dst=dst, pbv=pbv: nc.vector.tensor_copy(
                                out=dst, in_=pbv.rearrange("p (j t) -> p j t", j=8)),
                                reads=[pb.res], writes=[dres])
                    if router:
                        pl = banks[6 + tc % 2]
                        lg = LG[k]
                        for dc in range(16):
                            P.op("pe", lambda dc=dc, pl=pl, k=k: nc.tensor.matmul(
                                pl.t[:, 0:NE], lhsT=HTc[k].t[:, dc, :], rhs=Wr[:, dc, :], start=(dc == 0), stop=(dc == 15)),
                                reads=[HTc[k].res, Wr.res], writes=[pl.res], inc=(dc == 15))
                        P.op("dve", lambda pl=pl, lg=lg: nc.vector.tensor_reduce(
                            out=lg.t[:, 32:33], in_=pl.t[:, 0:NE], axis=AX.X, op=ALU.max, negate=True),
                            reads=[pl.res], writes=[lg.res])
                        P.op("act", lambda pl=pl, lg=lg: nc.scalar.activation(
                            out=lg.t[:, 0:NE], in_=pl.t[:, 0:NE], func=AF.Exp, bias=lg.t[:, 32:33], scale=1.0,
                            accum_out=lg.t[:, 33:34]), reads=[pl.res, lg.res], writes=[lg.res])
                        P.op("dve", lambda lg=lg: nc.vector.reciprocal(out=lg.t[:, 34:35], in_=lg.t[:, 33:34]),
                             reads=[lg.res], writes=[lg.res])
                        P.op("dve", lambda lg=lg, tc=tc: nc.vector.tensor_scalar(
                            out=AFF[:, tc, :], in0=lg.t[:, 0:NE], scalar1=lg.t[:, 34:35], scalar2=None, op0=ALU.mult),
                            reads=[lg.res], writes=[AFF.res])
                P.barrier()

        if want("inproj"):
            with SBT(nc, "hT", [128, 16, T], BF16) as hT_t:
                hT = Tile(hT_t, "hT")
                norm_phase(0, hT, False, False)
                with (SBT(nc, "iw", [128, 2, 16, 512], BF16) as iw_t, SBT(nc, "rope", [128, 16, 128], F32) as rope_t,
                      SBT(nc, "qk", [128, 2, 512], BF16) as qk_t, SBT(nc, "rt", [128, 2, 4, 256], F32) as rt_t,
                      SBT(nc, "qts", [128, 4, T], BF16) as qts_t, SBT(nc, "va", [128, 2, 4, 130], BF16) as va_t,
                      SBT(nc, "gs", [128, 2, 512], F32) as gs_t):
                    IW = [Tile(iw_t[:, i], f"iw{i}") for i in range(2)]
                    ROPE = Tile(rope_t, "rope")
                    QK = [Tile(qk_t[:, i, :], f"qk{i}") for i in range(2)]
                    RT = [Tile(rt_t[:, i], f"rt{i}") for i in range(2)]
                    QTS = Tile(qts_t, "qts")
                    VA = [Tile(va_t[:, i], f"va{i}") for i in range(2)]
                    GS = [Tile(gs_t[:, i, :], f"gs{i}") for i in range(2)]
                    P.dma("sp", lambda: nc.sync.dma_start(out=ROPE[:], in_=g.k_rope.rearrange("(c p) f -> p c f", p=128)),
                          reads=[rIN], writes=[ROPE.res])
                    for i in range(2):
                        P.op("pool", lambda i=i: nc.gpsimd.memset(VA[i].t[:, :, 128:130], 1.0), writes=[VA[i].res])
                    kinds = ["q", "k", "v", "v", "g", "g", "q", "q", "k", "k", "v", "v"]
                    qt_base = {0: 0, 1: 4, 6: 8, 7: 12, 8: 16, 9: 20}
                    v_base = {2: 0, 3: 4, 10: 8, 11: 12}
                    cnt = 0
                    pending = []

                    def flush_pending():
                        while pending:
                            pending.pop(0)()

                    for cb in range(12):
                        W = IW[cb % 2]
                        src = g.w_in[L][:, cb * 512:(cb + 1) * 512].rearrange("(c p) n -> p c n", p=128)
                        for hh in range(2):
                            P.dma("pool", lambda W=W, src=src, hh=hh: nc.gpsimd.dma_start(
                                out=W.t[:, hh * 8:(hh + 1) * 8, :], in_=src[:, hh * 8:(hh + 1) * 8, :]),
                                reads=[rIN], writes=[W.res])
                        kind = kinds[cb]
                        for tc in range(NCH):
                            pb = banks[cnt % 4]
                            k2 = cnt % 2
                            cnt += 1
                            for dc in range(16):
                                P.op("pe", lambda W=W, dc=dc, tc=tc, pb=pb: nc.tensor.matmul(
                                    pb.t[:, :], lhsT=hT.t[:, dc, tc * 128:(tc + 1) * 128], rhs=W.t[:, dc, :],
                                    start=(dc == 0), stop=(dc == 15)),
                                    reads=[hT.res, W.res], writes=[pb.res], inc=(dc == 15))
                            flush_pending()
                            if kind in ("q", "k"):
                                qk = QK[k2]
                                if tc < 2:
                                    if kind == "q":
                                        P.op("act", lambda qk=qk, pb=pb: nc.scalar.mul(qk.t, pb.t[:, :], 0.125),
                                             reads=[pb.res], writes=[qk.res])
                                    else:
                                        P.op("act", lambda qk=qk, pb=pb: nc.scalar.copy(out=qk.t, in_=pb.t[:, :]),
                                             reads=[pb.res], writes=[qk.res])
                                else:
                                    rt = RT[k2]
                                    o = 0 if kind == "q" else 64
                                    xc = tc - 2
                                    cosb = ROPE[:, xc:xc + 1, o:o + 32].to_broadcast([128, 8, 32])
                                    sinb = ROPE[:, xc:xc + 1, o + 32:o + 64].to_broadcast([128, 8, 32])
                                    pv = pb.t[:, :].rearrange("p (h two f) -> p h two f", h=8, two=2)
                                    x1, x2 = pv[:, :, 0, :], pv[:, :, 1, :]
                                    rtv = rt.t.rearrange("p a (h f) -> p a h f", h=8)
                                    for a, (xx, cc) in enumerate(((x1, cosb), (x2, sinb), (x1, sinb), (x2, cosb))):
                                        P.op("dve", lambda a=a, xx=xx, cc=cc, rtv=rtv: nc.vector.tensor_tensor(
                                            out=rtv[:, a], in0=xx, in1=cc, op=ALU.mult),
                                            reads=[pb.res, ROPE.res], writes=[rt.res])
                                    qv = qk.t.rearrange("p (h two f) -> p h two f", h=8, two=2)
                                    P.op("pool", lambda qv=qv, rtv=rtv: nc.gpsimd.tensor_tensor(
                                        out=qv[:, :, 0, :], in0=rtv[:, 0], in1=rtv[:, 1], op=ALU.subtract),
                                        reads=[rt.res], writes=[qk.res])
                                    P.op("pool", lambda qv=qv, rtv=rtv: nc.gpsimd.tensor_tensor(
                                        out=qv[:, :, 1, :], in0=rtv[:, 2], in1=rtv[:, 3], op=ALU.add),
                                        reads=[rt.res], writes=[qk.res])

                                def do_tr(qk=qk, k2=k2, tc=tc):
                                    pt = banks[4 + k2]
                                    ptv = bank_bf(4 + k2)
                                    for j in range(4):
                                        P.op("pe", lambda qk=qk, j=j, ptv=ptv: nc.tensor.transpose(
                                            out=ptv[:, j * 128:(j + 1) * 128], in_=qk.t[:, j * 128:(j + 1) * 128], identity=ident[:]),
                                            reads=[qk.res, ident.res], writes=[pt.res], inc=(j == 3))
                                    P.op("act", lambda ptv=ptv, tc=tc: nc.scalar.copy(
                                        out=QTS[:, :, tc * 128:(tc + 1) * 128], in_=ptv[:, 0:512].rearrange("p (j t) -> p j t", j=4)),
                                        reads=[pt.res], writes=[QTS.res])
                                pending.append(do_tr)
                            elif kind == "v":
                                va = VA[k2]
                                P.op("act", lambda va=va, pb=pb: nc.scalar.copy(
                                    out=va.t[:, :, 0:128], in_=pb.t[:, :].rearrange("p (h f) -> p h f", h=4)),
                                    reads=[pb.res], writes=[va.res])
                                hb = v_base[cb]
                                P.dma("sp", lambda va=va, tc=tc, hb=hb: nc.sync.dma_start(
                                    out=VD[tc * 128:(tc + 1) * 128, hb:hb + 4, :], in_=va.t), reads=[va.res], writes=[rVD])
                            else:
                                gs = GS[k2]
                                P.op("act", lambda gs=gs, pb=pb: nc.scalar.activation(out=gs.t, in_=pb.t[:, :], func=AF.Silu),
                                     reads=[pb.res], writes=[gs.res])
                                P.dma("sp", lambda gs=gs, tc=tc, cb=cb: nc.sync.dma_start(
                                    out=GD[tc * 128:(tc + 1) * 128, (cb - 4) * 512:(cb - 3) * 512], in_=gs.t),
                                    reads=[gs.res], writes=[rGD])
                        if kind in ("q", "k"):
                            flush_pending()
                            qb_ = qt_base[cb]
                            P.dma("sp", lambda qb_=qb_: nc.sync.dma_start(
                                out=QT[qb_:qb_ + 4].rearrange("j p t -> p j t"), in_=QTS[:]), reads=[QTS.res], writes=[rQT])
                    P.barrier()

        env = Ctx()
        env.__dict__.update(dict(nc=nc, P=P, g=g, L=L, last=last, tc0=tc0, lam_init=lam_init, banks=banks, bank_bf=bank_bf,
                                 ident=ident, identf=identf, AFF=AFF, IDXU=IDXU, GATE=GATE, XS=XS, MOD=MOD, QT=QT, VD=VD,
                                 GD=GD, MG=MG, HT2=HT2, rXS=rXS, rMOD=rMOD, rQT=rQT, rVD=rVD, rGD=rGD, rMG=rMG,
                                 rHT2=rHT2, rIN=rIN, norm_phase=norm_phase, load_bc=load_bc,
                                 mod_gen=mod_gen, mod_tiles=mod_tiles,
                                 next_mod=(L + 1 if (L + 1 < depth and want('mod')) else None)))
        if want("attn"):
            phase_attn(env)
        if want("outproj"):
            phase_outproj(env)
        if want("norm2"):
            norm_phase(1, None, True, True)
        if want("topk"):
            phase_topk(env)
        if want("moe"):
            phase_moe(env)

    if final:
        with (SBT(nc, "fG", [128, D], F32) as fG_t, SBT(nc, "fX", [128, 2, D], F32) as fX_t,
              SBT(nc, "fJ", [128, D], BF16) as fJ_t, SBT(nc, "fS", [128, 2, 4], F32) as fS_t,
              SBT(nc, "fY", [128, 2, D], F32) as fY_t):
            G = Tile(fG_t, "fG")
            J = Tile(fJ_t, "fJ")
            X = [Tile(fX_t[:, i, :], f"fX{i}") for i in range(2)]
            Y = [Tile(fY_t[:, i, :], f"fY{i}") for i in range(2)]
            S = [Tile(fS_t[:, i, :], f"fS{i}") for i in range(2)]
            rOUT = Res("out")
            P.dma("sp", lambda: nc.sync.dma_start(out=G[:], in_=g.norm_final[0:1, :].to_broadcast([128, D])),
                  reads=[rIN], writes=[G.res])
            for tc in range(2, NCH):
                k = tc % 2
                x_, y_, s_ = X[k], Y[k], S[k]
                P.dma("sp", lambda x_=x_, tc=tc: nc.sync.dma_start(out=x_.t, in_=XS[tc * 128:(tc + 1) * 128, :]),
                      reads=[rXS], writes=[x_.res])
                P.op("act", lambda x_=x_, s_=s_: nc.scalar.activation(
                    out=J[:], in_=x_.t, func=AF.Square, accum_out=s_.t[:, 0:1]), reads=[x_.res], writes=[J.res, s_.res])
                P.op("dve", lambda s_=s_: nc.vector.tensor_scalar(
                    out=s_.t[:, 1:2], in0=s_.t[:, 0:1], scalar1=1.0 / D, scalar2=EPS, op0=ALU.mult, op1=ALU.add),
                    reads=[s_.res], writes=[s_.res])
                P.op("act", lambda s_=s_: nc.scalar.activation(out=s_.t[:, 2:3], in_=s_.t[:, 1:2], func=AF.Sqrt),
                     reads=[s_.res], writes=[s_.res])
                P.op("dve", lambda s_=s_: nc.vector.reciprocal(out=s_.t[:, 3:4], in_=s_.t[:, 2:3]),
                     reads=[s_.res], writes=[s_.res])
                P.op("dve", lambda x_=x_, y_=y_, s_=s_: nc.vector.scalar_tensor_tensor(
                    out=y_.t, in0=x_.t, scalar=s_.t[:, 3:4], in1=G[:], op0=ALU.mult, op1=ALU.mult),
                    reads=[x_.res, s_.res, G.res], writes=[y_.res])
                P.dma("sp", lambda y_=y_, tc=tc: nc.sync.dma_start(out=out[(tc - 2) * 128:(tc - 1) * 128, :], in_=y_.t),
                      reads=[y_.res], writes=[rOUT])
            P.barrier()
    else:
        rOUT = Res("out")
        P.dma("sp", lambda: nc.sync.dma_start(out=out[:, :], in_=XS[CTX:T, :]), reads=[rXS], writes=[rOUT])
        P.barrier()
    nc._prog_ninst = P.ninst
    nc._used_inputs = list(g.used.keys())
    return nc


def phase_attn(E):
    nc, P, g, L = E.nc, E.P, E.g, E.L
    banks = E.banks
    lam_init = E.lam_init
    with (SBT(nc, "aStrip", [128, 2, SW], F32) as strip_t, SBT(nc, "aTh", [128, 2, SW], F32) as th_t,
          SBT(nc, "aT1", [128, SW], F32) as t1_t,
          SBT(nc, "aLg", [128, 48], F32) as lg_t, SBT(nc, "aLv", [128, 256 + 128 + 8], F32) as lv_t,
          SBT(nc, "aRG", [128, 1024], F32) as rg_t, SBT(nc, "aDG", [128, 1024], F32) as dg_t,
          SBT(nc, "aQ", [128, 2, T], BF16) as q_t, SBT(nc, "aK", [128, 2, T], BF16) as k_t,
          SBT(nc, "aV", [128, 4, NCH, 130], BF16) as v_t, SBT(nc, "aE", [128, 6, 512], BF16) as e_t,
          SBT(nc, "aO", [128, 8, 129], F32) as o_t, SBT(nc, "aW", [128, 3, 4, 128], F32) as w_t,
          SBT(nc, "aSt", [128, 64], F32) as st_t, SBT(nc, "aG", [128, 4, 4, 128], F32) as gt_t,
          SBT(nc, "aM", [128, 4, 4, 128], BF16) as m_t):
        STRIP = Tile(strip_t, "strip")
        TH = [Tile(th_t[:, i, :], f"th{i}") for i in range(2)]
        T1 = Tile(t1_t, "t1")
        LG = Tile(lg_t, "lg")
        LV = Tile(lv_t, "lv")
        RG = Tile(rg_t, "rg")
        DG = Tile(dg_t, "dg")
        Q = [Tile(q_t[:, i, :], f"aq{i}") for i in range(2)]
        K = [Tile(k_t[:, i, :], f"ak{i}") for i in range(2)]
        V = [Tile(v_t[:, i], f"av{i}") for i in range(4)]
        EE = [Tile(e_t[:, i, :], f"ae{i}") for i in range(6)]
        O = Tile(o_t, "ao")
        W = [Tile(w_t[:, i], f"aw{i}") for i in range(3)]
        ST = Tile(st_t, "ast")
        GT = [Tile(gt_t[:, i], f"ag{i}") for i in range(4)]
        M = [Tile(m_t[:, i], f"am{i}") for i in range(4)]
        rIN = E.rIN
        P.dma("sp", lambda: nc.sync.dma_start(out=STRIP[:], in_=g.k_strip[:, :, :]), reads=[rIN], writes=[STRIP.res])
        P.dma("sp", lambda: nc.sync.dma_start(out=LG[:, 0:16], in_=g.ret_log_decay[L:L + 1, :].to_broadcast([128, 16])),
              reads=[rIN], writes=[LG.res])
        P.op("act", lambda: nc.scalar.activation(out=LG[:, 16:32], in_=LG[:, 0:16], func=AF.Exp), reads=[LG.res], writes=[LG.res])
        P.op("act", lambda: nc.scalar.activation(out=LG[:, 32:48], in_=LG[:, 16:32], func=AF.Ln, scale=-1.0, bias=1.0),
             reads=[LG.res], writes=[LG.res])
        P.dma("sp", lambda: nc.sync.dma_start(out=LV[:, 0:256], in_=g.diff_lambda[L:L + 1, :].to_broadcast([128, 256])),
              reads=[rIN], writes=[LV.res])
        lvv = LV[:, 0:256].rearrange("p (a f) -> p a f", a=4)
        prod = LV[:, 256:384].rearrange("p (a f) -> p a f", a=2)
        P.op("dve", lambda: nc.vector.tensor_tensor(out=prod[:, 0, :], in0=lvv[:, 0, :], in1=lvv[:, 1, :], op=ALU.mult),
             reads=[LV.res], writes=[LV.res])
        P.op("dve", lambda: nc.vector.tensor_tensor(out=prod[:, 1, :], in0=lvv[:, 2, :], in1=lvv[:, 3, :], op=ALU.mult),
             reads=[LV.res], writes=[LV.res])
        P.op("dve", lambda: nc.vector.tensor_reduce(out=LV[:, 384:386], in_=prod, axis=AX.X, op=ALU.add),
             reads=[LV.res], writes=[LV.res])
        P.op("act", lambda: nc.scalar.activation(out=LV[:, 386:388], in_=LV[:, 384:386], func=AF.Exp), reads=[LV.res], writes=[LV.res])
        P.op("dve", lambda: nc.vector.tensor_tensor(out=LV[:, 388:389], in0=LV[:, 387:388], in1=LV[:, 386:387], op=ALU.subtract),
             reads=[LV.res], writes=[LV.res])
        P.op("dve", lambda: nc.vector.tensor_scalar(out=LV[:, 389:390], in0=LV[:, 388:389], scalar1=-lam_init, scalar2=None, op0=ALU.add),
             reads=[LV.res], writes=[LV.res])
        NLAM = LV[:, 389:390]
        P.dma("sp", lambda: nc.sync.dma_start(out=RG[:], in_=g.ret_norm[L:L + 1, :].to_broadcast([128, 1024])), reads=[rIN], writes=[RG.res])
        P.dma("sp", lambda: nc.sync.dma_start(out=DG[:], in_=g.diff_norm[L:L + 1, :].to_broadcast([128, 1024])), reads=[rIN], writes=[DG.res])
        P.op("dve", lambda: nc.vector.tensor_scalar(out=DG[:], in0=DG[:], scalar1=1.0 - lam_init, scalar2=None, op0=ALU.mult),
             reads=[DG.res], writes=[DG.res])

        P.op("pool", lambda: nc.gpsimd.memset(ST[:, 56:64], -0.5), writes=[ST.res])
        ecnt = [0]
        scnt = [0]

        def small_rstd(nqc, var_lo, out_lo):
            P.op("pool", lambda: nc.gpsimd.tensor_tensor(out=ST[:, out_lo:out_lo + nqc], in0=ST[:, var_lo:var_lo + nqc],
                                                         in1=ST[:, 56:56 + nqc], op=ALU.pow), reads=[ST.res], writes=[ST.res])

        def store_mg(mt, nqc, col0, row0):
            P.dma("sp", lambda: nc.sync.dma_start(
                out=E.MG[row0:row0 + nqc * 128, col0:col0 + 128].rearrange("(c p) f -> p c f", p=128), in_=mt.t[:, 0:nqc, :]),
                reads=[mt.res], writes=[E.rMG])

        def evac_ret(ab, nqc, h, row0, gt, mt):
            W0, W1, W2 = W
            for qc in range(nqc):
                P.op("act", lambda qc=qc: nc.scalar.activation(out=W1.t[:, qc, :], in_=ab.t[:, qc * 128:(qc + 1) * 128], func=AF.Copy,
                                                               accum_out=ST[:, qc:qc + 1]), reads=[ab.res], writes=[W1.res, ST.res])
                P.op("act", lambda qc=qc: nc.scalar.activation(out=W2.t[:, qc, :], in_=ab.t[:, qc * 128:(qc + 1) * 128], func=AF.Square,
                                                               accum_out=ST[:, 8 + qc:9 + qc]), reads=[ab.res], writes=[W2.res, ST.res])
            P.op("pool", lambda: nc.gpsimd.tensor_scalar(out=ST[:, 16:16 + nqc], in0=ST[:, 0:nqc], scalar1=1.0 / 128, scalar2=0.0, op0=ALU.mult, op1=ALU.add),
                 reads=[ST.res], writes=[ST.res])
            P.op("pool", lambda: nc.gpsimd.tensor_tensor(out=ST[:, 24:24 + nqc], in0=ST[:, 16:16 + nqc], in1=ST[:, 16:16 + nqc], op=ALU.mult),
                 reads=[ST.res], writes=[ST.res])
            P.op("pool", lambda: nc.gpsimd.tensor_scalar(out=ST[:, 8:8 + nqc], in0=ST[:, 8:8 + nqc], scalar1=1.0 / 128, scalar2=EPS, op0=ALU.mult, op1=ALU.add),
                 reads=[ST.res], writes=[ST.res])
            P.op("pool", lambda: nc.gpsimd.tensor_tensor(out=ST[:, 24:24 + nqc], in0=ST[:, 8:8 + nqc], in1=ST[:, 24:24 + nqc], op=ALU.subtract),
                 reads=[ST.res], writes=[ST.res])
            small_rstd(nqc, 24, 40)
            P.op("pool", lambda: nc.gpsimd.tensor_tensor(out=ST[:, 48:48 + nqc], in0=ST[:, 16:16 + nqc], in1=ST[:, 40:40 + nqc], op=ALU.mult),
                 reads=[ST.res], writes=[ST.res])
            P.op("pool", lambda: nc.gpsimd.tensor_scalar(out=ST[:, 48:48 + nqc], in0=ST[:, 48:48 + nqc], scalar1=-1.0, scalar2=0.0, op0=ALU.mult, op1=ALU.add),
                 reads=[ST.res], writes=[ST.res])
            for qc in range(nqc):
                P.op("act", lambda qc=qc: nc.scalar.activation(out=W0.t[:, qc, :], in_=ab.t[:, qc * 128:(qc + 1) * 128], func=AF.Identity,
                                                               scale=ST[:, 40 + qc:41 + qc], bias=ST[:, 48 + qc:49 + qc]),
                     reads=[ab.res, ST.res], writes=[W0.res])
            gv = RG[:, h * 128:(h + 1) * 128].unsqueeze(1).to_broadcast([128, nqc, 128])
            P.op("pool", lambda: nc.gpsimd.tensor_tensor(out=W1.t[:, 0:nqc, :], in0=W0.t[:, 0:nqc, :], in1=gv, op=ALU.mult),
                 reads=[W0.res, RG.res], writes=[W1.res])
            P.op("pool", lambda: nc.gpsimd.tensor_tensor(out=mt.t[:, 0:nqc, :], in0=W1.t[:, 0:nqc, :], in1=gt.t[:, 0:nqc, :], op=ALU.mult),
                 reads=[W1.res, gt.res], writes=[mt.res])
            store_mg(mt, nqc, h * 128, row0)

        def evac_diff(nqc, h, row0, mt):
            W0, W1, W2 = W
            na = 2 * nqc
            for b3 in range((na + 2) // 3):
                n_in = min(3, na - b3 * 3)
                P.op("dve", lambda b3=b3, n_in=n_in: nc.vector.tensor_copy(
                    out=O[:, b3 * 3:b3 * 3 + n_in, :], in_=banks[4 + b3].t[:, 0:n_in * 129].rearrange("p (c f) -> p c f", c=n_in)),
                    reads=[banks[4 + b3].res], writes=[O.res])
            P.op("dve", lambda: nc.vector.reciprocal(out=ST[:, 48:48 + na], in_=O[:, 0:na, 128]), reads=[O.res], writes=[ST.res])
            P.op("dve", lambda: nc.vector.tensor_scalar(out=ST[:, 48 + nqc:48 + na], in0=ST[:, 48 + nqc:48 + na], scalar1=NLAM, scalar2=None,
                                                        op0=ALU.mult), reads=[ST.res, LV.res], writes=[ST.res])
            P.op("dve", lambda: nc.vector.tensor_tensor(
                out=W1.t[:, 0:nqc, :], in0=O[:, 0:nqc, 0:128], in1=ST[:, 48:48 + nqc].unsqueeze(2).to_broadcast([128, nqc, 128]), op=ALU.mult),
                reads=[O.res, ST.res], writes=[W1.res])
            P.op("dve", lambda: nc.vector.tensor_tensor(
                out=W2.t[:, 0:nqc, :], in0=O[:, nqc:na, 0:128], in1=ST[:, 48 + nqc:48 + na].unsqueeze(2).to_broadcast([128, nqc, 128]), op=ALU.mult),
                reads=[O.res, ST.res], writes=[W2.res])
            P.op("pool", lambda: nc.gpsimd.tensor_tensor(out=W0.t[:, 0:nqc, :], in0=W1.t[:, 0:nqc, :], in1=W2.t[:, 0:nqc, :], op=ALU.add),
                 reads=[W1.res, W2.res], writes=[W0.res])
            P.op("pool", lambda: nc.gpsimd.tensor_tensor(out=W2.t[:, 0:nqc, :], in0=W0.t[:, 0:nqc, :], in1=W0.t[:, 0:nqc, :], op=ALU.mult),
                 reads=[W0.res], writes=[W2.res])
            P.op("dve", lambda: nc.vector.tensor_reduce(out=ST[:, 16:16 + nqc], in_=W2.t[:, 0:nqc, :], axis=AX.X, op=ALU.add),
                 reads=[W2.res], writes=[ST.res])
            P.op("dve", lambda: nc.vector.tensor_scalar(out=ST[:, 24:24 + nqc], in0=ST[:, 16:16 + nqc], scalar1=1.0 / 128, scalar2=EPS,
                                                        op0=ALU.mult, op1=ALU.add), reads=[ST.res], writes=[ST.res])
            small_rstd(nqc, 24, 40)
            P.op("dve", lambda: nc.vector.tensor_tensor(
                out=W1.t[:, 0:nqc, :], in0=W0.t[:, 0:nqc, :], in1=ST[:, 40:40 + nqc].unsqueeze(2).to_broadcast([128, nqc, 128]), op=ALU.mult),
                reads=[W0.res, ST.res], writes=[W1.res])
            gv = DG[:, h * 128:(h + 1) * 128].unsqueeze(1).to_broadcast([128, nqc, 128])
            P.op("pool", lambda: nc.gpsimd.tensor_tensor(out=mt.t[:, 0:nqc, :], in0=W1.t[:, 0:nqc, :], in1=gv, op=ALU.mult),
                 reads=[W1.res, DG.res], writes=[mt.res])
            store_mg(mt, nqc, 1024 + h * 128, row0)

        rcnt = [0]
        units = [("ret", c) for c in range(4)] + [("diff", h) for h in range(8)]
        for ui, (ukind, uidx) in enumerate(units):
            is_ret = ukind == "ret"
            k2 = ui % 2
            q_, k_ = Q[k2], K[k2]
            if is_ret:
                heads = [2 * uidx, 2 * uidx + 1]
                qsrc, ksrc = E.QT[uidx, :, :], E.QT[4 + uidx, :, :]
                vhead = [heads[0], heads[1]]
            else:
                heads = [uidx, uidx]
                qsrc, ksrc = E.QT[8 + uidx, :, :], E.QT[16 + uidx, :, :]
                vhead = [8 + uidx]
            P.dma("sp", lambda: nc.sync.dma_start(out=q_.t[:, :], in_=qsrc), reads=[E.rQT], writes=[q_.res])
            P.dma("sp", lambda: nc.sync.dma_start(out=k_.t[:, :], in_=ksrc), reads=[E.rQT], writes=[k_.res])
            vs = []
            for vi, vh in enumerate(vhead):
                v_ = V[k2 * 2 + vi]
                P.dma("sp", lambda v_=v_, vh=vh: nc.sync.dma_start(out=v_.t, in_=E.VD[:, vh, :].rearrange("(c p) e -> p c e", p=128)),
                      reads=[E.rVD], writes=[v_.res])
                vs.append(v_)
            if not is_ret:
                vs.append(vs[0])
            if is_ret:
                for pr in range(2):
                    h = heads[pr]
                    th = TH[pr]
                    P.op("dve", lambda h=h: nc.vector.tensor_scalar(out=T1[:], in0=STRIP[:, 0, :], scalar1=LG[:, 32 + h:33 + h], scalar2=None, op0=ALU.mult),
                         reads=[STRIP.res, LG.res], writes=[T1.res])
                    P.op("dve", lambda h=h: nc.vector.scalar_tensor_tensor(out=T1[:], in0=STRIP[:, 1, :], scalar=LG[:, 40 + h:41 + h], in1=T1[:],
                                                                          op0=ALU.mult, op1=ALU.add), reads=[STRIP.res, LG.res, T1.res], writes=[T1.res])
                    P.op("act", lambda th=th: nc.scalar.activation(out=th.t, in_=T1[:], func=AF.Exp), reads=[T1.res], writes=[th.res])
            groups = []
            if not E.last:
                ent = [(0, 0), (1, 128)]
                groups.append((0, 0, 256, ent, 0))
            for qb in range(4):
                if is_ret:
                    ent = [(0, -256), (1, -128)] + [(2 + j, j * 128) for j in range(16)] + [(0, 2048), (1, 2176)]
                else:
                    ent = [(j, 0) for j in range(NCH)]
                groups.append((CTX + qb * 512, CTX + qb * 512, 512, ent, qb * 512))
            for gi, (row0, qcol0, nq, ent, qpos0) in enumerate(groups):
                nqc = nq // 128
                gts = [GT[(gi % 2) * 2 + pr] for pr in range(2)]
                mts = [M[(gi % 2) * 2 + pr] for pr in range(2)]
                if is_ret:
                    for pr in range(2):
                        h = heads[pr]
                        P.dma("sp", lambda pr=pr, h=h: nc.sync.dma_start(
                            out=gts[pr].t[:, 0:nqc, :], in_=E.GD[row0:row0 + nq, h * 128:(h + 1) * 128].rearrange("(c p) f -> p c f", p=128)),
                            reads=[E.rGD], writes=[gts[pr].res])
                    rbank = [banks[4 + (rcnt[0] % 2) * 2 + pr] for pr in range(2)]
                    rcnt[0] += 1

                def emit_S(ei):
                    kc, pos = ent[ei]
                    outs = []
                    for pr in range(2):
                        bi = scnt[0] % 4
                        scnt[0] += 1
                        sb_ = banks[bi]
                        lhs = k_.t[pr * 64:(pr + 1) * 64, kc * 128:(kc + 1) * 128]
                        rhs = q_.t[pr * 64:(pr + 1) * 64, qcol0:qcol0 + nq]
                        P.op("pe", lambda sb_=sb_, lhs=lhs, rhs=rhs: nc.tensor.matmul(sb_.t[:, 0:nq], lhsT=lhs, rhs=rhs, start=True, stop=True),
                             reads=[k_.res, q_.res], writes=[sb_.res])
                        outs.append(sb_)
                    return outs

                pend_S = emit_S(0)
                for ei in range(len(ent)):
                    kc, pos = ent[ei]
                    cur_S = pend_S
                    if ei + 1 < len(ent):
                        pend_S = emit_S(ei + 1)
                    es = []
                    for pr in range(2):
                        et = EE[ecnt[0] % 6]
                        ecnt[0] += 1
                        sb_ = cur_S[pr]
                        if is_ret:
                            off = qpos0 - pos + 2176
                            th = TH[pr]
                            P.op("dve", lambda et=et, sb_=sb_, off=off, th=th: nc.vector.tensor_tensor(
                                out=et.t[:, 0:nq], in0=sb_.t[:, 0:nq], in1=th.t[:, off:off + nq], op=ALU.mult),
                                reads=[sb_.res, th.res], writes=[et.res])
                        else:
                            P.op("act", lambda et=et, sb_=sb_: nc.scalar.activation(out=et.t[:, 0:nq], in_=sb_.t[:, 0:nq], func=AF.Exp),
                                 reads=[sb_.res], writes=[et.res])
                        es.append(et)
                    for pr in range(2):
                        v_ = vs[pr]
                        for qc in range(nqc):
                            if is_ret:
                                ab = rbank[pr]
                                dst = ab.t[:, qc * 128:(qc + 1) * 128]
                                rhs = v_.t[:, kc, 0:128]
                                st_flag = (ei == 0 and qc == 0)
                            else:
                                a = pr * nqc + qc
                                ab = banks[4 + a // 3]
                                sl = a % 3
                                dst = ab.t[:, sl * 129:(sl + 1) * 129]
                                rhs = v_.t[:, kc, 0:129]
                                st_flag = (ei == 0 and sl == 0)
                            lastmm = (ei == len(ent) - 1)
                            P.op("pe", lambda dst=dst, rhs=rhs, et=es[pr], qc=qc, st_flag=st_flag, lastmm=lastmm: nc.tensor.matmul(
                                dst, lhsT=et.t[:, qc * 128:(qc + 1) * 128], rhs=rhs, start=st_flag, stop=lastmm, skip_group_check=True),
                                reads=[es[pr].res, v_.res], writes=[ab.res], inc=(lastmm or qc == nqc - 1))
                if is_ret:
                    for pr in range(2):
                        evac_ret(rbank[pr], nqc, heads[pr], row0, gts[pr], mts[pr])
                else:
                    evac_diff(nqc, uidx, row0, mts[0])
        P.barrier()


def phase_outproj(E):
    nc, P, g, L = E.nc, E.P, E.g, E.L
    banks, bank_bf = E.banks, E.bank_bf
    with (SBT(nc, "oW", [128, 16, D], BF16) as ow_t, SBT(nc, "oG", [128, 2, D], F32) as og_t,
          SBT(nc, "oM", [128, 2, D], BF16) as om_t, SBT(nc, "oMT", [128, 2, 16, 128], BF16) as omt_t,
          SBT(nc, "oX", [128, 2, D], F32) as ox_t, SBT(nc, "oT", [128, 2, 512], F32) as ot_t):
        Wo = Tile(ow_t, "oW")
        G2 = [Tile(og_t[:, i, :], f"oG{i}") for i in range(2)]
        Mc = [Tile(om_t[:, i, :], f"oM{i}") for i in range(2)]
        MT = [Tile(omt_t[:, i], f"oMT{i}") for i in range(2)]
        X = [Tile(ox_t[:, i, :], f"oX{i}") for i in range(2)]
        Tm = [Tile(ot_t[:, i, :], f"oT{i}") for i in range(2)]
        src = g.w_out[L].rearrange("(c p) n -> p c n", p=128)
        for q4 in range(4):
            P.dma("pool", lambda q4=q4: nc.gpsimd.dma_start(out=Wo[:, q4 * 4:(q4 + 1) * 4, :], in_=src[:, q4 * 4:(q4 + 1) * 4, :]),
                  reads=[E.rIN], writes=[Wo.res])
        E.load_bc(G2[1], E.MOD[0:1, 2 * D:3 * D])
        E.load_bc(G2[0], E.MOD[1:2, 2 * D:3 * D])
        cnt = 0

        def stage_a(tc):
            k = tc % 2
            m_, mt_, x_ = Mc[k], MT[k], X[k]
            P.dma("sp", lambda: nc.sync.dma_start(out=m_.t, in_=E.MG[tc * 128:(tc + 1) * 128, :]), reads=[E.rMG], writes=[m_.res])
            P.dma("sp", lambda: nc.sync.dma_start(out=x_.t, in_=E.XS[tc * 128:(tc + 1) * 128, :]), reads=[E.rXS], writes=[x_.res])
            for half in range(2):
                bi = (tc % 2) * 2 + half
                pb, pbv = banks[bi], bank_bf(bi)
                for j in range(8):
                    dc = half * 8 + j
                    P.op("pe", lambda dc=dc, j=j, pbv=pbv: nc.tensor.transpose(
                        out=pbv[:, j * 128:(j + 1) * 128], in_=m_.t[:, dc * 128:(dc + 1) * 128], identity=E.ident[:]),
                        reads=[m_.res, E.ident.res], writes=[pb.res], inc=(j == 7))
                dst = mt_.t[:, half * 8:(half + 1) * 8, :]
                if half == 0:
                    P.op("act", lambda dst=dst, pbv=pbv: nc.scalar.copy(out=dst, in_=pbv.rearrange("p (j t) -> p j t", j=8)),
                         reads=[pb.res], writes=[mt_.res])
                else:
                    P.op("dve", lambda dst=dst, pbv=pbv: nc.vector.tensor_copy(out=dst, in_=pbv.rearrange("p (j t) -> p j t", j=8)),
                         reads=[pb.res], writes=[mt_.res])

        tcs = list(range(E.tc0, NCH))
        stage_a(tcs[0])
        for ti, tc in enumerate(tcs):
            k = tc % 2
            isx = 1 if tc >= 2 else 0
            m_, mt_, x_ = Mc[k], MT[k], X[k]
            for db in range(4):
                pb = banks[4 + cnt % 4]
                tm = Tm[cnt % 2]
                cnt += 1
                for mc in range(16):
                    P.op("pe", lambda mc=mc, pb=pb, db=db: nc.tensor.matmul(
                        pb.t[:, :], lhsT=mt_.t[:, mc, :], rhs=Wo[:, mc, db * 512:(db + 1) * 512], start=(mc == 0), stop=(mc == 15)),
                        reads=[mt_.res, Wo.res], writes=[pb.res], inc=(mc == 15))
                if db == 0 and ti + 1 < len(tcs):
                    stage_a(tcs[ti + 1])
                P.op("dve", lambda pb=pb, tm=tm, db=db: nc.vector.tensor_tensor(
                    out=tm.t, in0=pb.t[:, :], in1=G2[isx].t[:, db * 512:(db + 1) * 512], op=ALU.mult),
                    reads=[pb.res, G2[isx].res], writes=[tm.res])
                P.op("pool", lambda tm=tm, db=db: nc.gpsimd.tensor_tensor(
                    out=x_.t[:, db * 512:(db + 1) * 512], in0=x_.t[:, db * 512:(db + 1) * 512], in1=tm.t, op=ALU.add),
                    reads=[tm.res, x_.res], writes=[x_.res])
            P.dma("sp", lambda: nc.sync.dma_start(out=E.XS[tc * 128:(tc + 1) * 128, :], in_=x_.t), reads=[x_.res], writes=[E.rXS])
        P.barrier()


def phase_topk(E):
    import contextlib
    with contextlib.ExitStack() as stack:
        E.mod_alloc = None
        if E.next_mod is not None:
            nc = E.nc
            E.mod_alloc = (stack.enter_context(SBT(nc, "mw0", [128, 16, 512], BF16)),
                           stack.enter_context(SBT(nc, "mw1", [128, 16, 512], BF16)),
                           stack.enter_context(SBT(nc, "mb", [2, 2, 512], F32)),
                           stack.enter_context(SBT(nc, "mo", [2, 2, 512], F32)))
        _phase_topk(E)


def _phase_topk(E):
    nc, P, g, L = E.nc, E.P, E.g, E.L
    banks = E.banks
    AFF, IDXU, GATE, identf = E.AFF, E.IDXU, E.GATE, E.identf
    with (SBT(nc, "kAT", [16, T], F32) as at_t, SBT(nc, "kWK", [16, T], F32) as wk_t,
          SBT(nc, "kM8", [16, 16], F32) as m8_t, SBT(nc, "kMK", [16, T], F32) as mk_t,
          SBT(nc, "kPS", [16, T], F32) as ps_t, SBT(nc, "kON", [16, T], F32) as on_t,
          SBT(nc, "kSL", [128, NCH, NE], F32) as sl_t, SBT(nc, "kPm", [128, 3, 256], F32) as pm_t,
          SBT(nc, "kTG", [128, NCH, NE, 2], F32) as tg_t, SBT(nc, "kIO", [128, NSLOT], F32) as io_t,
          SBT(nc, "kTK", [128, NCH], F32) as tk_t, SBT(nc, "kIG", [128, NE, 3, 2], F32) as ig_t):
        AT, WK, M8, MK, PS, ON = (Tile(t, n) for t, n in ((at_t, "kAT"), (wk_t, "kWK"), (m8_t, "kM8"), (mk_t, "kMK"), (ps_t, "kPS"), (on_t, "kON")))
        SL, TG, IO, TK, IG = (Tile(t, n) for t, n in ((sl_t, "kSL"), (tg_t, "kTG"), (io_t, "kIO"), (tk_t, "kTK"), (ig_t, "kIG")))
        PM = [Tile(pm_t[:, i, :], f"kPm{i}") for i in range(3)]
        mgen = None
        if E.next_mod is not None:
            w0, w1, mb_t, mo_t = E.mod_alloc
            ws, mb, mo = E.mod_tiles(w0, w1, mb_t, mo_t)
            mgen = E.mod_gen(E.next_mod, ws, mb, mo, [banks[7]])

        def mod_step(n=1):
            if mgen is not None:
                for _ in range(n):
                    next(mgen, None)

        P.dma("sp", lambda: nc.sync.dma_start(out=IO[:], in_=g.k_iota[:, :]), reads=[E.rIN], writes=[IO.res])
        P.dma("sp", lambda: nc.sync.dma_start(out=TK[:], in_=g.k_tokid[:, :]), reads=[E.rIN], writes=[TK.res])
        P.op("pool", lambda: nc.gpsimd.memset(ON[:], 1.0), writes=[ON.res])
        P.op("pool", lambda: nc.gpsimd.memset(IG[:], 0.0), writes=[IG.res])
        tcs = list(range(E.tc0, NCH))
        for gi in range(0, len(tcs), 4):
            grp = tcs[gi:gi + 4]
            pb = banks[(gi // 4) % 2]
            for j, tc in enumerate(grp):
                P.op("pe", lambda j=j, tc=tc, pb=pb: nc.tensor.transpose(
                    out=pb.t[0:16, j * 128:(j + 1) * 128], in_=AFF[:, tc, :], identity=identf[:]),
                    reads=[AFF.res, identf.res], writes=[pb.res], inc=(j == len(grp) - 1))
            c0 = grp[0] * 128
            n = len(grp) * 128
            P.op("act", lambda pb=pb, c0=c0, n=n: nc.scalar.copy(out=AT[:, c0:c0 + n], in_=pb.t[0:16, 0:n]), reads=[pb.res], writes=[AT.res])
        ranges = [(CTX, T, CAPX, 0)] + ([] if E.last else [(0, CTX, CAPC, 1)])
        for (a, b, cap, ti) in ranges:
            P.op("dve", lambda a=a, b=b: nc.vector.tensor_copy(out=WK[:, a:b], in_=AT[:, a:b]), reads=[AT.res], writes=[WK.res])
            nr = cap // 8
            for r in range(nr):
                P.op("dve", lambda a=a, b=b, ti=ti: nc.vector.max(out=M8[:, ti * 8:ti * 8 + 8], in_=WK[:, a:b]), reads=[WK.res], writes=[M8.res])
                mod_step(1)
                if r < nr - 1:
                    P.op("dve", lambda a=a, b=b, ti=ti: nc.vector.match_replace(
                        out=WK[:, a:b], in_to_replace=M8[:, ti * 8:ti * 8 + 8], in_values=WK[:, a:b], imm_value=-1.0),
                        reads=[WK.res, M8.res], writes=[WK.res])
            thr = M8[:, ti * 8 + 7:ti * 8 + 8]
            P.op("dve", lambda a=a, b=b, thr=thr: nc.vector.tensor_scalar(out=MK[:, a:b], in0=AT[:, a:b], scalar1=thr, scalar2=None, op0=ALU.is_ge),
                 reads=[AT.res, M8.res], writes=[MK.res])
            P.op("dve", lambda a=a, b=b: nc.vector.tensor_tensor_scan(out=PS[:, a:b], data0=ON[:, a:b], data1=MK[:, a:b], initial=0.0,
                                                                       op0=ALU.mult, op1=ALU.add), reads=[ON.res, MK.res], writes=[PS.res])
            if ti == 1:
                P.op("dve", lambda a=a, b=b: nc.vector.tensor_scalar(out=PS[:, a:b], in0=PS[:, a:b], scalar1=float(CAPX), scalar2=None, op0=ALU.add),
                     reads=[PS.res], writes=[PS.res])
            P.op("dve", lambda a=a, b=b: nc.vector.tensor_tensor(out=PS[:, a:b], in0=PS[:, a:b], in1=MK[:, a:b], op=ALU.mult),
                 reads=[PS.res, MK.res], writes=[PS.res])
            P.op("dve", lambda a=a, b=b: nc.vector.tensor_scalar(out=PS[:, a:b], in0=PS[:, a:b], scalar1=-1.0, scalar2=None, op0=ALU.add),
                 reads=[PS.res], writes=[PS.res])
        for gi in range(0, len(tcs), 8):
            grp = tcs[gi:gi + 8]
            pb = banks[2 + (gi // 8) % 2]
            for j, tc in enumerate(grp):
                P.op("pe", lambda j=j, tc=tc, pb=pb: nc.tensor.transpose(
                    out=pb.t[:, j * 16:(j + 1) * 16], in_=PS[:, tc * 128:(tc + 1) * 128], identity=identf[0:16, 0:16]),
                    reads=[PS.res, identf.res], writes=[pb.res], inc=(j == len(grp) - 1))
            P.op("act", lambda pb=pb, grp=grp: nc.scalar.copy(
                out=SL[:, grp[0]:grp[0] + len(grp), :], in_=pb.t[:, 0:len(grp) * 16].rearrange("p (c e) -> p c e", e=16)),
                reads=[pb.res], writes=[SL.res])
        P.op("dve", lambda: nc.vector.tensor_copy(out=TG[:, :, :, 0], in_=TK[:, :].unsqueeze(2).to_broadcast([128, NCH, NE])),
             reads=[TK.res], writes=[TG.res])
        P.op("dve", lambda: nc.vector.tensor_copy(out=TG[:, :, :, 1], in_=AFF[:]), reads=[AFF.res, TG.res], writes=[TG.res])
        pc = 0
        bA, bB, bC = banks[4], banks[5], banks[6]
        for e in range(NE):
            for tc in tcs:
                pm = PM[pc % 3]
                pc += 1
                if tc >= 2:
                    P.op("dve", lambda pm=pm, tc=tc: nc.vector.tensor_scalar(
                        out=pm.t[:, 0:256], in0=IO[:, 0:256], scalar1=SL[:, tc, e:e + 1], scalar2=None, op0=ALU.is_equal),
                        reads=[IO.res, SL.res], writes=[pm.res])
                    for cc, bb in ((0, bA), (1, bB)):
                        P.op("pe", lambda pm=pm, tc=tc, cc=cc, bb=bb: nc.tensor.matmul(
                            bb.t[:, e * 2:(e + 1) * 2], lhsT=pm.t[:, cc * 128:(cc + 1) * 128], rhs=TG[:, tc, e, :],
                            start=(tc == 2), stop=(tc == NCH - 1), skip_group_check=True),
                            reads=[pm.res, TG.res], writes=[bb.res], inc=(cc == 1))
                else:
                    P.op("dve", lambda pm=pm, tc=tc: nc.vector.tensor_scalar(
                        out=pm.t[:, 0:32], in0=IO[:, 256:288], scalar1=SL[:, tc, e:e + 1], scalar2=None, op0=ALU.is_equal),
                        reads=[IO.res, SL.res], writes=[pm.res])
                    P.op("pe", lambda pm=pm, tc=tc: nc.tensor.matmul(
                        bC.t[0:32, e * 2:(e + 1) * 2], lhsT=pm.t[:, 0:32], rhs=TG[:, tc, e, :],
                        start=(tc == 0), stop=(tc == 1), skip_group_check=True),
                        reads=[pm.res, TG.res], writes=[bC.res])
        mod_step(24)
        P.op("act", lambda: nc.scalar.copy(out=IG[:, :, 0, :], in_=bA.t[:, 0:32].rearrange("p (e two) -> p e two", two=2)),
             reads=[bA.res, IG.res], writes=[IG.res])
        P.op("act", lambda: nc.scalar.copy(out=IG[:, :, 1, :], in_=bB.t[:, 0:32].rearrange("p (e two) -> p e two", two=2)),
             reads=[bB.res, IG.res], writes=[IG.res])
        if not E.last:
            P.op("act", lambda: nc.scalar.copy(out=IG[0:32, :, 2, :], in_=bC.t[0:32, 0:32].rearrange("p (e two) -> p e two", two=2)),
                 reads=[bC.res, IG.res], writes=[IG.res])
        P.op("dve", lambda: nc.vector.tensor_copy(out=IDXU[:], in_=IG[:, :, :, 0]), reads=[IG.res], writes=[IDXU.res])
        P.op("dve", lambda: nc.vector.tensor_copy(out=GATE[:], in_=IG[:, :, :, 1]), reads=[IG.res], writes=[GATE.res])
        P.barrier()


def phase_moe(E):
    nc, P, g, L = E.nc, E.P, E.g, E.L
    banks, bank_bf = E.banks, E.bank_bf
    IDXU, GATE = E.IDXU, E.GATE
    ncc = 2 if E.last else 3
    nsl = CAPX if E.last else NSLOT
    with (SBT(nc, "eG5", [128, 2, D], F32) as g5_t, SBT(nc, "eXG", [128, 2, 3, D], BF16) as xg_t,
          SBT(nc, "eXT", [128, 2, 16, NSLOT], BF16) as xt_t, SBT(nc, "eGU", [128, 2, 2, 16, 512], BF16) as gu_t,
          SBT(nc, "eWD", [128, 8, D], BF16) as wd_t, SBT(nc, "eHT", [128, 8, NSLOT], BF16) as ht_t,
          SBT(nc, "eSG", [128, 2, 512], F32) as sg_t, SBT(nc, "eHS", [128, 3, FF], BF16) as hs_t, SBT(nc, "eY", [128, 2, D], F32) as y_t):
        G5 = [Tile(g5_t[:, i, :], f"eG5{i}") for i in range(2)]
        XG = [Tile(xg_t[:, i], f"eXG{i}") for i in range(2)]
        XT = [Tile(xt_t[:, i], f"eXT{i}") for i in range(2)]
        GU = [Tile(gu_t[:, i], f"eGU{i}") for i in range(2)]
        WD = Tile(wd_t, "eWD")
        HT = Tile(ht_t, "eHT")
        HS = Tile(hs_t, "eHS")
        SG = [Tile(sg_t[:, i, :], f"eSG{i}") for i in range(2)]
        Y = [Tile(y_t[:, i, :], f"eY{i}") for i in range(2)]
        E.load_bc(G5[1], E.MOD[0:1, 5 * D:6 * D])
        E.load_bc(G5[0], E.MOD[1:2, 5 * D:6 * D])
        ycnt = 0
        bcnt = 0

        def issue_gather(e):
            xg = XG[e % 2]
            for cc in range(ncc):
                M_ = 128 if cc < 2 else CAPC
                P.dma("pool", lambda cc=cc, M_=M_: nc.gpsimd.indirect_dma_start(
                    out=xg.t[0:M_, cc, :], out_offset=None, in_=E.HT2[:, :],
                    in_offset=bass.IndirectOffsetOnAxis(ap=IDXU[0:M_, e, cc:cc + 1], axis=0)),
                    reads=[IDXU.res, E.rHT2], writes=[xg.res])

        def issue_gu(e, fh):
            gu = GU[fh]
            for wi, wsrc in enumerate((g.w_gate, g.w_up)):
                src_ = wsrc[L, e][:, fh * 512:(fh + 1) * 512].rearrange("(c p) n -> p c n", p=128)
                for hh in range(2):
                    P.dma("pool", lambda wi=wi, src_=src_, hh=hh: nc.gpsimd.dma_start(
                        out=gu.t[:, wi, hh * 8:(hh + 1) * 8, :], in_=src_[:, hh * 8:(hh + 1) * 8, :]), reads=[E.rIN], writes=[gu.res])

        def issue_wd(e):
            srcd = g.w_down[L, e].rearrange("(c p) n -> p c n", p=128)
            for hh in range(2):
                P.dma("pool", lambda hh=hh: nc.gpsimd.dma_start(out=WD[:, hh * 4:(hh + 1) * 4, :], in_=srcd[:, hh * 4:(hh + 1) * 4, :]),
                      reads=[E.rIN], writes=[WD.res])

        def do_transposes(e):
            xg, xt = XG[e % 2], XT[e % 2]
            for dc in range(16):
                bi = dc % 2
                pb, pbv = banks[bi], bank_bf(bi)
                for cc in range(ncc):
                    M_ = 128 if cc < 2 else CAPC
                    P.op("pe", lambda cc=cc, M_=M_, dc=dc, pbv=pbv: nc.tensor.transpose(
                        out=pbv[:, cc * 128:cc * 128 + M_], in_=xg.t[0:M_, cc, dc * 128:(dc + 1) * 128], identity=E.ident[0:M_, 0:M_]),
                        reads=[xg.res, E.ident.res], writes=[pb.res], inc=(cc == ncc - 1))
                if dc % 2 == 0:
                    P.op("act", lambda dc=dc, pbv=pbv: nc.scalar.copy(out=xt.t[:, dc, 0:nsl], in_=pbv[:, 0:nsl]), reads=[pb.res], writes=[xt.res])
                else:
                    P.op("dve", lambda dc=dc, pbv=pbv: nc.vector.tensor_copy(out=xt.t[:, dc, 0:nsl], in_=pbv[:, 0:nsl]), reads=[pb.res], writes=[xt.res])

        issue_gather(0)
        issue_gu(0, 0)
        issue_gu(0, 1)
        issue_wd(0)
        do_transposes(0)
        for e in range(NE):
            xt = XT[e % 2]
            if e + 1 < NE:
                issue_gather(e + 1)
            for fh in range(2):
                gu = GU[fh]
                for sc in range(ncc):
                    M_ = 128 if sc < 2 else CAPC
                    pg, pu = banks[2 + (bcnt % 2) * 2], banks[3 + (bcnt % 2) * 2]
                    sg = SG[bcnt % 2]
                    bcnt += 1
                    for wi, pb in ((0, pg), (1, pu)):
                        for dc in range(16):
                            P.op("pe", lambda wi=wi, pb=pb, dc=dc, gu=gu, sc=sc, M_=M_: nc.tensor.matmul(
                                pb.t[0:M_, :], lhsT=xt.t[:, dc, sc * 128:sc * 128 + M_], rhs=gu.t[:, wi, dc, :],
                                start=(dc == 0), stop=(dc == 15)), reads=[gu.res, xt.res], writes=[pb.res], inc=(dc == 15))
                    P.op("act", lambda pg=pg, sg=sg, M_=M_: nc.scalar.activation(out=sg.t[0:M_, :], in_=pg.t[0:M_, :], func=AF.Silu),
                         reads=[pg.res], writes=[sg.res])
                    P.op("dve", lambda pu=pu, sg=sg, fh=fh, sc=sc, M_=M_: nc.vector.tensor_tensor(
                        out=HS[0:M_, sc, fh * 512:(fh + 1) * 512], in0=pu.t[0:M_, :], in1=sg.t[0:M_, :], op=ALU.mult),
                        reads=[pu.res, sg.res], writes=[HS.res])
                if e + 1 < NE:
                    issue_gu(e + 1, fh)
                    if fh == 1:
                        do_transposes(e + 1)
            for fc in range(8):
                bi = fc % 2
                pb, pbv = banks[bi], bank_bf(bi)
                for sc in range(ncc):
                    M_ = 128 if sc < 2 else CAPC
                    P.op("pe", lambda sc=sc, M_=M_, fc=fc, pbv=pbv: nc.tensor.transpose(
                        out=pbv[:, sc * 128:sc * 128 + M_], in_=HS[0:M_, sc, fc * 128:(fc + 1) * 128], identity=E.ident[0:M_, 0:M_]),
                        reads=[HS.res, E.ident.res], writes=[pb.res], inc=(sc == ncc - 1))
                if fc % 2 == 0:
                    P.op("act", lambda fc=fc, pbv=pbv: nc.scalar.copy(out=HT[:, fc, 0:nsl], in_=pbv[:, 0:nsl]), reads=[pb.res], writes=[HT.res])
                else:
                    P.op("dve", lambda fc=fc, pbv=pbv: nc.vector.tensor_copy(out=HT[:, fc, 0:nsl], in_=pbv[:, 0:nsl]), reads=[pb.res], writes=[HT.res])
            for cc in range(ncc):
                M_ = 128 if cc < 2 else CAPC
                isx = 1 if cc < 2 else 0
                y = Y[ycnt % 2]
                ycnt += 1
                for db in range(4):
                    pb = banks[6 + db % 2]
                    for fc in range(8):
                        P.op("pe", lambda fc=fc, pb=pb, db=db, cc=cc, M_=M_: nc.tensor.matmul(
                            pb.t[0:M_, :], lhsT=HT[:, fc, cc * 128:cc * 128 + M_], rhs=WD[:, fc, db * 512:(db + 1) * 512],
                            start=(fc == 0), stop=(fc == 7)), reads=[HT.res, WD.res], writes=[pb.res], inc=(fc == 7))
                    P.op("dve", lambda pb=pb, db=db, cc=cc, M_=M_, y=y, isx=isx: nc.vector.scalar_tensor_tensor(
                        out=y.t[0:M_, db * 512:(db + 1) * 512], in0=pb.t[0:M_, :], scalar=GATE[0:M_, e, cc:cc + 1],
                        in1=G5[isx].t[0:M_, db * 512:(db + 1) * 512], op0=ALU.mult, op1=ALU.mult),
                        reads=[pb.res, GATE.res, G5[isx].res], writes=[y.res])
                P.dma("pool", lambda cc=cc, M_=M_, y=y: nc.gpsimd.indirect_dma_start(
                    out=E.XS[:, :], out_offset=bass.IndirectOffsetOnAxis(ap=IDXU[0:M_, e, cc:cc + 1], axis=0),
                    in_=y.t[0:M_, :], in_offset=None, compute_op=ALU.add),
                    reads=[y.res, IDXU.res, E.rXS], writes=[E.rXS], serial=True)
            if e + 1 < NE:
                issue_wd(e + 1)
        P.barrier()


NCORES = 4


def kernel(x, c, ctx, c_ctx, w_ada, b_ada, norm_mix, norm_ffn, w_in, w_out, ret_log_decay,
           ret_norm, diff_lambda, diff_norm, w_router, w_gate, w_up, w_down, norm_final):
    f = lambda a: np.ascontiguousarray(np.asarray(a, dtype=np.float32))
    nc = build(depth=DEPTH)
    shared = dict(
        w_ada=f(w_ada), b_ada=f(b_ada), norm_mix=f(norm_mix), norm_ffn=f(norm_ffn), w_in=f(w_in), w_out=f(w_out),
        ret_log_decay=f(ret_log_decay).reshape(DEPTH, 16), ret_norm=f(ret_norm),
        diff_lambda=f(diff_lambda).reshape(DEPTH, 256), diff_norm=f(diff_norm), w_router=f(w_router),
        w_gate=f(w_gate), w_up=f(w_up), w_down=f(w_down), norm_final=f(norm_final).reshape(1, D))
    shared.update(host_consts())
    x, c, ctx, c_ctx = f(x), f(c), f(ctx), f(c_ctx)
    in_maps = []
    for b in range(NCORES):
        m = dict(shared)
        m.update(x=x[b], ctx=ctx[b], cvec=np.ascontiguousarray(np.stack([c[b], c_ctx])))
        in_maps.append({k: m[k] for k in nc._used_inputs})
    res = run_bass_kernel_spmd(nc, in_maps, core_ids=list(range(NCORES)))
    return np.stack([np.asarray(res.results[b]["out"], dtype=np.float32) for b in range(NCORES)])
```
